# Optimizing a Trainium2 kernel written in Bass

```python
import jax
import jax.numpy as jnp
from jax import lax
import numpy as np

D_MODEL = 1024
BATCH = 8
SEQ = 2048
DEPTH = 2

GRID_W = 64
CTX_LEN = 256

RET_HEADS = 4
RET_HD = 128
RET_W = RET_HEADS * RET_HD
RET_CHUNK = 128
RET_GN_EPS = 1e-5

NA_HEADS = 8
NA_HD = 64
NA_W = NA_HEADS * NA_HD
NA_KR_MAX = 8
NA_KC = 16

MIX_W = RET_W + NA_W
IN_W = 4 * RET_W + 3 * NA_W

RWKV_HD = 64
RWKV_HEADS = D_MODEL // RWKV_HD
DECAY_LORA = 64
AAA_LORA = 64
GATE_LORA = 160
RWKV_GN_EPS = 64e-5

FFN_HIDDEN = 2816
N_EXPERTS = 8
TOP_K = 2
EXPERT_HIDDEN = 3584

N_EVEN = (DEPTH + 1) // 2
N_ODD = DEPTH // 2

ROPE_BASE = 10000.0
NORM_EPS = 1e-6
F32 = jnp.float32

kernel_name = 'hybrid_retention_natten_rwkv7_moe_dit'


def rms_norm(x, w):
    x32 = x.astype(F32)
    y = x32 * lax.rsqrt(jnp.mean(x32 * x32, axis=-1, keepdims=True) + NORM_EPS)
    return (y * w.astype(F32)).astype(x.dtype)


def modulate(x, w, shift, scale):
    return rms_norm(x, w) * (1.0 + scale) + shift


def head_group_norm(y, eps):
    mu = jnp.mean(y, axis=-1, keepdims=True)
    yc = y - mu
    return yc * lax.rsqrt(jnp.mean(yc * yc, axis=-1, keepdims=True) + eps)


def to_heads(t, n_heads):
    b, l, _ = t.shape
    return t.reshape(b, l, n_heads, -1).transpose(0, 2, 1, 3)


def from_heads(t):
    b, h, l, d = t.shape
    return t.transpose(0, 2, 1, 3).reshape(b, l, h * d)


def rope_1d(x, pos):
    half = x.shape[-1] // 2
    freqs = ROPE_BASE ** (-jnp.arange(half, dtype=F32) / half)
    ang = pos.astype(F32)[:, None] * freqs[None, :]
    cos, sin = jnp.cos(ang), jnp.sin(ang)
    x1, x2 = x[..., :half], x[..., half:]
    return jnp.concatenate([x1 * cos - x2 * sin, x1 * sin + x2 * cos], axis=-1)


def rope_2d(x, rpos, cpos):
    h = x.shape[-1] // 2
    xf = x.astype(F32)
    return jnp.concatenate([rope_1d(xf[..., :h], rpos), rope_1d(xf[..., h:], cpos)], axis=-1).astype(x.dtype)


def retention_chunked(q, k, v, log_g, s0, return_out):
    b, h, l, dk = q.shape
    dv = v.shape[-1]
    n = l // RET_CHUNK
    qc = q.reshape(b, h, n, RET_CHUNK, dk)
    kc = k.reshape(b, h, n, RET_CHUNK, dk)
    vc = v.reshape(b, h, n, RET_CHUNK, dv)
    idx = jnp.arange(RET_CHUNK, dtype=F32)
    k_dec = jnp.exp(log_g[:, None] * (RET_CHUNK - 1.0 - idx)[None, :])
    kv = jnp.einsum('bhncd,hc,bhnce->nbhde', kc, k_dec, vc)
    chunk_dec = jnp.exp(log_g * RET_CHUNK)[None, :, None, None]

    def step(s, kv_n):
        return s * chunk_dec + kv_n, s

    s_fin, s_prev = lax.scan(step, s0, kv)
    if not return_out:
        return None, s_fin
    diff = idx[:, None] - idx[None, :]
    dmat = jnp.where(diff >= 0, jnp.exp(log_g[:, None, None] * jnp.maximum(diff, 0.0)[None]), 0.0)
    scores = jnp.einsum('bhncd,bhnsd->bhncs', qc, kc) * dmat[None, :, None]
    intra = jnp.einsum('bhncs,bhnse->bhnce', scores, vc)
    q_dec = jnp.exp(log_g[:, None] * (idx + 1.0)[None, :])
    cross = jnp.einsum('bhncd,nbhde->bhnce', qc, s_prev) * q_dec[None, :, None, :, None]
    return (intra + cross).reshape(b, h, l, dv), s_fin


def retention_mix(q_c, k_c, v_c, q_l, k_l, v_l, log_g_f, log_g_b, ctx_out):
    b, h, _, dk = q_c.shape
    s0 = jnp.zeros((b, h, dk, v_c.shape[-1]), F32)

    def flip(t):
        return jnp.flip(t, axis=2)

    oc_f, s_f = retention_chunked(q_c, k_c, v_c, log_g_f, s0, ctx_out)
    ol_f, _ = retention_chunked(q_l, k_l, v_l, log_g_f, s_f, True)
    oc_b, s_b = retention_chunked(flip(q_c), flip(k_c), flip(v_c), log_g_b, s0, ctx_out)
    ol_b, _ = retention_chunked(flip(q_l), flip(k_l), flip(v_l), log_g_b, s_b, True)
    y_l = ol_f + flip(ol_b)
    y_c = oc_f + flip(oc_b) if ctx_out else None
    return y_c, y_l


def retention_readout(y, g, gn_w):
    yn = from_heads(head_group_norm(y, RET_GN_EPS)) * gn_w.astype(F32)
    return (yn * jax.nn.silu(g.astype(F32))).astype(g.dtype)


def neighborhood_attention(q, k, v, k_c, v_c, rpb, rows):
    b, h, l, d = q.shape
    kr = min(NA_KR_MAX, rows)
    nwin = kr * NA_KC
    scale = d ** -0.5
    qg = q.reshape(b, h, rows, GRID_W, d)
    kg = k.reshape(b, h, rows, GRID_W, d)
    vg = v.reshape(b, h, rows, GRID_W, d)
    cols = np.arange(GRID_W)
    col_start = np.clip(cols - NA_KC // 2, 0, GRID_W - NA_KC)
    col_idx = col_start[:, None] + np.arange(NA_KC)[None, :]
    col_off = col_idx - cols[:, None] + (NA_KC - 1)
    rpb_c = rpb[:, :, col_off]

    def row_fn(r):
        r0 = jnp.clip(r - kr // 2, 0, rows - kr)
        q_row = lax.dynamic_index_in_dim(qg, r, axis=2, keepdims=False)
        k_win = lax.dynamic_slice_in_dim(kg, r0, kr, axis=2)[:, :, :, col_idx]
        v_win = lax.dynamic_slice_in_dim(vg, r0, kr, axis=2)[:, :, :, col_idx]
        row_off = r0 + jnp.arange(kr) - r + (NA_KR_MAX - 1)
        bias = jnp.transpose(rpb_c[:, row_off], (0, 2, 1, 3))
        s_win = jnp.einsum('bhqd,bhrqcd->bhqrc', q_row, k_win) * scale + bias[None]
        s_ctx = jnp.einsum('bhqd,bhkd->bhqk', q_row, k_c) * scale
        s = jnp.concatenate([s_win.reshape(b, h, GRID_W, nwin), s_ctx], axis=-1).astype(F32)
        p = jax.nn.softmax(s, axis=-1).astype(v.dtype)
        p_win = p[..., :nwin].reshape(b, h, GRID_W, kr, NA_KC)
        return (jnp.einsum('bhqrc,bhrqcd->bhqd', p_win, v_win)
                + jnp.einsum('bhqk,bhkd->bhqd', p[..., nwin:], v_c))

    out = lax.map(row_fn, jnp.arange(rows))
    return jnp.transpose(out, (1, 2, 0, 3, 4)).reshape(b, h, l, d)


def context_attention(q, k, v):
    s = jnp.einsum('bhqd,bhkd->bhqk', q, k) * (q.shape[-1] ** -0.5)
    p = jax.nn.softmax(s.astype(F32), axis=-1).astype(v.dtype)
    return jnp.einsum('bhqk,bhkd->bhqd', p, v)


def even_mixer(h_c, h_l, w_in, dec_f, dec_b, gn_w, qn_w, kn_w, rpb, w_out, rows, ctx_out):
    l = h_l.shape[1]
    t = jnp.arange(l)
    rpos, cpos = t // GRID_W, t % GRID_W
    cuts = [int(u) for u in np.cumsum([RET_W] * 4 + [NA_W] * 3)[:-1]]
    rq_l, rk_l, rv_l, rg_l, nq_l, nk_l, nv_l = jnp.split(h_l @ w_in, cuts, axis=-1)
    rq_c, rk_c, rv_c, rg_c, nq_c, nk_c, nv_c = jnp.split(h_c @ w_in, cuts, axis=-1)
    kscale = RET_HD ** -0.5
    q_l = rope_2d(to_heads(rq_l, RET_HEADS), rpos, cpos).astype(F32)
    k_l = rope_2d(to_heads(rk_l, RET_HEADS), rpos, cpos).astype(F32) * kscale
    v_l = to_heads(rv_l, RET_HEADS).astype(F32)
    q_c = to_heads(rq_c, RET_HEADS).astype(F32)
    k_c = to_heads(rk_c, RET_HEADS).astype(F32) * kscale
    v_c = to_heads(rv_c, RET_HEADS).astype(F32)
    log_g_f = -jnp.exp(dec_f.astype(F32))
    log_g_b = -jnp.exp(dec_b.astype(F32))
    y_c, y_l = retention_mix(q_c, k_c, v_c, q_l, k_l, v_l, log_g_f, log_g_b, ctx_out)
    ret_l = retention_readout(y_l, rg_l, gn_w)
    aq_l = rms_norm(to_heads(nq_l, NA_HEADS), qn_w)
    ak_l = rms_norm(to_heads(nk_l, NA_HEADS), kn_w)
    av_l = to_heads(nv_l, NA_HEADS)
    aq_c = rms_norm(to_heads(nq_c, NA_HEADS), qn_w)
    ak_c = rms_norm(to_heads(nk_c, NA_HEADS), kn_w)
    av_c = to_heads(nv_c, NA_HEADS)
    na_l = from_heads(neighborhood_attention(aq_l, ak_l, av_l, ak_c, av_c, rpb, rows))
    out_l = jnp.concatenate([ret_l, na_l], axis=-1) @ w_out
    out_c = None
    if ctx_out:
        na_c = from_heads(context_attention(aq_c, ak_c, av_c))
        out_c = jnp.concatenate([retention_readout(y_c, rg_c, gn_w), na_c], axis=-1) @ w_out
    return out_c, out_l


def centred_shift(x):
    xp = jnp.pad(x, ((0, 0), (1, 1), (0, 0)))
    return 0.5 * (xp[:, :-2] + xp[:, 2:])


def rwkv_features(h, mu, w_rkv, w0, w1, w2, a0, a1, a2, k_k, k_a, full):
    def hs(t):
        return t.astype(F32).reshape(t.shape[:-1] + (RWKV_HEADS, RWKV_HD))

    xx = centred_shift(h) - h
    xm = h[None] + xx[None] * mu[:, None, None, :]
    lo = 0 if full else 1
    proj = jnp.einsum('sbld,sde->sble', xm[lo:3], w_rkv[lo:3])
    r = hs(proj[0]) if full else None
    k, v = proj[-2], proj[-1]
    w_lora = jnp.einsum('zblr,zrd->zbld', jnp.tanh(jnp.einsum('bld,zdr->zblr', xm[3], w1)), w2)
    log_w = -jax.nn.softplus(-(w0[:, None, None, :] + w_lora).astype(F32)) - 0.5
    decay = jnp.exp(-jnp.exp(log_w))
    a = jax.nn.sigmoid((a0[:, None, None, :] + jnp.einsum('zblr,zrd->zbld', jnp.einsum('bld,zdr->zblr', xm[4], a1), a2)).astype(F32))
    kk = hs(k * k_k)
    kk = kk / jnp.maximum(jnp.sqrt(jnp.sum(kk * kk, axis=-1, keepdims=True)), 1e-12)
    k_dir = hs(k.astype(F32)[None] * (1.0 + (a - 1.0) * k_a.astype(F32)))
    return r, k_dir, hs(v), kk, hs(a), hs(decay), xm[5]


def rwkv7_scan(seqs, s0):
    with_y = len(seqs) == 6

    def step(S, inp):
        w_t, k_t, v_t, a_t, b_t = inp[:5]
        S = (S * w_t[:, :, None, :]
             + jnp.einsum('bhij,bhj->bhi', S, a_t)[..., None] * b_t[:, :, None, :]
             + v_t[..., None] * k_t[:, :, None, :])
        y_t = jnp.einsum('bhij,bhj->bhi', S, inp[5]) if with_y else None
        return S, y_t

    xs = tuple(jnp.moveaxis(t, 1, 0) for t in seqs)
    s_fin, ys = lax.scan(step, s0, xs)
    return (jnp.moveaxis(ys, 0, 1) if with_y else None), s_fin


def rwkv_direction(feat, z, s0, reverse, with_y):
    r, k_dir, v, kk, a, decay, _ = feat
    seqs = [decay[z], k_dir[z], v, -kk, kk * a[z]]
    if with_y:
        seqs.append(r)
    if reverse:
        seqs = [jnp.flip(t, axis=1) for t in seqs]
    y, s = rwkv7_scan(seqs, s0)
    if reverse and with_y:
        y = jnp.flip(y, axis=1)
    return y, s


def rwkv_readout(y, feat, g1, g2, r_k, ln_w, ln_b, w_o):
    r, k_dir, v, _, _, _, xg = feat
    b, l, hh, n = y.shape
    yn = head_group_norm(y, RWKV_GN_EPS).reshape(b, l, hh * n) * ln_w.astype(F32) + ln_b.astype(F32)
    coeff = jnp.sum(r[None] * k_dir * r_k.astype(F32).reshape(hh, n), axis=-1, keepdims=True).sum(axis=0)
    bonus = (coeff * v).reshape(b, l, hh * n)
    g = jax.nn.sigmoid(xg @ g1) @ g2
    return ((yn + bonus).astype(xg.dtype) * g) @ w_o


def odd_mixer(h_c, h_l, mu, w_rkv, w0, w1, w2, a0, a1, a2, g1, g2, k_k, k_a, r_k, ln_w, ln_b, w_o, ctx_out):
    feat_c = rwkv_features(h_c, mu, w_rkv, w0, w1, w2, a0, a1, a2, k_k, k_a, ctx_out)
    feat_l = rwkv_features(h_l, mu, w_rkv, w0, w1, w2, a0, a1, a2, k_k, k_a, True)
    s0 = jnp.zeros((h_c.shape[0], RWKV_HEADS, RWKV_HD, RWKV_HD), F32)
    yc_f, sc_f = rwkv_direction(feat_c, 0, s0, False, ctx_out)
    yc_b, sc_b = rwkv_direction(feat_c, 1, s0, True, ctx_out)
    yl_f, _ = rwkv_direction(feat_l, 0, sc_f, False, True)
    yl_b, _ = rwkv_direction(feat_l, 1, sc_b, True, True)
    out_l = rwkv_readout(yl_f + yl_b, feat_l, g1, g2, r_k, ln_w, ln_b, w_o)
    out_c = rwkv_readout(yc_f + yc_b, feat_c, g1, g2, r_k, ln_w, ln_b, w_o) if ctx_out else None
    return out_c, out_l


def swiglu(x, w13, w2):
    gate, up = jnp.split(x @ w13, 2, axis=-1)
    return (jax.nn.silu(gate) * up) @ w2


def moe_swiglu(h, router, w13, w2):
    b, l, d = h.shape
    t = h.reshape(-1, d)
    logits = (t @ router).astype(F32)
    top_v, top_i = lax.top_k(logits, TOP_K)
    top_w = jax.nn.softmax(top_v, axis=-1)
    gates = jnp.sum(jax.nn.one_hot(top_i, N_EXPERTS, dtype=F32) * top_w[..., None], axis=1)
    out = jnp.zeros(t.shape, F32)
    for e in range(N_EXPERTS):
        out = out + gates[:, e:e + 1] * swiglu(t, w13[e], w2[e]).astype(F32)
    return out.astype(h.dtype).reshape(b, l, d)


def setup_inputs(seed: int = 0) -> dict:
    key = jax.random.key(seed)
    ks = iter(jax.random.split(key, 48))

    def nrm(shape, scale):
        return jax.random.normal(next(ks), shape, F32) * scale

    def unif(shape, lo, hi):
        return jax.random.uniform(next(ks), shape, F32, lo, hi)

    d = D_MODEL
    ret_dec = jnp.log(-jnp.log1p(-(2.0 ** (-5.0 - jnp.arange(RET_HEADS, dtype=F32)))))
    return {
        'x': nrm((BATCH, SEQ, d), 1.0),
        'c': nrm((BATCH, d), 1.0),
        'ctx': nrm((BATCH, CTX_LEN, d), 1.0),
        'c_ctx': nrm((d,), 1.0),
        'ada_w': nrm((DEPTH, d, 6 * d), 0.5 * d ** -0.5),
        'ada_b': nrm((DEPTH, 6 * d), 0.02),
        'norm_mix_w': 1.0 + nrm((DEPTH, d), 0.02),
        'norm_ffn_w': 1.0 + nrm((DEPTH, d), 0.02),
        'ev_w_in': nrm((N_EVEN, d, IN_W), d ** -0.5),
        'ev_ret_decay_f': ret_dec + nrm((N_EVEN, RET_HEADS), 0.05),
        'ev_ret_decay_b': ret_dec + nrm((N_EVEN, RET_HEADS), 0.05),
        'ev_ret_gn_w': 1.0 + nrm((N_EVEN, RET_W), 0.02),
        'ev_na_qn_w': 1.0 + nrm((N_EVEN, NA_HD), 0.02),
        'ev_na_kn_w': 1.0 + nrm((N_EVEN, NA_HD), 0.02),
        'ev_na_rpb': nrm((N_EVEN, NA_HEADS, 2 * NA_KR_MAX - 1, 2 * NA_KC - 1), 0.1),
        'ev_w_out': nrm((N_EVEN, MIX_W, d), MIX_W ** -0.5),
        'ev_ffn_w13': nrm((N_EVEN, d, 2 * FFN_HIDDEN), d ** -0.5),
        'ev_ffn_w2': nrm((N_EVEN, FFN_HIDDEN, d), FFN_HIDDEN ** -0.5),
        'od_mu': unif((N_ODD, 6, d), 0.0, 1.0),
        'od_w_rkv': nrm((N_ODD, 3, d, d), d ** -0.5),
        'od_w0': unif((N_ODD, 2, d), -6.5, -1.5),
        'od_w1': nrm((N_ODD, 2, d, DECAY_LORA), d ** -0.5),
        'od_w2': nrm((N_ODD, 2, DECAY_LORA, d), 0.1 * DECAY_LORA ** -0.5),
        'od_a0': nrm((N_ODD, 2, d), 0.1),
        'od_a1': nrm((N_ODD, 2, d, AAA_LORA), d ** -0.5),
        'od_a2': nrm((N_ODD, 2, AAA_LORA, d), 0.1 * AAA_LORA ** -0.5),
        'od_g1': nrm((N_ODD, d, GATE_LORA), d ** -0.5),
        'od_g2': nrm((N_ODD, GATE_LORA, d), GATE_LORA ** -0.5),
        'od_k_k': 0.85 + nrm((N_ODD, d), 0.05),
        'od_k_a': 1.0 + nrm((N_ODD, d), 0.05),
        'od_r_k': nrm((N_ODD, d), 0.1),
        'od_ln_w': 1.0 + nrm((N_ODD, d), 0.02),
        'od_ln_b': nrm((N_ODD, d), 0.02),
        'od_w_o': nrm((N_ODD, d, d), d ** -0.5),
        'od_router': nrm((N_ODD, d, N_EXPERTS), d ** -0.5),
        'od_moe_w13': nrm((N_ODD, N_EXPERTS, d, 2 * EXPERT_HIDDEN), d ** -0.5),
        'od_moe_w2': nrm((N_ODD, N_EXPERTS, EXPERT_HIDDEN, d), EXPERT_HIDDEN ** -0.5),
    }


def reference(x, c, ctx, c_ctx, ada_w, ada_b, norm_mix_w, norm_ffn_w, ev_w_in, ev_ret_decay_f, ev_ret_decay_b,
              ev_ret_gn_w, ev_na_qn_w, ev_na_kn_w, ev_na_rpb, ev_w_out, ev_ffn_w13, ev_ffn_w2, od_mu, od_w_rkv,
              od_w0, od_w1, od_w2, od_a0, od_a1, od_a2, od_g1, od_g2, od_k_k, od_k_a, od_r_k, od_ln_w, od_ln_b,
              od_w_o, od_router, od_moe_w13, od_moe_w2):
    rows = x.shape[1] // GRID_W
    s_lat = jax.nn.silu(c)
    s_ctx = jax.nn.silu(c_ctx)
    x_l, x_c = x, ctx
    for i in range(DEPTH):
        j = i // 2
        ctx_out = i < DEPTH - 1
        m_l = jnp.split((s_lat @ ada_w[i] + ada_b[i])[:, None, :], 6, axis=-1)
        m_c = jnp.split(s_ctx @ ada_w[i] + ada_b[i], 6, axis=-1)
        h_l = modulate(x_l, norm_mix_w[i], m_l[0], m_l[1])
        h_c = modulate(x_c, norm_mix_w[i], m_c[0], m_c[1])
        if i % 2 == 0:
            o_c, o_l = even_mixer(h_c, h_l, ev_w_in[j], ev_ret_decay_f[j], ev_ret_decay_b[j], ev_ret_gn_w[j],
                                  ev_na_qn_w[j], ev_na_kn_w[j], ev_na_rpb[j], ev_w_out[j], rows, ctx_out)

            def ffn(hh, j=j):
                return swiglu(hh, ev_ffn_w13[j], ev_ffn_w2[j])
        else:
            o_c, o_l = odd_mixer(h_c, h_l, od_mu[j], od_w_rkv[j], od_w0[j], od_w1[j], od_w2[j], od_a0[j], od_a1[j],
                                 od_a2[j], od_g1[j], od_g2[j], od_k_k[j], od_k_a[j], od_r_k[j], od_ln_w[j],
                                 od_ln_b[j], od_w_o[j], ctx_out)

            def ffn(hh, j=j):
                return moe_swiglu(hh, od_router[j], od_moe_w13[j], od_moe_w2[j])
        x_l = x_l + m_l[2] * o_l
        x_l = x_l + m_l[5] * ffn(modulate(x_l, norm_ffn_w[i], m_l[3], m_l[4]))
        if ctx_out:
            x_c = x_c + m_c[2] * o_c
            x_c = x_c + m_c[5] * ffn(modulate(x_c, norm_ffn_w[i], m_c[3], m_c[4]))
    return x_l
```

```python
import numpy as np
import concourse.bass as bass
import concourse.mybir as mybir
from concourse.bass_utils import run_bass_kernel_spmd

F32 = mybir.dt.float32
BF16 = mybir.dt.bfloat16
AF = mybir.ActivationFunctionType
ALU = mybir.AluOpType
AX = mybir.AxisListType

D = 1024
NCTX = 256
NLAT = 2048
NTOK = NCTX + NLAT
EPS = 1e-6


class H:
    __slots__ = ("name", "ws", "rs")

    def __init__(self, name=""):
        self.name = name
        self.ws = {}
        self.rs = {}


class T(H):
    __slots__ = ("t",)

    def __init__(self, t, name=""):
        H.__init__(self, name)
        self.t = t

    def __getitem__(self, k):
        return self.t[k]


class KB:
    ENG = ("pe", "dve", "act", "pool", "sp")

    def __init__(self, n_dma_sems=48):
        nc = bass.Bass("TRN2", target_bir_lowering=False)
        self.nc = nc
        self.eng = dict(pe=nc.tensor, dve=nc.vector, act=nc.scalar, pool=nc.gpsimd, sp=nc.sync)
        self.esem = {e: nc.alloc_semaphore("es_" + e) for e in self.ENG}
        self.ecnt = {e: 0 for e in self.ENG}
        self.known = {e: {} for e in self.ENG}
        self.dsems = [nc.alloc_semaphore("ds_%d" % i) for i in range(n_dma_sems)]
        self.dval = [0] * n_dma_sems
        self.dnext = 0
        self.n_ins = 0
        self.n_wait = 0
        self.uid = 0
        self.stacks = []
        self.stage_i = 0
        self.pending = None
        self.attach_waits = True
        self.snaps = {}

    def sb(self, shape, dtype=F32, name=None):
        self.uid += 1
        name = (name or "sb") + "_%d" % self.uid
        if self.stacks:
            t = self.stacks[-1].enter_context(self.nc.sbuf_tensor(name, list(shape), dtype))
        else:
            t = self.nc.alloc_sbuf_tensor(name, list(shape), dtype)
        return T(t, name)

    def scope(self):
        kb = self

        class _S:
            def __enter__(s2):
                import contextlib
                st = contextlib.ExitStack()
                kb.stacks.append(st)
                return st

            def __exit__(s2, *a):
                kb.barrier()
                st = kb.stacks.pop()
                st.close()
                return False
        return _S()

    def barrier(self):
        for e in self.ENG:
            for i, v in enumerate(self.dval):
                if v:
                    self._wait(e, ("dma", i), v)
            for p in self.ENG:
                if p != e and self.ecnt[p]:
                    self._wait(e, ("eng", p), self.ecnt[p])

    def ps(self, shape=(128, 512), dtype=F32, name=None):
        self.uid += 1
        name = name or "ps%d" % self.uid
        return T(self.nc.alloc_psum_tensor(name, list(shape), dtype), name)

    def dram(self, name, shape, dtype=F32, kind="Internal"):
        return T(self.nc.dram_tensor(name, list(shape), dtype, kind=kind), name)

    def _wait(self, e, key, val):
        if key[0] == "eng":
            if key[1] == e and e == "pe":
                return
            sem = self.esem[key[1]]
        else:
            sem = self.dsems[key[1]]
            val = max(val, self.dval[key[1]])
        if self.known[e].get(key, 0) >= val:
            return
        self.known[e][key] = val
        self.n_wait += 1
        sn = self.snaps.get((key, val))
        if sn is not None:
            kn = self.known[e]
            for k2, v2 in sn.items():
                if kn.get(k2, 0) < v2:
                    kn[k2] = v2
        if self.pending is not None:
            self.pending.append((sem, val))
        else:
            self.eng[e].wait_ge(sem, val)

    def _deps(self, e, reads, writes):
        me = ("eng", e)
        for h in reads:
            for k, v in h.ws.items():
                self._wait(e, k, v)
        for h in writes:
            for k, v in h.ws.items():
                self._wait(e, k, v)
            for k, v in h.rs.items():
                self._wait(e, k, v)

    def _commit(self, key, val, reads, writes):
        for h in reads:
            if h.rs.get(key, 0) < val:
                h.rs[key] = val
        for h in writes:
            if h.ws.get(key, 0) < val:
                h.ws[key] = val

    def op(self, e, fn, reads=(), writes=()):
        if self.attach_waits:
            self.pending = []
            self._deps(e, reads, writes)
            pend, self.pending = self.pending, None
            for (sem, val) in pend[:-1]:
                self.eng[e].wait_ge(sem, val)
            ins = fn(self.eng[e])
            if pend:
                ins._wait_ge(pend[-1][0], pend[-1][1])
        else:
            self._deps(e, reads, writes)
            ins = fn(self.eng[e])
        self.ecnt[e] += 1
        ins.then_inc(self.esem[e], 1)
        self.snaps[(("eng", e), self.ecnt[e])] = dict(self.known[e])
        self._commit(("eng", e), self.ecnt[e], reads, writes)
        self.n_ins += 1
        return ins

    def dma(self, out, in_, reads=(), writes=(), q="sp", **kw):
        self.pending = []
        self._deps(q, reads, writes)
        pend, self.pending = self.pending, None
        for (sem, val) in pend[:-1]:
            self.eng[q].wait_ge(sem, val)
        i = self.dnext
        self.dnext = (self.dnext + 1) % len(self.dsems)
        ins = self.eng[q].dma_start(out=out, in_=in_, **kw)
        if pend:
            ins._wait_ge(pend[-1][0], pend[-1][1])
        self.dval[i] += 16
        ins.then_inc(self.dsems[i], 16)
        self.snaps[(("dma", i), self.dval[i])] = dict(self.known[q])
        self._commit(("dma", i), self.dval[i], reads, writes)
        self.n_ins += 1
        return ins

    def set_stage(self, n=4, size=1024):
        self.stage = [self.sb([128, size], F32, "stage%d" % i) for i in range(n)]
        self.stage_size = size

    def load_cast(self, dst_ap, src_ap, dst_h, engs=("act", "pool", "act")):
        shp = list(dst_ap.shape)
        P = shp[0]
        n = 1
        for d_ in shp[1:]:
            n *= d_
        assert n <= self.stage_size, (shp, self.stage_size)
        st = self.stage[self.stage_i % len(self.stage)]
        eng = engs[self.stage_i % len(engs)]
        self.stage_i += 1
        v = st[0:P, 0:n]
        if len(shp) == 3:
            v = v.rearrange("p (a b) -> p a b", a=shp[1])
        elif len(shp) == 4:
            v = v.rearrange("p (a b c) -> p a b c", a=shp[1], b=shp[2])
        self.dma(v, src_ap, writes=[st])
        if eng == "act":
            self.op("act", lambda e: e.activation(dst_ap, v, AF.Copy), reads=[st], writes=[dst_h])
        else:
            self.op(eng, lambda e: e.tensor_copy(dst_ap, v), reads=[st], writes=[dst_h])

    def finish(self):
        for i, v in enumerate(self.dval):
            if v:
                self._wait("sp", ("dma", i), v)
        for e in self.ENG:
            if e != "sp" and self.ecnt[e]:
                self._wait("sp", ("eng", e), self.ecnt[e])


class Ctx:
    pass


def fm(ap_dram, t0, n):
    return ap_dram.rearrange("(c p) t -> p c t", p=128)[:, :, t0:t0 + n]


TILES = [(0, 256, True)] + [(256 + 512 * i, 512, False) for i in range(4)]


def phase_mod(kb, cx, c_lat, c_ctx, ada_w, ada_b):
    mod = cx.mod
    sT = kb.sb([128, 8, 2], F32, "sT")
    cin = kb.sb([128, 8, 2], F32, "cin")
    with kb.nc.allow_non_contiguous_dma(reason="tiny"):
        kb.dma(cin[:, :, 0], c_lat[:].rearrange("(c p) -> p c", p=128), writes=[cin])
        kb.dma(cin[:, :, 1], c_ctx[:].rearrange("(c p) -> p c", p=128), writes=[cin])
    kb.op("act", lambda e: e.activation(sT[:], cin[:], AF.Silu), reads=[cin], writes=[sT])
    ident = cx.ident_f32
    wbuf = [kb.sb([128, 8, 512], F32, "adaw%d" % i) for i in range(4)]
    mrow = kb.sb([2, 6144], F32, "mrow")
    brow = kb.sb([2, 6144], F32, "brow")
    pm = cx.psum[0]
    pt = cx.psum[1]
    it = 0
    for L in range(2):
        kb.dma(brow[0:1, :], ada_b[L:L + 1, :], writes=[brow])
        kb.dma(brow[1:2, :], ada_b[L:L + 1, :], writes=[brow])
        for nt in range(12):
            wb = wbuf[it % 4]
            it += 1
            kb.dma(wb[:], ada_w[L].rearrange("(c p) n -> p c n", p=128)[:, :, nt * 512:(nt + 1) * 512], writes=[wb])
            for kc in range(8):
                kb.op("pe", lambda e: e.matmul(pm[0:2, :], sT[:, kc, :], wb[:, kc, :], start=(kc == 0), stop=(kc == 7)),
                      reads=[sT, wb], writes=[pm])
            kb.op("dve", lambda e: e.tensor_tensor(mrow[:, nt * 512:(nt + 1) * 512], pm[0:2, :], brow[:, nt * 512:(nt + 1) * 512], ALU.add),
                  reads=[pm, brow], writes=[mrow])
        for c in range(48):
            kb.op("pe", lambda e: e.matmul(pt[:, 2 * c:2 * c + 2], mrow[0:2, c * 128:(c + 1) * 128], ident[0:2, 0:2], start=True, stop=True),
                  reads=[mrow, ident], writes=[pt])
        kb.op("dve", lambda e: e.tensor_copy(mod[:, L, :, :], pt[:, 0:96].rearrange("p (c s) -> p s c", s=2)), reads=[pt], writes=[mod])


def mod_vec(cx, L, s, idx):
    return cx.mod[:, L, s, idx * 8:(idx + 1) * 8]


def phase_modulate(kb, cx, XT, L, normw_dram, shift_idx, scale_idx, hT, tiles=TILES, hT_t0=0):
    nw = kb.sb([128, 8], F32)
    with kb.nc.allow_non_contiguous_dma(reason="tiny"):
        kb.dma(nw[:], normw_dram.rearrange("(c p) -> p c", p=128), writes=[nw])
    gmul = kb.sb([128, 2, 8], F32)
    for s in range(2):
        kb.op("dve", lambda e: e.scalar_tensor_tensor(gmul[:, s, :], mod_vec(cx, L, s, scale_idx), 1.0, nw[:], ALU.add, ALU.mult),
              reads=[cx.mod, nw], writes=[gmul])
    xb = cx.xbuf
    for ti, (t0, n, is_ctx) in enumerate(tiles):
        s = 1 if is_ctx else 0
        x = xb[ti % 2]
        sq = cx.sqbuf[ti % 2]
        sqb = cx.sqb[ti % 2]
        rstd = cx.rstd[ti % 2]
        ps = cx.psum[2 + (ti % 2)]
        kb.dma(x[:, :, 0:n], fm(XT[:], t0, n), reads=[XT], writes=[x])
        kb.op("act", lambda e: e.activation(sqb[:, :, 0:n], x[:, :, 0:n], AF.Square), reads=[x], writes=[sqb])
        for c in range(8):
            kb.op("pe", lambda e: e.matmul(ps[:, 0:n], cx.ones_bf[:], sqb[:, c, 0:n], start=(c == 0), stop=(c == 7)),
                  reads=[sqb, cx.ones_bf], writes=[ps])
        kb.op("act", lambda e: e.activation(rstd[:, 0:n], ps[:, 0:n], AF.Sqrt, bias=cx.eps_col[:, 0:1], scale=1.0 / D), reads=[ps, cx.eps_col], writes=[rstd])
        kb.op("dve", lambda e: e.reciprocal(rstd[:, 0:n], rstd[:, 0:n]), reads=[rstd], writes=[rstd])
        for c in range(8):
            kb.op("dve", lambda e: e.tensor_tensor(sq[:, c, 0:n], x[:, c, 0:n], rstd[:, 0:n], ALU.mult), reads=[x, rstd], writes=[sq])
            kb.op("act", lambda e: e.activation(hT[:, c, t0 - hT_t0:t0 - hT_t0 + n], sq[:, c, 0:n], AF.Identity,
                                                bias=mod_vec(cx, L, s, shift_idx)[:, c:c + 1], scale=gmul[:, s, c:c + 1]),
                  reads=[sq, cx.mod, gmul], writes=[hT])


def phase_ffn(kb, cx, XT, L, hT, hT_t0, tiles, w13_list, w2_list, hidden, gate_idx, gates=None, hc_group=4):
    nE = len(w13_list)
    nhc = hidden // 128
    groups = [(g0, min(hc_group, nhc - g0)) for g0 in range(0, nhc, hc_group)]
    ntok = sum(n for _, n, _ in tiles)
    acc = cx.ffn_acc
    w13b = cx.w13buf
    w2b = cx.w2buf
    act = cx.actbuf
    tmp = cx.ffn_tmp
    it = 0
    pidx = [0]
    units = []
    gi = 0
    for e_i in range(nE):
        for (g0, gn) in groups:
            for ti in range(len(tiles)):
                units.append((e_i, g0, gn, ti, gi))
            gi += 1
    offs = []
    o_ = 0
    for (t0, n, _) in tiles:
        offs.append(o_)
        o_ += n
    wcur = {}

    def load_w(gidx, e_i, g0, gn):
        wa = w13b[gidx % 2]
        wb = w2b[gidx % 2]
        w13 = w13_list[e_i].rearrange("(c p) n -> p c n", p=128)
        for half in range(2):
            for cc in range(0, 8, 2):
                kb.load_cast(wa[:, cc:cc + 2, half, 0:gn * 128], w13[:, cc:cc + 2, half * hidden + g0 * 128: half * hidden + (g0 + gn) * 128], wa)
        w2 = w2_list[e_i].rearrange("(c p) n -> p c n", p=128)
        for cc in range(gn):
            kb.load_cast(wb[:, cc, :], w2[:, g0 + cc, :], wb)
        wcur[gidx] = (wa, wb)

    ginfo = {}
    for (e_i, g0, gn, ti, gidx) in units:
        ginfo[gidx] = (e_i, g0, gn)
    ngroups = len(ginfo)

    def P1(u):
        e_i, g0, gn, ti, gidx = units[u]
        wa, wb = wcur[gidx]
        t0, n, is_ctx = tiles[ti]
        a = act[u % 2]
        for hc in range(gn):
            pg = cx.psum[pidx[0] % 8]; pidx[0] += 1
            pu = cx.psum[pidx[0] % 8]; pidx[0] += 1
            for kc in range(8):
                kb.op("pe", lambda e: e.matmul(pg[:, 0:n], wa[:, kc, 0, hc * 128:(hc + 1) * 128], hT[:, kc, t0 - hT_t0:t0 - hT_t0 + n],
                                               start=(kc == 0), stop=(kc == 7)), reads=[wa, hT], writes=[pg])
            for kc in range(8):
                kb.op("pe", lambda e: e.matmul(pu[:, 0:n], wa[:, kc, 1, hc * 128:(hc + 1) * 128], hT[:, kc, t0 - hT_t0:t0 - hT_t0 + n],
                                               start=(kc == 0), stop=(kc == 7)), reads=[wa, hT], writes=[pu])
            tt = tmp[hc % 2]
            kb.op("act", lambda e: e.activation(tt[:, 0:n], pg[:, 0:n], AF.Silu), reads=[pg], writes=[tt])
            if gates is None:
                kb.op("dve", lambda e: e.tensor_tensor(a[:, hc, 0:n], tt[:, 0:n], pu[:, 0:n], ALU.mult), reads=[tt, pu], writes=[a])
            else:
                kb.op("dve", lambda e: e.tensor_tensor(tt[:, 0:n], tt[:, 0:n], pu[:, 0:n], ALU.mult), reads=[tt, pu], writes=[tt])
                gt = gates[ti]
                kb.op("dve", lambda e: e.tensor_tensor(a[:, hc, 0:n], tt[:, 0:n], gt[:, e_i, 0:n], ALU.mult), reads=[tt, gt], writes=[a])

    def P2(u):
        e_i, g0, gn, ti, gidx = units[u]
        wa, wb = wcur[gidx]
        t0, n, is_ctx = tiles[ti]
        a = act[u % 2]
        off = offs[ti]
        for dc in range(8):
            po = cx.psum[pidx[0] % 8]; pidx[0] += 1
            for hc in range(gn):
                kb.op("pe", lambda e: e.matmul(po[:, 0:n], wb[:, hc, dc * 128:(dc + 1) * 128], a[:, hc, 0:n],
                                               start=(hc == 0), stop=(hc == gn - 1)), reads=[wb, a], writes=[po])
            if gidx == 0:
                kb.op("dve", lambda e: e.tensor_copy(acc[:, dc, off:off + n], po[:, 0:n]), reads=[po], writes=[acc])
            else:
                kb.op("dve", lambda e: e.tensor_tensor(acc[:, dc, off:off + n], acc[:, dc, off:off + n], po[:, 0:n], ALU.add), reads=[po, acc], writes=[acc])

    load_w(0, *ginfo[0])
    for u in range(len(units) + 1):
        if u < len(units):
            P1(u)
        if u >= 1:
            P2(u - 1)
        if u < len(units) and units[u][3] == 0 and units[u][4] + 1 < ngroups:
            g1 = units[u][4] + 1
            load_w(g1, *ginfo[g1])
    off = 0
    k = 0
    for ti, (t0, n, is_ctx) in enumerate(tiles):
        s = 1 if is_ctx else 0
        for c in range(8):
            x = tmp[k % 2]; k += 1
            kb.dma(x[:, 0:n], XT[c * 128:(c + 1) * 128, t0:t0 + n], reads=[XT], writes=[x])
            kb.op("dve", lambda e: e.scalar_tensor_tensor(x[:, 0:n], acc[:, c, off:off + n], mod_vec(cx, L, s, gate_idx)[:, c:c + 1], x[:, 0:n], ALU.mult, ALU.add),
                  reads=[acc, cx.mod, x], writes=[x])
            kb.dma(XT[c * 128:(c + 1) * 128, t0:t0 + n], x[:, 0:n], reads=[x], writes=[XT])
        off += n


def setup_common(kb, cx, consts):
    cx.psd = [kb.ps((128, 1024), F32, "psd%d" % i) for i in range(4)]
    cx.psum = []
    for i in range(4):
        cx.psum.append(T(cx.psd[i].t[:, 0:512], "psum%d" % (2 * i)))
        cx.psum.append(T(cx.psd[i].t[:, 512:1024], "psum%d" % (2 * i + 1)))
    cx.ident_f32 = kb.sb([128, 128], F32, "ident_f32")
    cx.ones_f32 = kb.sb([128, 128], F32, "ones_f32")
    cx.eps_col = kb.sb([128, 1], F32, "eps_col")
    kb.dma(cx.ident_f32[:], consts["ident"][:], writes=[cx.ident_f32])
    kb.op("dve", lambda e: e.memset(cx.ones_f32[:], 1.0), writes=[cx.ones_f32])
    kb.op("dve", lambda e: e.memset(cx.eps_col[:], EPS), writes=[cx.eps_col])
    cx.gn_eps_col = kb.sb([128, 1], F32, "gn_eps_col")
    kb.op("dve", lambda e: e.memset(cx.gn_eps_col[:], 1e-5), writes=[cx.gn_eps_col])
    cx.ident_bf = kb.sb([128, 128], BF16, "ident_bf")
    kb.op("dve", lambda e: e.tensor_copy(cx.ident_bf[:], cx.ident_f32[:]), reads=[cx.ident_f32], writes=[cx.ident_bf])


def setup_modbuf(kb, cx):
    cx.xbuf = [kb.sb([128, 8, 512], F32, "xbuf%d" % i) for i in range(2)]
    cx.sqbuf = [kb.sb([128, 8, 512], F32, "sqbuf%d" % i) for i in range(2)]
    cx.sqb = [kb.sb([128, 8, 512], BF16, "sqb%d" % i) for i in range(2)]
    cx.rstd = [kb.sb([128, 512], F32, "rstd%d" % i) for i in range(2)]
    cx.ones_bf = kb.sb([128, 128], BF16, "ones_bf")
    kb.op("dve", lambda e: e.memset(cx.ones_bf[:], 1.0), writes=[cx.ones_bf])


def setup_ffn(kb, cx, ntok_max, hc_group=4):
    cx.ffn_acc = kb.sb([128, 8, ntok_max], F32, "ffn_acc")
    cx.w13buf = [kb.sb([128, 8, 2, hc_group * 128], BF16, "w13b%d" % i) for i in range(2)]
    cx.w2buf = [kb.sb([128, hc_group, 1024], BF16, "w2b%d" % i) for i in range(2)]
    cx.actbuf = [kb.sb([128, hc_group, 512], BF16, "actb%d" % i) for i in range(2)]
    cx.ffn_tmp = [kb.sb([128, 512], F32, "ffnt%d" % i) for i in range(2)]
    kb.set_stage(4, 1024)


def phase_mod_scoped(kb, cx, c_lat, c_ctx, ada_w, ada_b):
    cx.mod = kb.sb([128, 2, 2, 48], F32, "mod")
    with kb.scope():
        phase_mod(kb, cx, c_lat, c_ctx, ada_w, ada_b)


RET_PF = {0: 0, 1: 1}
RET_PB = {0: 1, 1: 0}
for _i in range(16):
    RET_PF[2 + _i] = 2 + _i
    RET_PB[2 + _i] = 2 + (15 - _i)


def bc(ap, shape):
    return ap.broadcast_to(list(shape))


def phase_retention(kb, cx, hT, w_in, dec_f, dec_b, gn_w, consts, MIXT, na_args=None):
    ps = cx.psum
    identb = cx.ident_bf
    wb = kb.sb([128, 8, 2048], BF16, "w_ret")
    wv = w_in.rearrange("(c p) n -> p c n", p=128)
    na = None
    if na_args is not None:
        na = NaProj(kb, cx, w_in, na_args["qn_w"], na_args["kn_w"])
    with kb.scope():
        kb.set_stage(8, 1024)
        for g in range(4):
            for cc in range(0, 8, 2):
                kb.load_cast(wb[:, cc:cc + 2, g * 512:(g + 1) * 512], wv[:, cc:cc + 2, g * 512:(g + 1) * 512], wb, engs=("act", "pool", "dve"))
        if na is not None:
            na.load_weights()
    if na is not None:
        na.alloc()
    cos_t = kb.sb([128, 18, 128], F32, "cos_t")
    sin_t = kb.sb([128, 18, 128], F32, "sin_t")
    kb.dma(cos_t[:], consts["rope_cos"][:].rearrange("(n p) d -> p n d", p=128), writes=[cos_t])
    kb.dma(sin_t[:], consts["rope_sin"][:].rearrange("(n p) d -> p n d", p=128), writes=[sin_t])
    qT = kb.sb([128, 18, 4, 128], BF16, "qT")
    kT = kb.sb([128, 18, 4, 128], BF16, "kT")
    v_all = kb.sb([128, 18, 512], BF16, "v_all")
    t1 = kb.sb([128, 512], F32, "rt1")
    t2 = kb.sb([128, 512], F32, "rt2")
    qk_tm = [kb.sb([128, 512], BF16, "qk_tm%d" % i) for i in range(2)]
    lg = kb.sb([128, 8], F32, "lg")
    kb.dma(lg[:, 0:4], dec_f.partition_broadcast(128), writes=[lg])
    kb.dma(lg[:, 4:8], dec_b.partition_broadcast(128), writes=[lg])
    kb.op("act", lambda e: e.activation(lg[:], lg[:], AF.Exp), reads=[lg], writes=[lg])
    kb.op("dve", lambda e: e.tensor_scalar(lg[:], lg[:], -1.0, None, ALU.mult), reads=[lg], writes=[lg])
    cst = kb.sb([128, 5, 128], F32, "ret_cst")
    kb.dma(cst[:], consts["ret_cst"][:].rearrange("k p t -> p k t"), writes=[cst])
    Dsame = kb.sb([128, 4, 128], F32, "Dsame")
    Dfull = kb.sb([128, 2, 4, 128], F32, "Dfull")
    GP = kb.sb([128, 2, 4, 18], F32, "GP")
    nlg = kb.sb([128, 4], F32, "nlg")
    kidx = kb.sb([128, 18], F32, "kidx")
    kb.dma(kidx[:], consts["kidx128"][:].partition_broadcast(128), writes=[kidx])
    ksc = 128.0 ** -0.5
    for h in range(4):
        kb.op("act", lambda e: e.activation(t1[:, 0:128], cst[:, 1, :], AF.Exp, scale=lg[:, h:h + 1]), reads=[cst, lg], writes=[t1])
        kb.op("dve", lambda e: e.scalar_tensor_tensor(t1[:, 0:128], t1[:, 0:128], ksc, cst[:, 3, :], ALU.mult, ALU.mult), reads=[t1, cst], writes=[t1])
        kb.op("act", lambda e: e.activation(t2[:, 0:128], cst[:, 2, :], AF.Exp, scale=lg[:, 4 + h:5 + h]), reads=[cst, lg], writes=[t2])
        kb.op("dve", lambda e: e.scalar_tensor_tensor(t2[:, 0:128], t2[:, 0:128], ksc, cst[:, 4, :], ALU.mult, ALU.mult), reads=[t2, cst], writes=[t2])
        kb.op("dve", lambda e: e.tensor_tensor(Dsame[:, h, :], t1[:, 0:128], t2[:, 0:128], ALU.add), reads=[t1, t2], writes=[Dsame])
        kb.op("act", lambda e: e.activation(Dfull[:, 0, h, :], cst[:, 0, :], AF.Exp, scale=lg[:, h:h + 1]), reads=[cst, lg], writes=[Dfull])
        kb.op("dve", lambda e: e.tensor_scalar(nlg[:, h:h + 1], lg[:, 4 + h:5 + h], -1.0, None, ALU.mult), reads=[lg], writes=[nlg])
        kb.op("act", lambda e: e.activation(Dfull[:, 1, h, :], cst[:, 0, :], AF.Exp, scale=nlg[:, h:h + 1]), reads=[cst, nlg], writes=[Dfull])
        for d in range(2):
            kb.op("act", lambda e: e.activation(GP[:, d, h, :], kidx[:], AF.Exp, scale=lg[:, 4 * d + h:4 * d + h + 1]), reads=[kidx, lg], writes=[GP])
    kb.op("dve", lambda e: e.tensor_scalar(Dfull[:].rearrange("p a b c -> p (a b c)"), Dfull[:].rearrange("p a b c -> p (a b c)"), ksc, None, ALU.mult), reads=[Dfull], writes=[Dfull])
    for n in range(18):
        tsl = slice(n * 128, (n + 1) * 128)
        pq, pk, pv = ps[0], ps[1], ps[2]
        for g, pp in ((0, pq), (1, pk), (2, pv)):
            for kc in range(8):
                kb.op("pe", lambda e: e.matmul(pp[:], hT[:, kc, tsl], wb[:, kc, g * 512:(g + 1) * 512], start=(kc == 0), stop=(kc == 7)),
                      reads=[hT, wb], writes=[pp])
        kb.op("act", lambda e: e.activation(v_all[:, n, :], pv[:], AF.Copy), reads=[pv], writes=[v_all])
        for gi, (pp, dstT) in enumerate(((pq, qT), (pk, kT))):
            tm = qk_tm[gi]
            kb.op("dve", lambda e: e.tensor_tensor(t1[:].rearrange("p (h d) -> p h d", h=4), pp[:].rearrange("p (h d) -> p h d", h=4),
                                                   bc(cos_t[:, n:n + 1, :], [128, 4, 128]), ALU.mult), reads=[pp, cos_t], writes=[t1])
            ppv = pp[:].rearrange("p (hb two f) -> p hb two f", two=2, f=32)
            t2v = t2[:].rearrange("p (hb two f) -> p hb two f", two=2, f=32)
            snv = sin_t[:, n, :].rearrange("p (b two f) -> p b two f", two=2, f=32)
            for half in range(2):
                kb.op("dve", lambda e: e.tensor_tensor(t2v[:, :, half, :].rearrange("p (h b) f -> p h b f", h=4),
                                                       ppv[:, :, 1 - half, :].rearrange("p (h b) f -> p h b f", h=4),
                                                       bc(snv[:, :, half, :].unsqueeze(1), [128, 4, 2, 32]), ALU.mult), reads=[pp, sin_t], writes=[t2])
            kb.op("dve", lambda e: e.tensor_tensor(tm[:], t1[:], t2[:], ALU.add), reads=[t1, t2], writes=[tm])
            ptr = ps[3 + gi]
            for h in range(4):
                kb.op("pe", lambda e: e.transpose(ptr[:].bitcast(BF16)[:, h * 128:(h + 1) * 128], tm[:, h * 128:(h + 1) * 128], identb[:]),
                      reads=[tm, identb], writes=[ptr])
            kb.op("act", lambda e: e.activation(dstT[:, n, :, :].rearrange("p h t -> p (h t)"), ptr[:].bitcast(BF16)[:, 0:512], AF.Copy), reads=[ptr], writes=[dstT])
        if na is not None:
            na.tile(n, hT, na_args["NQT"], na_args["NKT"], na_args["NV"], [ps[5], ps[6], ps[7]])
    gnw = kb.sb([128, 512], F32, "gnw")
    kb.dma(gnw[:], gn_w.partition_broadcast(128), writes=[gnw])
    ysb = kb.sb([128, 4, 128], F32, "ysb")
    ysq = kb.sb([128, 4, 128], F32, "ysq")
    st = kb.sb([128, 8], F32, "ystat")
    sg = kb.sb([128, 512], F32, "sg")
    mixb = kb.sb([128, 512], BF16, "mixb")
    mixT_sb = [kb.sb([128, 4, 128], BF16, "mixTsb%d" % i) for i in range(2)]
    pT = [kb.sb([128, 128], BF16, "pT%d" % i) for i in range(4)]
    cnt = 0
    for n in range(18):
        tsl = slice(n * 128, (n + 1) * 128)
        terms = [(n, None, None)]
        for m in range(18):
            if m == n:
                continue
            if RET_PF[m] < RET_PF[n]:
                terms.append((m, 0, RET_PF[n] - RET_PF[m]))
            if RET_PB[m] < RET_PB[n]:
                terms.append((m, 1, RET_PB[n] - RET_PB[m]))
        py = ps[5]
        items = [(h, ti, m, d, dp) for h in range(4) for ti, (m, d, dp) in enumerate(terms)]
        pas = [ps[1], ps[2], ps[6], ps[7]]

        def QK(i):
            h, ti, m, d, dp = items[i]
            pa = pas[i % 4]
            kb.op("pe", lambda e: e.matmul(pa[:, 0:128], kT[:, m, h, :], qT[:, n, h, :], start=True, stop=True), reads=[kT, qT], writes=[pa])

        def PV(i):
            h, ti, m, d, dp = items[i]
            pa = pas[i % 4]
            pt_ = pT[i % 4]
            if d is None:
                kb.op("dve", lambda e: e.tensor_tensor(pt_[:], pa[:, 0:128], Dsame[:, h, :], ALU.mult), reads=[pa, Dsame], writes=[pt_])
            else:
                kb.op("dve", lambda e: e.scalar_tensor_tensor(pt_[:], pa[:, 0:128], GP[:, d, h, dp:dp + 1], Dfull[:, d, h, :], ALU.mult, ALU.mult),
                      reads=[pa, GP, Dfull], writes=[pt_])
            kb.op("pe", lambda e: e.matmul(py[:, h * 128:(h + 1) * 128], pt_[:], v_all[:, m, h * 128:(h + 1) * 128], start=(ti == 0), stop=(ti == len(terms) - 1)),
                  reads=[pt_, v_all], writes=[py])
        LOOK = 2
        for i in range(min(LOOK, len(items))):
            QK(i)
        for i in range(len(items)):
            if i + LOOK < len(items):
                QK(i + LOOK)
            PV(i)
        pg = ps[0]
        for kc in range(8):
            kb.op("pe", lambda e: e.matmul(pg[:], hT[:, kc, tsl], wb[:, kc, 1536:2048], start=(kc == 0), stop=(kc == 7)), reads=[hT, wb], writes=[pg])
        kb.op("act", lambda e: e.activation(sg[:], pg[:], AF.Silu), reads=[pg], writes=[sg])
        kb.op("act", lambda e: e.activation(ysb[:].rearrange("p h e -> p (h e)"), py[:], AF.Copy), reads=[py], writes=[ysb])
        kb.op("dve", lambda e: e.tensor_reduce(st[:, 0:4], ysb[:], AX.X, ALU.add), reads=[ysb], writes=[st])
        kb.op("dve", lambda e: e.tensor_scalar(st[:, 0:4], st[:, 0:4], 1.0 / 128, None, ALU.mult), reads=[st], writes=[st])
        kb.op("dve", lambda e: e.tensor_tensor(ysb[:], ysb[:], bc(st[:, 0:4].unsqueeze(2), [128, 4, 128]), ALU.subtract), reads=[ysb, st], writes=[ysb])
        kb.op("dve", lambda e: e.tensor_tensor(ysq[:], ysb[:], ysb[:], ALU.mult), reads=[ysb], writes=[ysq])
        kb.op("dve", lambda e: e.tensor_reduce(st[:, 4:8], ysq[:], AX.X, ALU.add), reads=[ysq], writes=[st])
        kb.op("act", lambda e: e.activation(st[:, 4:8], st[:, 4:8], AF.Sqrt, bias=cx.gn_eps_col[:, 0:1], scale=1.0 / 128), reads=[st, cx.gn_eps_col], writes=[st])
        kb.op("dve", lambda e: e.reciprocal(st[:, 4:8], st[:, 4:8]), reads=[st], writes=[st])
        kb.op("dve", lambda e: e.tensor_tensor(ysb[:], ysb[:], bc(st[:, 4:8].unsqueeze(2), [128, 4, 128]), ALU.mult), reads=[ysb, st], writes=[ysb])
        kb.op("dve", lambda e: e.tensor_tensor(ysq[:].rearrange("p h e -> p (h e)"), ysb[:].rearrange("p h e -> p (h e)"), gnw[:], ALU.mult), reads=[ysb, gnw], writes=[ysq])
        kb.op("dve", lambda e: e.tensor_tensor(mixb[:], ysq[:].rearrange("p h e -> p (h e)"), sg[:], ALU.mult), reads=[ysq, sg], writes=[mixb])
        ptr = ps[3]
        for h in range(4):
            kb.op("pe", lambda e: e.transpose(ptr[:].bitcast(BF16)[:, h * 128:(h + 1) * 128], mixb[:, h * 128:(h + 1) * 128], identb[:]),
                  reads=[mixb, identb], writes=[ptr])
        mo = mixT_sb[n % 2]
        kb.op("act", lambda e: e.activation(mo[:].rearrange("p h t -> p (h t)"), ptr[:].bitcast(BF16)[:, 0:512], AF.Copy), reads=[ptr], writes=[mo])
        kb.dma(MIXT[0:512, tsl].rearrange("(c p) t -> p c t", p=128), mo[:], reads=[mo], writes=[MIXT])


class NaProj:
    def __init__(self, kb, cx, w_in, qn_w, kn_w):
        self.kb, self.cx = kb, cx
        self.w_in, self.qn_w, self.kn_w = w_in, qn_w, kn_w
        self.wb = kb.sb([128, 8, 1536], BF16, "w_na")
        self.k = 0

    def load_weights(self):
        kb = self.kb
        wv = self.w_in.rearrange("(c p) n -> p c n", p=128)
        for g in range(3):
            for cc in range(0, 8, 2):
                kb.load_cast(self.wb[:, cc:cc + 2, g * 512:(g + 1) * 512], wv[:, cc:cc + 2, 2048 + g * 512:2048 + (g + 1) * 512], self.wb, engs=("act", "pool", "dve"))

    def alloc(self):
        kb = self.kb
        nw = kb.sb([128, 2, 64], F32, "na_nw")
        kb.dma(nw[:, 0, :], self.qn_w.partition_broadcast(128), writes=[nw])
        kb.dma(nw[:, 1, :], self.kn_w.partition_broadcast(128), writes=[nw])
        kb.op("dve", lambda e: e.tensor_scalar(nw[:, 0, :], nw[:, 0, :], 0.125, None, ALU.mult), reads=[nw], writes=[nw])
        self.nw = nw
        self.sq = kb.sb([128, 8, 64], F32, "na_sq")
        self.st = kb.sb([128, 8], F32, "na_st")
        self.tmb = [kb.sb([128, 8, 64], BF16, "na_tm%d" % i) for i in range(2)]
        self.outT = [kb.sb([64, 8, 128], BF16, "na_oT%d" % i) for i in range(2)]
        self.vext = [kb.sb([64, 8, 65], BF16, "na_vx%d" % i) for i in range(2)]
        for i in range(2):
            kb.op("dve", lambda e: e.memset(self.vext[i][:], 1.0), writes=[self.vext[i]])

    def tile(self, n, hT, NQT, NKT, NV, banks):
        kb, cx, wb, nw, sq, st = self.kb, self.cx, self.wb, self.nw, self.sq, self.st
        identb = cx.ident_bf
        tsl = slice(n * 128, (n + 1) * 128)
        for gi, DST in ((0, NQT), (1, NKT)):
            pp = banks[gi]
            for kc in range(8):
                kb.op("pe", lambda e: e.matmul(pp[:], hT[:, kc, tsl], wb[:, kc, gi * 512:(gi + 1) * 512], start=(kc == 0), stop=(kc == 7)), reads=[hT, wb], writes=[pp])
            ppv = pp[:].rearrange("p (h d) -> p h d", h=8)
            kb.op("act", lambda e: e.activation(sq[:], ppv, AF.Square), reads=[pp], writes=[sq])
            kb.op("dve", lambda e: e.tensor_reduce(st[:], sq[:], AX.X, ALU.add), reads=[sq], writes=[st])
            kb.op("act", lambda e: e.activation(st[:], st[:], AF.Sqrt, bias=cx.eps_col[:, 0:1], scale=1.0 / 64), reads=[st, cx.eps_col], writes=[st])
            kb.op("dve", lambda e: e.reciprocal(st[:], st[:]), reads=[st], writes=[st])
            kb.op("dve", lambda e: e.tensor_tensor(sq[:], ppv, bc(st[:].unsqueeze(2), [128, 8, 64]), ALU.mult), reads=[pp, st], writes=[sq])
            tm = self.tmb[gi]
            kb.op("dve", lambda e: e.tensor_tensor(tm[:], sq[:], bc(nw[:, gi:gi + 1, :], [128, 8, 64]), ALU.mult), reads=[sq, nw], writes=[tm])
            ptr = banks[2]
            for h in range(8):
                kb.op("pe", lambda e: e.transpose(ptr[:].bitcast(BF16)[0:64, h * 128:(h + 1) * 128], tm[:, h, :], identb[:]), reads=[tm, identb], writes=[ptr])
            oT = self.outT[gi]
            kb.op("act", lambda e: e.activation(oT[:].rearrange("p h t -> p (h t)"), ptr[:].bitcast(BF16)[0:64, :], AF.Copy), reads=[ptr], writes=[oT])
            kb.dma(DST[:, :, tsl].rearrange("h d t -> d h t"), oT[:], reads=[oT], writes=[DST])
        for sub in range(2):
            pp = banks[sub]
            t0 = n * 128 + sub * 64
            for kc in range(8):
                kb.op("pe", lambda e: e.matmul(pp[0:64, :], hT[:, kc, t0:t0 + 64], wb[:, kc, 1024:1536], start=(kc == 0), stop=(kc == 7)), reads=[hT, wb], writes=[pp])
            vx = self.vext[self.k % 2]; self.k += 1
            kb.op("act", lambda e: e.activation(vx[:, :, 0:64], pp[0:64, :].rearrange("p (h d) -> p h d", h=8), AF.Copy), reads=[pp], writes=[vx])
            kb.dma(NV[2 * n + sub], vx[:].rearrange("p h d -> p (h d)"), reads=[vx], writes=[NV])


def phase_na_proj(kb, cx, hT, w_in, qn_w, kn_w, NQT, NKT, NV):
    na = NaProj(kb, cx, w_in, qn_w, kn_w)
    with kb.scope():
        kb.set_stage(8, 1024)
        na.load_weights()
    na.alloc()
    for n in range(18):
        na.tile(n, hT, NQT, NKT, NV, [cx.psum[0], cx.psum[1], cx.psum[2]])


def phase_na_attn(kb, cx, NQT, NKT, NV, consts, MIXT):
    ps = cx.psum
    identb = cx.ident_bf
    qT = kb.sb([64, 8, NTOK], BF16, "naqT")
    kT = kb.sb([64, 8, NTOK], BF16, "nakT")
    nv = kb.sb([64, 36, 520], BF16, "nanv")
    for h in range(8):
        kb.dma(qT[:, h, :], NQT[h], reads=[NQT], writes=[qT])
        kb.dma(kT[:, h, :], NKT[h], reads=[NKT], writes=[kT])
    for g in range(0, 36, 6):
        kb.dma(nv[:, g:g + 6, :], NV[g:g + 6].rearrange("n p f -> p n f"), reads=[NV], writes=[nv])
    E = kb.sb([64, 15, 512], F32, "naE")
    cm = kb.sb([64, 64], F32, "nacm")
    kb.dma(cm[:], consts["na_colmask"][:], writes=[cm])
    for ro in range(15):
        kb.dma(E[:, ro, :], consts["na_G"][ro], writes=[E])
    for ro in range(15):
        kb.op("act", lambda e: e.activation(E[:, ro, :], E[:, ro, :], AF.Exp), reads=[E], writes=[E])
        kb.op("dve", lambda e: e.tensor_tensor(E[:, ro, :].rearrange("p (h q) -> p h q", h=8), E[:, ro, :].rearrange("p (h q) -> p h q", h=8),
                                               bc(cm[:].unsqueeze(1), [64, 8, 64]), ALU.mult), reads=[E, cm], writes=[E])
    pf = [kb.sb([64, 512], F32, "napf%d" % i) for i in range(2)]
    pball = [kb.sb([64, 12, 512], BF16, "napb%d" % i) for i in range(2)]
    rc = kb.sb([64, 8], F32, "narc")
    ob = kb.sb([64, 8, 64], BF16, "naob")
    oT = [kb.sb([128, 4, 64], BF16, "naoT%d" % i) for i in range(2)]
    pss_l = [ps[2], ps[3], ps[4], ps[7]]
    cnt = [0]

    def keys_of(qt):
        if qt < 4:
            return [(kt, None) for kt in range(4)]
        r = qt - 4
        r0 = min(max(r - 4, 0), 24)
        return [(kt, None) for kt in range(4)] + [(4 + r0 + j_, r0 + j_ - r + 7) for j_ in range(8)]

    def S1(qt):
        q0 = qt * 64
        P_ = pball[qt % 2]
        for ki, (kt, ro) in enumerate(keys_of(qt)):
            k0 = kt * 64
            pss = pss_l[cnt[0] % 4]
            for h in range(8):
                kb.op("pe", lambda e: e.matmul(pss[0:64, h * 64:(h + 1) * 64], kT[:, h, k0:k0 + 64], qT[:, h, q0:q0 + 64], start=True, stop=True),
                      reads=[kT, qT], writes=[pss])
            if ro is None:
                kb.op("act", lambda e: e.activation(P_[:, ki, :], pss[0:64, :], AF.Exp), reads=[pss], writes=[P_])
            else:
                p_f = pf[cnt[0] % 2]
                kb.op("act", lambda e: e.activation(p_f[:], pss[0:64, :], AF.Exp), reads=[pss], writes=[p_f])
                kb.op("dve", lambda e: e.tensor_tensor(P_[:, ki, :], p_f[:], E[:, ro, :], ALU.mult), reads=[p_f, E], writes=[P_])
            cnt[0] += 1

    def S2(qt):
        q0 = qt * 64
        P_ = pball[qt % 2]
        keys = keys_of(qt)
        accA, accB = ps[0], ps[1]
        for h in range(8):
            acc = accA if h < 4 else accB
            hh = h % 4
            for ki, (kt, ro) in enumerate(keys):
                kb.op("pe", lambda e: e.matmul(acc[0:64, hh * 65:(hh + 1) * 65], P_[:, ki, h * 64:(h + 1) * 64], nv[:, kt, h * 65:(h + 1) * 65],
                                               start=(ki == 0), stop=(ki == len(keys) - 1)), reads=[P_, nv], writes=[acc])
        for half, acc in ((0, accA), (1, accB)):
            av = acc[0:64, 0:260].rearrange("p (h d) -> p h d", h=4)
            kb.op("dve", lambda e: e.reciprocal(rc[:, half * 4:(half + 1) * 4], av[:, :, 64]), reads=[acc], writes=[rc])
            kb.op("dve", lambda e: e.tensor_tensor(ob[:, half * 4:(half + 1) * 4, :], av[:, :, 0:64], bc(rc[:, half * 4:(half + 1) * 4].unsqueeze(2), [64, 4, 64]), ALU.mult),
                  reads=[acc, rc], writes=[ob])
        ptr = ps[5 + (qt % 2)]
        obv = ob[:].rearrange("p h d -> p (h d)")
        for c in range(4):
            kb.op("pe", lambda e: e.transpose(ptr[:].bitcast(BF16)[:, c * 64:(c + 1) * 64], obv[:, c * 128:(c + 1) * 128], identb[0:64, 0:64]), reads=[ob, identb], writes=[ptr])
        o = oT[qt % 2]
        kb.op("act", lambda e: e.activation(o[:].rearrange("p c t -> p (c t)"), ptr[:].bitcast(BF16)[:, 0:256], AF.Copy), reads=[ptr], writes=[o])
        kb.dma(MIXT[512:1024, q0:q0 + 64].rearrange("(c p) t -> p c t", p=128), o[:], reads=[o], writes=[MIXT])
    S1(0)
    for qt in range(36):
        if qt + 1 < 36:
            S1(qt + 1)
        S2(qt)


def phase_wout(kb, cx, L, w_out, MIXT, XT_in, XT_out, gate_idx=2, tiles=TILES):
    ps = cx.psum
    wb = kb.sb([128, 8, 1024], BF16, "w_out")
    wv = w_out.rearrange("(c p) n -> p c n", p=128)
    with kb.scope():
        kb.set_stage(8, 1024)
        for cc in range(8):
            kb.load_cast(wb[:, cc, :], wv[:, cc, :], wb, engs=("act", "pool", "dve"))
    mx = [kb.sb([128, 8, 512], BF16, "wo_mx%d" % i) for i in range(2)]
    xb = [kb.sb([128, 512], F32, "wo_x%d" % i) for i in range(3)]
    k = 0
    for ti, (t0, n, is_ctx) in enumerate(tiles):
        s = 1 if is_ctx else 0
        m = mx[ti % 2]
        kb.dma(m[:, :, 0:n], fm(MIXT[:], t0, n), reads=[MIXT], writes=[m])
        for dc in range(8):
            po = ps[dc]
            for kc in range(8):
                kb.op("pe", lambda e: e.matmul(po[:, 0:n], wb[:, kc, dc * 128:(dc + 1) * 128], m[:, kc, 0:n], start=(kc == 0), stop=(kc == 7)), reads=[wb, m], writes=[po])
            x = xb[k % 3]; k += 1
            kb.dma(x[:, 0:n], XT_in[dc * 128:(dc + 1) * 128, t0:t0 + n], reads=[XT_in], writes=[x])
            kb.op("dve", lambda e: e.scalar_tensor_tensor(x[:, 0:n], po[:, 0:n], mod_vec(cx, L, s, gate_idx)[:, dc:dc + 1], x[:, 0:n], ALU.mult, ALU.add),
                  reads=[po, cx.mod, x], writes=[x])
            kb.dma(XT_out[dc * 128:(dc + 1) * 128, t0:t0 + n], x[:, 0:n], reads=[x], writes=[XT_out])


def phase_gates(kb, cx, hT, hT_t0, router, gates_all):
    ps = cx.psum
    rb = kb.sb([128, 8, 8], BF16, "router_bf")
    kb.set_stage(1, 64)
    with kb.nc.allow_non_contiguous_dma(reason="tiny"):
        kb.load_cast(rb[:], router.rearrange("(c p) e -> p c e", p=128), rb)
    NT = 16
    lg = kb.sb([128, NT, 8], F32, "g_lg")
    l2 = kb.sb([128, NT, 8], F32, "g_l2")
    m1 = kb.sb([128, NT, 8], F32, "g_m1")
    m2 = kb.sb([128, NT, 8], F32, "g_m2")
    v1 = kb.sb([128, NT], F32, "g_v1")
    v2 = kb.sb([128, NT], F32, "g_v2")
    w1 = kb.sb([128, NT], F32, "g_w1")
    w2 = kb.sb([128, NT], F32, "g_w2")
    gt = kb.sb([128, NT, 8], F32, "g_gt")
    dg = [kb.sb([128, 8, 128], F32, "g_dg%d" % i) for i in range(2)]
    for n in range(NT):
        t0 = 256 + n * 128 - hT_t0
        pl = ps[n % 2]
        for kc in range(8):
            kb.op("pe", lambda e: e.matmul(pl[:, 0:8], hT[:, kc, t0:t0 + 128], rb[:, kc, :], start=(kc == 0), stop=(kc == 7)), reads=[hT, rb], writes=[pl])
        kb.op("act", lambda e: e.activation(lg[:, n, :], pl[:, 0:8], AF.Copy), reads=[pl], writes=[lg])

    def b3(x):
        return bc(x[:].unsqueeze(2), [128, NT, 8])
    kb.op("dve", lambda e: e.tensor_reduce(v1[:], lg[:], AX.X, ALU.max), reads=[lg], writes=[v1])
    kb.op("dve", lambda e: e.tensor_tensor(m1[:], lg[:], b3(v1), ALU.is_equal), reads=[lg, v1], writes=[m1])
    kb.op("dve", lambda e: e.scalar_tensor_tensor(l2[:], m1[:], -1e30, lg[:], ALU.mult, ALU.add), reads=[m1, lg], writes=[l2])
    kb.op("dve", lambda e: e.tensor_reduce(v2[:], l2[:], AX.X, ALU.max), reads=[l2], writes=[v2])
    kb.op("dve", lambda e: e.tensor_tensor(m2[:], l2[:], b3(v2), ALU.is_equal), reads=[l2, v2], writes=[m2])
    kb.op("dve", lambda e: e.tensor_tensor(w1[:], v2[:], v1[:], ALU.subtract), reads=[v1, v2], writes=[w1])
    kb.op("act", lambda e: e.activation(w1[:], w1[:], AF.Exp), reads=[w1], writes=[w1])
    kb.op("dve", lambda e: e.tensor_scalar(w1[:], w1[:], 1.0, None, ALU.add), reads=[w1], writes=[w1])
    kb.op("dve", lambda e: e.reciprocal(w1[:], w1[:]), reads=[w1], writes=[w1])
    kb.op("dve", lambda e: e.tensor_scalar(w2[:], w1[:], -1.0, 1.0, ALU.mult, ALU.add), reads=[w1], writes=[w2])
    kb.op("dve", lambda e: e.tensor_tensor(m1[:], m1[:], b3(w1), ALU.mult), reads=[m1, w1], writes=[m1])
    kb.op("dve", lambda e: e.tensor_tensor(m2[:], m2[:], b3(w2), ALU.mult), reads=[m2, w2], writes=[m2])
    kb.op("dve", lambda e: e.tensor_tensor(gt[:], m1[:], m2[:], ALU.add), reads=[m1, m2], writes=[gt])
    for n in range(NT):
        d = dg[n % 2]
        kb.op("dve", lambda e: e.tensor_tensor(d[:], bc(cx.ident_f32[:].unsqueeze(1), [128, 8, 128]), bc(gt[:, n, :].unsqueeze(2), [128, 8, 128]), ALU.mult),
              reads=[gt, cx.ident_f32], writes=[d])
        for half in range(2):
            pg = ps[2 + (2 * n + half) % 4]
            kb.op("pe", lambda e: e.matmul(pg[:], cx.ones_f32[:], d[:, half * 4:(half + 1) * 4, :], start=True, stop=True), reads=[d, cx.ones_f32], writes=[pg])
            kb.op("act", lambda e: e.activation(gates_all[:, half * 4:(half + 1) * 4, n * 128:(n + 1) * 128], pg[:].rearrange("p (e t) -> p e t", e=4), AF.Copy),
                  reads=[pg], writes=[gates_all])


W_NAMES = ["ada_w", "ada_b", "norm_mix_w", "norm_ffn_w", "ev_w_in", "ev_ret_decay_f", "ev_ret_decay_b", "ev_ret_gn_w", "ev_na_qn_w",
           "ev_na_kn_w", "ev_w_out", "ev_ffn_w13", "ev_ffn_w2", "od_router", "od_moe_w13", "od_moe_w2"]


def build_program(shapes, const_shapes):
    kb = KB(); cx = Ctx()

    def ein(name, shape):
        return kb.dram(name, list(shape), F32, "ExternalInput")
    c_lat = ein("c_lat", [1024]); c_ctx = ein("c_ctx", [1024])
    w = {k: ein(k, shapes[k]) for k in W_NAMES + RW_NAMES}
    consts = {k: ein(k, v) for k, v in const_shapes.items()}
    XT0 = ein("xT", [1024, NTOK])
    outT = kb.dram("outT", [1024, NLAT], F32, "ExternalOutput")
    XT1 = kb.dram("XT1", [1024, NTOK], F32)
    MIXT = kb.dram("MIXT", [1024, NTOK], BF16)
    NQT = kb.dram("NQT", [8, 64, NTOK], BF16); NKT = kb.dram("NKT", [8, 64, NTOK], BF16); NV = kb.dram("NV", [36, 64, 520], BF16)
    setup_common(kb, cx, consts)
    phase_mod_scoped(kb, cx, c_lat, c_ctx, w["ada_w"], w["ada_b"])
    with kb.scope():
        hT = kb.sb([128, 8, NTOK], BF16, "hT")
        with kb.scope():
            setup_modbuf(kb, cx)
            phase_modulate(kb, cx, XT0, 0, w["norm_mix_w"][0], 0, 1, hT)
        with kb.scope():
            phase_retention(kb, cx, hT, w["ev_w_in"][0], w["ev_ret_decay_f"][0], w["ev_ret_decay_b"][0], w["ev_ret_gn_w"][0], consts, MIXT,
                            na_args=dict(qn_w=w["ev_na_qn_w"][0], kn_w=w["ev_na_kn_w"][0], NQT=NQT, NKT=NKT, NV=NV))
    with kb.scope():
        phase_na_attn(kb, cx, NQT, NKT, NV, consts, MIXT)
    with kb.scope():
        phase_wout(kb, cx, 0, w["ev_w_out"][0], MIXT, XT0, XT1)
    with kb.scope():
        hT = kb.sb([128, 8, NTOK], BF16, "hT")
        with kb.scope():
            setup_modbuf(kb, cx)
            phase_modulate(kb, cx, XT1, 0, w["norm_ffn_w"][0], 3, 4, hT)
        with kb.scope():
            setup_ffn(kb, cx, NTOK)
            phase_ffn(kb, cx, XT1, 0, hT, 0, TILES, [w["ev_ffn_w13"][0]], [w["ev_ffn_w2"][0]], 2816, 5)
    XT2 = kb.dram("XT2", [1024, NTOK], F32)
    layer1_mixer(kb, cx, w, consts, XT1, XT2, MIXT)
    LT = TILES[1:]
    with kb.scope():
        hT = kb.sb([128, 8, NLAT], BF16, "hT")
        with kb.scope():
            setup_modbuf(kb, cx)
            phase_modulate(kb, cx, XT2, 1, w["norm_ffn_w"][1], 3, 4, hT, tiles=LT, hT_t0=256)
        gates_all = kb.sb([128, 8, NLAT], BF16, "gates_all")
        with kb.scope():
            phase_gates(kb, cx, hT, 256, w["od_router"][0], gates_all)
        gl = [T(gates_all.t[:, :, i * 512:(i + 1) * 512]) for i in range(4)]
        for g_ in gl:
            g_.ws = gates_all.ws; g_.rs = gates_all.rs
        with kb.scope():
            setup_ffn(kb, cx, NLAT)
            phase_ffn(kb, cx, XT2, 1, hT, 256, LT, [w["od_moe_w13"][0][e] for e in range(8)], [w["od_moe_w2"][0][e] for e in range(8)], 3584, 5, gates=gl)
    kb.dma(outT[:], XT2[:, 256:NTOK], reads=[XT2], writes=[outT])
    kb.finish()
    return kb


def host_consts():
    c = {}
    c["ident"] = np.eye(128, dtype=np.float32)
    half = 32
    freqs = (10000.0 ** (-np.arange(half, dtype=np.float32) / half)).astype(np.float32)
    t = np.arange(2048)
    rpos = (t // 64).astype(np.float32); cpos = (t % 64).astype(np.float32)
    cos = np.ones((2304, 128), np.float32); sin = np.zeros((2304, 128), np.float32)
    for blk, pos in ((0, rpos), (1, cpos)):
        ang = pos[:, None] * freqs[None, :]
        cs, sn = np.cos(ang).astype(np.float32), np.sin(ang).astype(np.float32)
        cos[256:, blk * 64:blk * 64 + 32] = cs; cos[256:, blk * 64 + 32:blk * 64 + 64] = cs
        sin[256:, blk * 64:blk * 64 + 32] = -sn; sin[256:, blk * 64 + 32:blk * 64 + 64] = sn
    c["rope_cos"] = cos; c["rope_sin"] = sin
    s = np.arange(128, dtype=np.float32)[:, None]; tt = np.arange(128, dtype=np.float32)[None, :]
    d = tt - s
    c["ret_cst"] = np.stack([d, np.maximum(d, 0), np.maximum(-d, 0), (d >= 0).astype(np.float32), (d <= 0).astype(np.float32)]).astype(np.float32)
    c["kidx128"] = (128.0 * np.arange(18)).astype(np.float32)
    return c


def na_tables(rpb):
    kc = np.arange(64)[:, None]; q = np.arange(64)[None, :]
    idx = np.clip(kc - q + 15, 0, 30)
    G = rpb[:, :, idx]
    G = np.ascontiguousarray(np.transpose(G, (1, 2, 0, 3))).reshape(15, 64, 512).astype(np.float32)
    cs = np.clip(np.arange(64) - 8, 0, 48)
    cm = ((kc >= cs[None, :]) & (kc < cs[None, :] + 16)).astype(np.float32)
    return G, cm


_PROG_CACHE = {}


def kernel(**inputs):
    inp = {k: np.ascontiguousarray(np.asarray(v, dtype=np.float32)) for k, v in inputs.items()}
    hc = host_consts()
    G, cm = na_tables(inp["ev_na_rpb"][0])
    hc["na_G"] = G
    hc["na_colmask"] = cm
    hc.update(rwkv_consts())
    shapes = {k: inp[k].shape for k in W_NAMES + RW_NAMES}
    cshapes = {k: v.shape for k, v in hc.items()}
    key = "prog"
    kb = build_program(shapes, cshapes)
    n = 8
    in_maps = []
    for b in range(n):
        m = {"c_lat": inp["c"][b], "c_ctx": inp["c_ctx"],
             "xT": np.ascontiguousarray(np.concatenate([inp["ctx"][b], inp["x"][b]], 0).T)}
        for k in W_NAMES + RW_NAMES:
            m[k] = inp[k]
        m.update(hc)
        in_maps.append(m)
    res = run_bass_kernel_spmd(kb.nc, in_maps, core_ids=list(range(n)))
    out = np.stack([np.ascontiguousarray(res.results[b]["outT"].T) for b in range(n)], 0)
    return out.astype(np.float32)


DEC_C = 0.6065306597126334
NCH = NTOK // 64


class BankRR:
    def __init__(self, banks):
        self.b = banks
        self.i = 0

    def __call__(self):
        p = self.b[self.i % len(self.b)]
        self.i += 1
        return p


def phase_rwkv_feat(kb, cx, HT, w, consts, D_):
    nb = BankRR(cx.psum)
    W3 = kb.sb([128, 3, 8, 1024], BF16, "rw_W3")
    w1b = kb.sb([128, 2, 8, 64], BF16, "rw_w1")
    a1b = kb.sb([128, 2, 8, 64], BF16, "rw_a1")
    w2b = kb.sb([64, 2, 1024], BF16, "rw_w2")
    a2b = kb.sb([64, 2, 1024], BF16, "rw_a2")
    g1b = kb.sb([128, 8, 160], BF16, "rw_g1")
    g2b = kb.sb([128, 1024], BF16, "rw_g2")
    g2c = kb.sb([32, 1024], BF16, "rw_g2c")
    with kb.scope():
        kb.set_stage(8, 1024)
        for s in range(3):
            wv = w["od_w_rkv"][0][s].rearrange("(c p) n -> p c n", p=128)
            for cc in range(8):
                kb.load_cast(W3[:, s, cc, :], wv[:, cc, :], W3, engs=("act", "pool", "dve"))
        for z in range(2):
            kb.load_cast(w1b[:, z, :, :], w["od_w1"][0][z].rearrange("(c p) r -> p c r", p=128), w1b)
            kb.load_cast(a1b[:, z, :, :], w["od_a1"][0][z].rearrange("(c p) r -> p c r", p=128), a1b)
            kb.load_cast(w2b[:, z, :], w["od_w2"][0][z], w2b)
            kb.load_cast(a2b[:, z, :], w["od_a2"][0][z], a2b)
        for hf in range(2):
            kb.load_cast(g1b[:, hf * 4:(hf + 1) * 4, :], w["od_g1"][0].rearrange("(c p) r -> p c r", p=128)[:, hf * 4:(hf + 1) * 4, :], g1b)
        kb.load_cast(g2b[:], w["od_g2"][0][0:128, :], g2b)
        kb.load_cast(g2c[:], w["od_g2"][0][128:160, :], g2c)
    mu = kb.sb([128, 6, 8], F32, "rw_mu")
    with kb.nc.allow_non_contiguous_dma(reason="tiny"):
        for s in range(6):
            kb.dma(mu[:, s, :], w["od_mu"][0][s].rearrange("(c p) -> p c", p=128), writes=[mu])
    tabs = kb.sb([128, 7, 1024], F32, "rw_tabs")
    srcs = [w["od_w0"][0][0], w["od_w0"][0][1], w["od_a0"][0][0], w["od_a0"][0][1], w["od_k_k"][0], w["od_k_a"][0], w["od_r_k"][0]]
    for i, s_ in enumerate(srcs):
        kb.dma(tabs[:, i, :], s_.partition_broadcast(128), writes=[tabs])
    CM = kb.sb([128, 5, 128], F32, "rw_CM")
    kb.dma(CM[:], consts["rw_CM"][:].rearrange("k s t -> s k t"), writes=[CM])
    tiny = kb.sb([128, 1], F32, "rw_tiny")
    kb.op("dve", lambda e: e.memset(tiny[:], 1e-24), writes=[tiny])
    xx = kb.sb([128, 8, 128], F32, "rw_xx")
    XM = [[kb.sb([128, 8, 128], BF16, "rw_xm%d_%d" % (i, s)) for s in range(6)] for i in range(2)]
    HID = [(kb.sb([64, 2, 128], BF16, "rw_hw%d" % i), kb.sb([64, 2, 128], BF16, "rw_ha%d" % i),
            kb.sb([128, 128], BF16, "rw_hg%d" % i), kb.sb([32, 128], BF16, "rw_hg2%d" % i)) for i in range(2)]

    FW = 512
    NHU = FW // 64

    def mkws(tag):
        d = {}
        for nm in ("r_sb", "k_sb", "v_sb", "kk", "kkn", "tmp", "tmp2", "tmp3", "sig0", "sig1", "asg0", "asg1", "kd0", "kd1", "bz"):
            d[nm] = kb.sb([128, FW], F32, "rw_%s_%s" % (nm, tag))
        d["st8"] = kb.sb([128, NHU], F32, "rw_st_%s" % tag)
        return d
    WSS = [mkws("a")]
    OBL = [kb.sb([128, 9, FW], BF16, "rw_OB%d" % i) for i in range(2)]
    OFL = [kb.sb([128, 4, FW], F32, "rw_OF%d" % i) for i in range(2)]
    ucnt = [0]

    class _V:
        def __init__(self, tile, slot):
            self.tile, self.slot = tile, slot

        def __getitem__(self, k):
            return self.tile.t[:, self.slot, :][k]

    hloc_l = [kb.sb([128, 8, 130], BF16, "rw_hloc%d" % i) for i in range(2)]

    def prologue(n):
        xm = XM[n % 2]
        hw_b, ha_b, hg_b, hg2_b = HID[n % 2]
        t0 = n * 128
        tsl = slice(t0, t0 + 128)
        left_b = (t0 == 0 or t0 == 256)
        right_b = (t0 + 128 == 256 or t0 + 128 == NTOK)
        lo = 1 if left_b else 0
        hi = 127 if right_b else 128
        hloc = hloc_l[n % 2]
        g0 = t0 if left_b else t0 - 1
        g1 = t0 + 128 if right_b else t0 + 129
        kb.dma(hloc[:, :, g0 - (t0 - 1):g1 - (t0 - 1)], fm(HT[:], g0, g1 - g0), reads=[HT], writes=[hloc])
        kb.op("dve", lambda e: e.tensor_tensor(xx[:, :, lo:hi], hloc[:, :, lo:hi], hloc[:, :, lo + 2:hi + 2], ALU.add), reads=[hloc], writes=[xx])
        if left_b:
            kb.op("dve", lambda e: e.tensor_copy(xx[:, :, 0:1], hloc[:, :, 2:3]), reads=[hloc], writes=[xx])
        if right_b:
            kb.op("dve", lambda e: e.tensor_copy(xx[:, :, 127:128], hloc[:, :, 127:128]), reads=[hloc], writes=[xx])
        kb.op("dve", lambda e: e.tensor_scalar(xx[:], xx[:], 0.5, None, ALU.mult), reads=[xx], writes=[xx])
        kb.op("dve", lambda e: e.tensor_tensor(xx[:], xx[:], hloc[:, :, 1:129], ALU.subtract), reads=[xx, hloc], writes=[xx])
        for s in range(6):
            eng = "dve" if s % 2 == 0 else "pool"
            kb.op(eng, lambda e: e.tensor_tensor(xm[s][:], xx[:], bc(mu[:, s, :].unsqueeze(2), [128, 8, 128]), ALU.mult), reads=[xx, mu], writes=[xm[s]])
            kb.op(eng, lambda e: e.tensor_tensor(xm[s][:], xm[s][:], hloc[:, :, 1:129], ALU.add), reads=[xm[s], hloc], writes=[xm[s]])
            yield
        ph = nb()
        for z in range(2):
            for kc in range(8):
                kb.op("pe", lambda e: e.matmul(ph[0:64, z * 128:(z + 1) * 128], w1b[:, z, kc, :], xm[3][:, kc, :], start=(kc == 0), stop=(kc == 7)), reads=[w1b, xm[3]], writes=[ph])
        kb.op("act", lambda e: e.activation(hw_b[:].rearrange("p z t -> p (z t)"), ph[0:64, 0:256], AF.Tanh), reads=[ph], writes=[hw_b])
        yield
        ph = nb()
        for z in range(2):
            for kc in range(8):
                kb.op("pe", lambda e: e.matmul(ph[0:64, z * 128:(z + 1) * 128], a1b[:, z, kc, :], xm[4][:, kc, :], start=(kc == 0), stop=(kc == 7)), reads=[a1b, xm[4]], writes=[ph])
        kb.op("act", lambda e: e.activation(ha_b[:].rearrange("p z t -> p (z t)"), ph[0:64, 0:256], AF.Copy), reads=[ph], writes=[ha_b])
        yield
        ph = nb()
        for kc in range(8):
            kb.op("pe", lambda e: e.matmul(ph[:, 0:128], g1b[:, kc, 0:128], xm[5][:, kc, :], start=(kc == 0), stop=(kc == 7)), reads=[g1b, xm[5]], writes=[ph])
        kb.op("act", lambda e: e.activation(hg_b[:], ph[:, 0:128], AF.Sigmoid), reads=[ph], writes=[hg_b])
        yield
        ph = nb()
        for kc in range(8):
            kb.op("pe", lambda e: e.matmul(ph[0:32, 0:128], g1b[:, kc, 128:160], xm[5][:, kc, :], start=(kc == 0), stop=(kc == 7)), reads=[g1b, xm[5]], writes=[ph])
        kb.op("act", lambda e: e.activation(hg2_b[:], ph[0:32, 0:128], AF.Sigmoid), reads=[ph], writes=[hg2_b])
        yield

    def units(n):
        t0 = n * 128
        tsl = slice(t0, t0 + 128)
        xm = XM[n % 2]
        hw_b, ha_b, hg_b, hg2_b = HID[n % 2]
        def unit(q, WS):
            r_sb, k_sb, v_sb, kk, kkn, tmp, tmp2, tmp3, bz, st8 = (WS[k_] for k_ in ('r_sb', 'k_sb', 'v_sb', 'kk', 'kkn', 'tmp', 'tmp2', 'tmp3', 'bz', 'st8'))
            sig = [WS['sig0'], WS['sig1']]; asg = [WS['asg0'], WS['asg1']]; kd = [WS['kd0'], WS['kd1']]
            fsl = slice(q * FW, (q + 1) * FW)
            OB = OBL[ucnt[0] % 2]
            OF = OFL[ucnt[0] % 2]
            ucnt[0] += 1
            for s, dst in ((0, r_sb), (1, k_sb), (2, v_sb)):
                pp = nb()
                for kc in range(8):
                    kb.op("pe", lambda e: e.matmul(pp[:], xm[s][:, kc, :], W3[:, s, kc, fsl], start=(kc == 0), stop=(kc == 7)), reads=[xm[s], W3], writes=[pp])
                kb.op("act", lambda e: e.activation(dst[:], pp[:], AF.Copy), reads=[pp], writes=[dst])
            o = _V(OB, 0)
            kb.op("pool", lambda e: e.tensor_copy(o[:], v_sb[:]), reads=[v_sb], writes=[OB])
            yield
            for z in range(2):
                pp = nb()
                kb.op("pe", lambda e: e.matmul(pp[:], hw_b[:, z, :], w2b[:, z, fsl], start=True, stop=True), reads=[hw_b, w2b], writes=[pp])
                kb.op("dve", lambda e: e.tensor_tensor(sig[z][:], pp[:], tabs[:, z, fsl], ALU.add), reads=[pp, tabs], writes=[sig[z]])
                kb.op("act", lambda e: e.activation(sig[z][:], sig[z][:], AF.Sigmoid), reads=[sig[z]], writes=[sig[z]])
                pp = nb()
                kb.op("pe", lambda e: e.matmul(pp[:], ha_b[:, z, :], a2b[:, z, fsl], start=True, stop=True), reads=[ha_b, a2b], writes=[pp])
                kb.op("dve", lambda e: e.tensor_tensor(asg[z][:], pp[:], tabs[:, 2 + z, fsl], ALU.add), reads=[pp, tabs], writes=[asg[z]])
                kb.op("act", lambda e: e.activation(asg[z][:], asg[z][:], AF.Sigmoid), reads=[asg[z]], writes=[asg[z]])
                yield
            kb.op("dve", lambda e: e.tensor_tensor(kk[:], k_sb[:], tabs[:, 4, fsl], ALU.mult), reads=[k_sb, tabs], writes=[kk])
            kb.op("act", lambda e: e.activation(tmp[:], kk[:], AF.Square), reads=[kk], writes=[tmp])
            kb.op("dve", lambda e: e.tensor_reduce(st8[:], tmp[:].rearrange("p (h j) -> p h j", h=8), AX.X, ALU.add), reads=[tmp], writes=[st8])
            kb.op("act", lambda e: e.activation(st8[:], st8[:], AF.Sqrt, bias=tiny[:, 0:1], scale=1.0), reads=[st8, tiny], writes=[st8])
            kb.op("dve", lambda e: e.reciprocal(st8[:], st8[:]), reads=[st8], writes=[st8])
            kb.op("dve", lambda e: e.tensor_tensor(kkn[:].rearrange("p (h j) -> p h j", h=8), kk[:].rearrange("p (h j) -> p h j", h=8),
                                                   bc(st8[:].unsqueeze(2), [128, 8, 64]), ALU.mult), reads=[kk, st8], writes=[kkn])
            yield
            for z in range(2):
                kb.op("dve", lambda e: e.scalar_tensor_tensor(tmp[:], asg[z][:], -1.0, tabs[:, 5, fsl], ALU.add, ALU.mult), reads=[asg[z], tabs], writes=[tmp])
                kb.op("dve", lambda e: e.scalar_tensor_tensor(kd[z][:], tmp[:], 1.0, k_sb[:], ALU.add, ALU.mult), reads=[tmp, k_sb], writes=[kd[z]])
                kb.op("pool", lambda e: e.tensor_tensor(bz[:], kkn[:], asg[z][:], ALU.mult), reads=[kkn, asg[z]], writes=[bz])
                pci = nb()
                kb.op("pe", lambda e: e.matmul(pci[:], CM[:, 2 * z, :], sig[z][:], start=True, stop=True), reads=[CM, sig[z]], writes=[pci])
                kb.op("act", lambda e: e.activation(tmp2[:], pci[:], AF.Exp, scale=-DEC_C), reads=[pci], writes=[tmp2])
                o = _V(OB, 1 + z)
                kb.op("dve", lambda e: e.tensor_tensor(o[:], r_sb[:], tmp2[:], ALU.mult), reads=[r_sb, tmp2], writes=[OB])
                yield
                kb.op("act", lambda e: e.activation(tmp3[:], pci[:], AF.Exp, scale=DEC_C), reads=[pci], writes=[tmp3])
                o = _V(OB, 3 + z)
                kb.op("dve", lambda e: e.tensor_tensor(o[:], bz[:], tmp3[:], ALU.mult), reads=[bz, tmp3], writes=[OB])
                o = _V(OB, 5 + z)
                kb.op("pool", lambda e: e.tensor_tensor(o[:], kd[z][:], tmp3[:], ALU.mult), reads=[kd[z], tmp3], writes=[OB])
                yield
                pce = nb()
                kb.op("pe", lambda e: e.matmul(pce[:], CM[:, 2 * z + 1, :], sig[z][:], start=True, stop=True), reads=[CM, sig[z]], writes=[pce])
                kb.op("act", lambda e: e.activation(tmp2[:], pce[:], AF.Exp, scale=-DEC_C), reads=[pce], writes=[tmp2])
                o = _V(OB, 7 + z)
                kb.op("dve", lambda e: e.scalar_tensor_tensor(o[:], kkn[:], -1.0, tmp2[:], ALU.mult, ALU.mult), reads=[kkn, tmp2], writes=[OB])
                ptot = nb()
                kb.op("pe", lambda e: e.matmul(ptot[:], CM[:, 4, :], sig[z][:], start=True, stop=True), reads=[CM, sig[z]], writes=[ptot])
                o2 = _V(OF, z)
                kb.op("act", lambda e: e.activation(o2[:], ptot[:], AF.Exp, scale=-DEC_C), reads=[ptot], writes=[OF])
                yield
            kb.op("dve", lambda e: e.tensor_tensor(tmp[:], r_sb[:], tabs[:, 6, fsl], ALU.mult), reads=[r_sb, tabs], writes=[tmp])
            kb.op("pool", lambda e: e.tensor_tensor(tmp2[:], kd[0][:], kd[1][:], ALU.add), reads=[kd[0], kd[1]], writes=[tmp2])
            kb.op("dve", lambda e: e.tensor_tensor(tmp[:], tmp[:], tmp2[:], ALU.mult), reads=[tmp, tmp2], writes=[tmp])
            kb.op("dve", lambda e: e.tensor_reduce(st8[:], tmp[:].rearrange("p (h j) -> p h j", h=8), AX.X, ALU.add), reads=[tmp], writes=[st8])
            o2 = _V(OF, 2)
            kb.op("dve", lambda e: e.tensor_tensor(o2[:].rearrange("p (h j) -> p h j", h=8), v_sb[:].rearrange("p (h j) -> p h j", h=8),
                                                   bc(st8[:].unsqueeze(2), [128, 8, 64]), ALU.mult), reads=[v_sb, st8], writes=[OF])
            yield
            pg = nb()
            kb.op("pe", lambda e: e.matmul(pg[:], hg_b[:], g2b[:, fsl], start=True, stop=False), reads=[hg_b, g2b], writes=[pg])
            kb.op("pe", lambda e: e.matmul(pg[:], hg2_b[:], g2c[:, fsl], start=False, stop=True), reads=[hg2_b, g2c], writes=[pg])
            o2 = _V(OF, 3)
            kb.op("act", lambda e: e.activation(o2[:], pg[:], AF.Copy), reads=[pg], writes=[OF])
            kb.dma(D_["BIG"][tsl, :, fsl], OB[:], reads=[OB], writes=[D_["BIG"]])
            kb.dma(D_["BIGF"][tsl, :, fsl], OF[:], reads=[OF], writes=[D_["BIGF"]])


        for q_ in range(2):
            yield from unit(q_, WSS[0])

    run_gens([prologue(0)])
    for n in range(18):
        gens = [units(n)]
        if n + 1 < 18:
            gens.append(prologue(n + 1))
        run_gens(gens)


class RwkvScan:
    def __init__(self, kb, cx, z, D_, consts):
        self.kb, self.cx, self.z, self.D_ = kb, cx, z, D_
        self.nd = cx.nd
        MK = kb.sb([128, 4, 64], F32, "sc_MK")
        kb.dma(MK[:], consts["rw_MK2"][:].rearrange("k r c -> r k c"), writes=[MK])
        self.MK = MK
        self.I2 = kb.sb([128, 64], F32, "sc_I2")
        kb.dma(self.I2[:], consts["rw_I2"][:], writes=[self.I2])
        Ef = kb.sb([64, 2, 128], F32, "sc_Ef")
        kb.dma(Ef[:], consts["rw_E"][:].rearrange("k r c -> r k c"), writes=[Ef])
        self.E = kb.sb([64, 2, 128], BF16, "sc_E")
        kb.op("dve", lambda e: e.tensor_copy(self.E[:], Ef[:]), reads=[Ef], writes=[self.E])
        if z == 0:
            self.mT_strict, self.mT_incl, self.mL_strict = 0, 1, 2
        else:
            self.mT_strict, self.mT_incl, self.mL_strict = 2, 3, 0

        def b16(name):
            return kb.sb([128, 8, 64], BF16, name)
        self.U0f = kb.sb([128, 8, 64], F32, "sc_U0f")
        self.U0b = b16("sc_U0b")
        kb.op("dve", lambda e: e.memset(self.U0f[:], 0.0), writes=[self.U0f])
        kb.op("dve", lambda e: e.memset(self.U0b[:], 0.0), writes=[self.U0b])
        self.tin = [[kb.sb([64, 1024], BF16, "sc_in%d_%d" % (i, j)) for j in range(5)] for i in range(2)]
        self.etin = [kb.sb([64, 1024], F32, "sc_et%d" % i) for i in range(2)]
        self.yout = kb.sb([128, 8, 64], F32, "sc_yo")
        self.sets = []
        for i in range(2):
            d = {}
            for nm in ("aT", "rT", "LakT", "MrbT", "MrkT", "QTb", "Bst", "Kst", "Vst"):
                d[nm] = b16("sc_%s%d" % (nm, i))
            d["etT"] = kb.sb([128, 8, 64], F32, "sc_etT%d" % i)
            self.sets.append(d)
        self.bT, self.kT = b16("sc_bT"), b16("sc_kT")
        self.Lp = [b16("sc_L%d" % i) for i in range(2)]
        self.LTp = [b16("sc_LT%d" % i) for i in range(2)]
        self.LTf = kb.sb([128, 8, 64], F32, "sc_LTf")
        self.QTf = kb.sb([128, 8, 64], F32, "sc_QTf")
        self.Xb, self.Pb = b16("sc_Xb"), b16("sc_Pb")
        self.order = (list(range(4)) + list(range(4, NCH))) if z == 0 else ([3, 2, 1, 0] + list(range(NCH - 1, 3, -1)))

    def mm_heads(self, pd, specs, reads):
        kb = self.kb
        for p in range(8):
            for h2 in range(2):
                ps_ = slice(h2 * 64, (h2 + 1) * 64)
                for k, (lt, rt) in enumerate(specs):
                    kb.op("pe", lambda e: e.matmul(pd[ps_, p * 64:(p + 1) * 64], lt[ps_, p, :], rt[ps_, p, :], start=(k == 0), stop=(k == len(specs) - 1)),
                          reads=reads, writes=[pd])

    def prep(self, ci):
        kb, cx, z, D_, nd, MK = self.kb, self.cx, self.z, self.D_, self.nd, self.MK
        identb, identf = cx.ident_bf, cx.ident_f32
        c = self.order[ci]
        c0 = c * 64
        ti = self.tin[ci % 2]
        S = self.sets[ci % 2]
        aT, rT, etT, LakT, MrbT, MrkT, QTb = S["aT"], S["rT"], S["etT"], S["LakT"], S["MrbT"], S["MrkT"], S["QTb"]
        bT, kT, LTf, QTf = self.bT, self.kT, self.LTf, self.QTf
        names = ["AT", "RT", "BT", "KT"]
        for j, nm in enumerate(names):
            kb.dma(ti[j][:], D_[nm][z][c0:c0 + 64, :], reads=[D_[nm][z]], writes=[ti[j]])
        kb.dma(ti[4][:], D_["V"][c0:c0 + 64, :], reads=[D_["V"]], writes=[ti[4]])
        et = self.etin[ci % 2]
        kb.dma(et[:], D_["ETOT"][z][c0:c0 + 64, :], reads=[D_["ETOT"][z]], writes=[et])
        A_, R_, B_, K_, V_ = ti
        for src, dst in ((A_, aT), (R_, rT), (B_, bT), (K_, kT)):
            pd = nd()
            pdb = pd[:].bitcast(BF16)
            for p in range(8):
                kb.op("pe", lambda e: e.transpose(pdb[:, p * 64:(p + 1) * 64], src[:, p * 128:(p + 1) * 128], identb[0:64, 0:64]), reads=[src, identb], writes=[pd])
            kb.op("act", lambda e: e.activation(dst[:].rearrange("p h t -> p (h t)"), pdb[:, 0:512], AF.Copy), reads=[pd], writes=[dst])
            yield
        pd = nd()
        for p in range(8):
            kb.op("pe", lambda e: e.transpose(pd[:, p * 64:(p + 1) * 64], et[:, p * 128:(p + 1) * 128], identf[0:64, 0:64]), reads=[et, identf], writes=[pd])
        kb.op("act", lambda e: e.activation(etT[:].rearrange("p h t -> p (h t)"), pd[:], AF.Copy), reads=[pd], writes=[etT])
        yield
        for src, nm in ((B_, "Bst"), (K_, "Kst"), (V_, "Vst")):
            dst = S[nm]
            pd = nd()
            sv = src[:].rearrange("s (p h2 i) -> s p h2 i", h2=2, i=64)
            for h2 in range(2):
                kb.op("pe", lambda e: e.matmul(pd[:], self.E[:, h2, :], sv[:, :, h2, :], start=(h2 == 0), stop=(h2 == 1)), reads=[src, self.E], writes=[pd])
            kb.op("dve", lambda e: e.tensor_copy(dst[:].rearrange("p h t -> p (h t)"), pd[:]), reads=[pd], writes=[dst])
            yield

        def v3(p):
            return p[:].rearrange("p (h t) -> p h t", h=8)

        def pair(lhsT_t, rhs_t, mask_i, dst, eng="dve"):
            pd = nd()
            self.mm_heads(pd, [(lhsT_t, rhs_t)], [lhsT_t, rhs_t])
            kb.op(eng, lambda e: e.tensor_tensor(dst[:], v3(pd), bc(MK[:, mask_i, :].unsqueeze(1), [128, 8, 64]), ALU.mult), reads=[pd, MK], writes=[dst])
        pair(bT, aT, self.mT_strict, LTf)
        yield
        L1, L1T = self.Lp[0], self.LTp[0]
        pair(aT, bT, self.mL_strict, L1)
        yield
        pair(kT, aT, self.mT_strict, LakT)
        yield
        pair(bT, rT, self.mT_incl, MrbT)
        yield
        pair(kT, rT, self.mT_incl, MrkT)
        kb.op("act", lambda e: e.activation(L1T[:], LTf[:], AF.Copy), reads=[LTf], writes=[L1T])
        kb.op("dve", lambda e: e.tensor_tensor(QTf[:], LTf[:], bc(self.I2[:].unsqueeze(1), [128, 8, 64]), ALU.add), reads=[LTf, self.I2], writes=[QTf])
        kb.op("act", lambda e: e.activation(QTb[:], QTf[:], AF.Copy), reads=[QTf], writes=[QTb])
        yield
        for lvl in range(5):
            L2, L2T = self.Lp[(lvl + 1) % 2], self.LTp[(lvl + 1) % 2]
            pd = nd()
            self.mm_heads(pd, [(L1T, L1)], [L1T, L1])
            kb.op("act", lambda e: e.activation(L2[:].rearrange("p h t -> p (h t)"), pd[:], AF.Copy), reads=[pd], writes=[L2])
            if lvl < 4:
                pd = nd()
                self.mm_heads(pd, [(L1, L1T)], [L1T, L1])
                kb.op("dve", lambda e: e.tensor_copy(L2T[:].rearrange("p h t -> p (h t)"), pd[:]), reads=[pd], writes=[L2T])
            yield
            pd = nd()
            self.mm_heads(pd, [(L2, QTb)], [L2, QTb])
            kb.op("dve", lambda e: e.tensor_tensor(QTf[:].rearrange("p h t -> p (h t)"), QTf[:].rearrange("p h t -> p (h t)"), pd[:], ALU.add), reads=[pd, QTf], writes=[QTf])
            kb.op("act", lambda e: e.activation(QTb[:], QTf[:], AF.Copy), reads=[QTf], writes=[QTb])
            L1, L1T = L2, L2T
            yield

    def seq(self, ci):
        kb, cx, z, D_, nd = self.kb, self.cx, self.z, self.D_, self.nd
        c = self.order[ci]
        is_lat = c >= 4
        c0 = c * 64
        S = self.sets[ci % 2]
        aT, rT, etT, LakT, MrbT, MrkT, QTb = S["aT"], S["rT"], S["etT"], S["LakT"], S["MrbT"], S["MrkT"], S["QTb"]
        Bst, Kst, Vst = S["Bst"], S["Kst"], S["Vst"]
        U0f, U0b, Xb, Pb = self.U0f, self.U0b, self.Xb, self.Pb
        pd = nd()
        self.mm_heads(pd, [(LakT, Vst), (aT, U0b)], [LakT, Vst, aT, U0b])
        kb.op("act", lambda e: e.activation(Xb[:].rearrange("p h t -> p (h t)"), pd[:], AF.Copy), reads=[pd], writes=[Xb])
        yield
        pd = nd()
        self.mm_heads(pd, [(QTb, Xb)], [QTb, Xb])
        kb.op("act", lambda e: e.activation(Pb[:].rearrange("p h t -> p (h t)"), pd[:], AF.Copy), reads=[pd], writes=[Pb])
        yield
        if is_lat:
            pd = nd()
            self.mm_heads(pd, [(rT, U0b), (MrbT, Pb), (MrkT, Vst)], [rT, U0b, MrbT, Pb, MrkT, Vst])
            yo = self.yout
            kb.op("act", lambda e: e.activation(yo[:].rearrange("p h t -> p (h t)"), pd[:], AF.Copy), reads=[pd], writes=[yo])
            yd = D_["Y"][z][c0 - 256:c0 - 192, :].rearrange("t (p h2 i) -> t p h2 i", h2=2, i=64)
            for h2 in range(2):
                kb.dma(yd[:, :, h2, :], yo[h2 * 64:(h2 + 1) * 64, :, :], reads=[yo], writes=[D_["Y"][z]])
            yield
        pd = nd()
        self.mm_heads(pd, [(Bst, Pb), (Kst, Vst)], [Bst, Pb, Kst, Vst])
        kb.op("dve", lambda e: e.tensor_tensor(U0f[:].rearrange("p h t -> p (h t)"), U0f[:].rearrange("p h t -> p (h t)"), pd[:], ALU.add), reads=[pd, U0f], writes=[U0f])
        kb.op("dve", lambda e: e.tensor_tensor(U0f[:], U0f[:], etT[:], ALU.mult), reads=[U0f, etT], writes=[U0f])
        kb.op("act", lambda e: e.activation(U0b[:], U0f[:], AF.Copy), reads=[U0f], writes=[U0b])
        yield


def run_gens(gens):
    gens = list(gens)
    while gens:
        for g in list(gens):
            try:
                next(g)
            except StopIteration:
                gens.remove(g)


def phase_rwkv_scans(kb, cx, D_, consts):
    cx.nd = BankRR(cx.psum)
    sc = [RwkvScan(kb, cx, z, D_, consts) for z in range(2)]
    run_gens([s_.prep(0) for s_ in sc])
    for ci in range(NCH):
        gens = []
        for s_ in sc:
            if ci + 1 < NCH:
                gens.append(s_.prep(ci + 1))
            gens.append(s_.seq(ci))
        run_gens(gens)


def phase_rwkv_readout(kb, cx, w, D_, MIXT):
    ps = cx.psum
    identb = cx.ident_bf
    tabs = kb.sb([128, 2, 1024], F32, "ro_tabs")
    kb.dma(tabs[:, 0, :], w["od_ln_w"][0].partition_broadcast(128), writes=[tabs])
    kb.dma(tabs[:, 1, :], w["od_ln_b"][0].partition_broadcast(128), writes=[tabs])
    epsc = kb.sb([128, 1], F32, "ro_eps")
    kb.op("dve", lambda e: e.memset(epsc[:], 64e-5), writes=[epsc])
    ys = [kb.sb([128, 16, 64], F32, "ro_ys%d" % i) for i in range(2)]
    bo = [kb.sb([128, 1024], F32, "ro_bo%d" % i) for i in range(2)]
    gg = [kb.sb([128, 1024], F32, "ro_g%d" % i) for i in range(2)]
    sq_l = [kb.sb([128, 16, 64], F32, "ro_sq%d" % i) for i in range(2)]
    st_l = [kb.sb([128, 32], F32, "ro_st%d" % i) for i in range(2)]
    ob_l = [kb.sb([128, 1024], BF16, "ro_ob%d" % i) for i in range(2)]
    oT = [kb.sb([128, 8, 128], BF16, "ro_oT%d" % i) for i in range(2)]
    for n in range(16):
        rs = slice(n * 128, (n + 1) * 128)
        tsl = slice(256 + n * 128, 256 + (n + 1) * 128)
        y, b_, g_ = ys[n % 2], bo[n % 2], gg[n % 2]
        sq, st, ob = sq_l[n % 2], st_l[n % 2], ob_l[n % 2]
        yv = y[:].rearrange("p h j -> p (h j)")
        kb.dma(yv, D_["Y"][0][rs, :], reads=[D_["Y"][0]], writes=[y])
        kb.dma(sq[:].rearrange("p h j -> p (h j)"), D_["Y"][1][rs, :], reads=[D_["Y"][1]], writes=[sq])
        kb.op("dve", lambda e: e.tensor_tensor(y[:], y[:], sq[:], ALU.add), reads=[y, sq], writes=[y])
        kb.dma(b_[:], D_["BONUS"][tsl, :], reads=[D_["BONUS"]], writes=[b_])
        kb.dma(g_[:], D_["G"][tsl, :], reads=[D_["G"]], writes=[g_])
        kb.op("dve", lambda e: e.tensor_reduce(st[:, 0:16], y[:], AX.X, ALU.add), reads=[y], writes=[st])
        kb.op("dve", lambda e: e.tensor_scalar(st[:, 0:16], st[:, 0:16], 1.0 / 64, None, ALU.mult), reads=[st], writes=[st])
        kb.op("dve", lambda e: e.tensor_tensor(y[:], y[:], bc(st[:, 0:16].unsqueeze(2), [128, 16, 64]), ALU.subtract), reads=[y, st], writes=[y])
        kb.op("act", lambda e: e.activation(sq[:], y[:], AF.Square), reads=[y], writes=[sq])
        kb.op("dve", lambda e: e.tensor_reduce(st[:, 16:32], sq[:], AX.X, ALU.add), reads=[sq], writes=[st])
        kb.op("act", lambda e: e.activation(st[:, 16:32], st[:, 16:32], AF.Sqrt, bias=epsc[:, 0:1], scale=1.0 / 64), reads=[st, epsc], writes=[st])
        kb.op("dve", lambda e: e.reciprocal(st[:, 16:32], st[:, 16:32]), reads=[st], writes=[st])
        kb.op("dve", lambda e: e.tensor_tensor(y[:], y[:], bc(st[:, 16:32].unsqueeze(2), [128, 16, 64]), ALU.mult), reads=[y, st], writes=[y])
        kb.op("dve", lambda e: e.tensor_tensor(yv, yv, tabs[:, 0, :], ALU.mult), reads=[y, tabs], writes=[y])
        kb.op("pool", lambda e: e.tensor_tensor(b_[:], b_[:], tabs[:, 1, :], ALU.add), reads=[b_, tabs], writes=[b_])
        kb.op("dve", lambda e: e.tensor_tensor(yv, yv, b_[:], ALU.add), reads=[y, b_], writes=[y])
        kb.op("dve", lambda e: e.tensor_tensor(ob[:], yv, g_[:], ALU.mult), reads=[y, g_], writes=[ob])
        o = oT[n % 2]
        for half in range(2):
            ptr = ps[(2 * n + half) % 4]
            for c in range(4):
                cc = half * 4 + c
                kb.op("pe", lambda e: e.transpose(ptr[:].bitcast(BF16)[:, c * 128:(c + 1) * 128], ob[:, cc * 128:(cc + 1) * 128], identb[:]), reads=[ob, identb], writes=[ptr])
            kb.op("act", lambda e: e.activation(o[:, half * 4:(half + 1) * 4, :].rearrange("p c t -> p (c t)"), ptr[:].bitcast(BF16)[:, 0:512], AF.Copy), reads=[ptr], writes=[o])
        kb.dma(fm(MIXT[:], 256 + n * 128, 128), o[:], reads=[o], writes=[MIXT])


RW_NAMES = ["od_mu", "od_w_rkv", "od_w0", "od_w1", "od_w2", "od_a0", "od_a1", "od_a2", "od_g1", "od_g2", "od_k_k", "od_k_a", "od_r_k",
            "od_ln_w", "od_ln_b", "od_w_o"]


def rwkv_dram(kb):
    D_ = {}
    BIG = kb.dram("rw_BIG", [NTOK, 9, 1024], BF16)
    BIGF = kb.dram("rw_BIGF", [NTOK, 4, 1024], F32)
    D_["BIG"], D_["BIGF"] = BIG, BIGF

    def view(big, slot, name):
        v = T(big.t[:, slot, :], name)
        v.ws, v.rs = big.ws, big.rs
        return v
    D_["V"] = view(BIG, 0, "rw_V")
    for k_, nm in enumerate(("RT", "BT", "KT", "AT")):
        D_[nm] = [view(BIG, 1 + 2 * k_ + z, "rw_%s%d" % (nm, z)) for z in range(2)]
    D_["ETOT"] = [view(BIGF, z, "rw_ETOT%d" % z) for z in range(2)]
    D_["BONUS"] = view(BIGF, 2, "rw_BONUS")
    D_["G"] = view(BIGF, 3, "rw_G")
    D_["Y"] = [kb.dram("rw_Y%d" % z, [NLAT, 1024], F32) for z in range(2)]
    return D_


def layer1_mixer(kb, cx, w, consts, XT1, XT2, MIXT):
    D_ = rwkv_dram(kb)
    HT = kb.dram("rw_HT", [1024, NTOK], BF16)
    with kb.scope():
        hT = kb.sb([128, 8, NTOK], BF16, "hT")
        with kb.scope():
            setup_modbuf(kb, cx)
            phase_modulate(kb, cx, XT1, 1, w["norm_mix_w"][1], 0, 1, hT)
        for c in range(8):
            kb.dma(HT[c * 128:(c + 1) * 128, :], hT[:, c, :], reads=[hT], writes=[HT])
    with kb.scope():
        phase_rwkv_feat(kb, cx, HT, w, consts, D_)
    with kb.scope():
        phase_rwkv_scans(kb, cx, D_, consts)
    with kb.scope():
        phase_rwkv_readout(kb, cx, w, D_, MIXT)
    with kb.scope():
        phase_wout(kb, cx, 1, w["od_w_o"][0], MIXT, XT1, XT2, tiles=TILES[1:])
    return D_


def rwkv_consts():
    c = {}
    s = np.arange(128)[:, None]; t = np.arange(128)[None, :]
    same = (s // 64) == (t // 64)
    c["rw_CM"] = np.stack([same & (s <= t), same & (s < t), same & (s >= t), same & (s > t), same]).astype(np.float32)
    r = np.arange(64)[:, None]; cc = np.arange(64)[None, :]
    c["rw_MK"] = np.stack([r < cc, r <= cc, r > cc, r >= cc]).astype(np.float32)
    c["rw_MK2"] = np.concatenate([c["rw_MK"], c["rw_MK"]], axis=1)
    c["rw_I2"] = np.concatenate([np.eye(64), np.eye(64)], 0).astype(np.float32)
    E = np.zeros((2, 64, 128), np.float32)
    E[0, np.arange(64), np.arange(64)] = 1.0
    E[1, np.arange(64), 64 + np.arange(64)] = 1.0
    c["rw_E"] = E
    return c
```

```python
import numpy as np
import concourse.bass as bass
import concourse.mybir as mybir
from concourse.bass_utils import run_bass_kernel_spmd

F32 = mybir.dt.float32
BF16 = mybir.dt.bfloat16
AF = mybir.ActivationFunctionType
ALU = mybir.AluOpType
AX = mybir.AxisListType

D = 1024
NCTX = 256
NLAT = 2048
NTOK = NCTX + NLAT
EPS = 1e-6


class H:
    __slots__ = ("name", "ws", "rs")

    def __init__(self, name=""):
        self.name = name
        self.ws = {}
        self.rs = {}


class T(H):
    __slots__ = ("t",)

    def __init__(self, t, name=""):
        H.__init__(self, name)
        self.t = t

    def __getitem__(self, k):
        return self.t[k]


class KB:
    ENG = ("pe", "dve", "act", "pool", "sp")

    def __init__(self, n_dma_sems=48):
        nc = bass.Bass("TRN2", target_bir_lowering=False)
        self.nc = nc
        self.eng = dict(pe=nc.tensor, dve=nc.vector, act=nc.scalar, pool=nc.gpsimd, sp=nc.sync)
        self.esem = {e: nc.alloc_semaphore("es_" + e) for e in self.ENG}
        self.ecnt = {e: 0 for e in self.ENG}
        self.known = {e: {} for e in self.ENG}
        self.dsems = [nc.alloc_semaphore("ds_%d" % i) for i in range(n_dma_sems)]
        self.dval = [0] * n_dma_sems
        self.dnext = 0
        self.n_ins = 0
        self.n_wait = 0
        self.uid = 0
        self.stacks = []
        self.stage_i = 0
        self.pending = None
        self.attach_waits = True
        self.snaps = {}

    def sb(self, shape, dtype=F32, name=None):
        self.uid += 1
        name = (name or "sb") + "_%d" % self.uid
        if self.stacks:
            t = self.stacks[-1].enter_context(self.nc.sbuf_tensor(name, list(shape), dtype))
        else:
            t = self.nc.alloc_sbuf_tensor(name, list(shape), dtype)
        return T(t, name)

    def scope(self):
        kb = self

        class _S:
            def __enter__(s2):
                import contextlib
                st = contextlib.ExitStack()
                kb.stacks.append(st)
                return st

            def __exit__(s2, *a):
                kb.barrier()
                st = kb.stacks.pop()
                st.close()
                return False
        return _S()

    def barrier(self):
        for e in self.ENG:
            for i, v in enumerate(self.dval):
                if v:
                    self._wait(e, ("dma", i), v)
            for p in self.ENG:
                if p != e and self.ecnt[p]:
                    self._wait(e, ("eng", p), self.ecnt[p])

    def ps(self, shape=(128, 512), dtype=F32, name=None):
        self.uid += 1
        name = name or "ps%d" % self.uid
        return T(self.nc.alloc_psum_tensor(name, list(shape), dtype), name)

    def dram(self, name, shape, dtype=F32, kind="Internal"):
        return T(self.nc.dram_tensor(name, list(shape), dtype, kind=kind), name)

    def _wait(self, e, key, val):
        if key[0] == "eng":
            if key[1] == e and e == "pe":
                return
            sem = self.esem[key[1]]
        else:
            sem = self.dsems[key[1]]
            val = max(val, self.dval[key[1]])
        if self.known[e].get(key, 0) >= val:
            return
        self.known[e][key] = val
        self.n_wait += 1
        sn = self.snaps.get((key, val))
        if sn is not None:
            kn = self.known[e]
            for k2, v2 in sn.items():
                if kn.get(k2, 0) < v2:
                    kn[k2] = v2
        if self.pending is not None:
            self.pending.append((sem, val))
        else:
            self.eng[e].wait_ge(sem, val)

    def _deps(self, e, reads, writes):
        me = ("eng", e)
        for h in reads:
            for k, v in h.ws.items():
                self._wait(e, k, v)
        for h in writes:
            for k, v in h.ws.items():
                self._wait(e, k, v)
            for k, v in h.rs.items():
                self._wait(e, k, v)

    def _commit(self, key, val, reads, writes):
        for h in reads:
            if h.rs.get(key, 0) < val:
                h.rs[key] = val
        for h in writes:
            if h.ws.get(key, 0) < val:
                h.ws[key] = val

    def op(self, e, fn, reads=(), writes=()):
        if self.attach_waits:
            self.pending = []
            self._deps(e, reads, writes)
            pend, self.pending = self.pending, None
            for (sem, val) in pend[:-1]:
                self.eng[e].wait_ge(sem, val)
            ins = fn(self.eng[e])
            if pend:
                ins._wait_ge(pend[-1][0], pend[-1][1])
        else:
            self._deps(e, reads, writes)
            ins = fn(self.eng[e])
        self.ecnt[e] += 1
        ins.then_inc(self.esem[e], 1)
        self.snaps[(("eng", e), self.ecnt[e])] = dict(self.known[e])
        self._commit(("eng", e), self.ecnt[e], reads, writes)
        self.n_ins += 1
        return ins

    def dma(self, out, in_, reads=(), writes=(), q="sp", **kw):
        self.pending = []
        self._deps(q, reads, writes)
        pend, self.pending = self.pending, None
        for (sem, val) in pend[:-1]:
            self.eng[q].wait_ge(sem, val)
        i = self.dnext
        self.dnext = (self.dnext + 1) % len(self.dsems)
        ins = self.eng[q].dma_start(out=out, in_=in_, **kw)
        if pend:
            ins._wait_ge(pend[-1][0], pend[-1][1])
        self.dval[i] += 16
        ins.then_inc(self.dsems[i], 16)
        self.snaps[(("dma", i), self.dval[i])] = dict(self.known[q])
        self._commit(("dma", i), self.dval[i], reads, writes)
        self.n_ins += 1
        return ins

    def set_stage(self, n=4, size=1024):
        self.stage = [self.sb([128, size], F32, "stage%d" % i) for i in range(n)]
        self.stage_size = size

    def load_cast(self, dst_ap, src_ap, dst_h, engs=("act", "pool", "act")):
        shp = list(dst_ap.shape)
        P = shp[0]
        n = 1
        for d_ in shp[1:]:
            n *= d_
        assert n <= self.stage_size, (shp, self.stage_size)
        st = self.stage[self.stage_i % len(self.stage)]
        eng = engs[self.stage_i % len(engs)]
        self.stage_i += 1
        v = st[0:P, 0:n]
        if len(shp) == 3:
            v = v.rearrange("p (a b) -> p a b", a=shp[1])
        elif len(shp) == 4:
            v = v.rearrange("p (a b c) -> p a b c", a=shp[1], b=shp[2])
        self.dma(v, src_ap, writes=[st])
        if eng == "act":
            self.op("act", lambda e: e.activation(dst_ap, v, AF.Copy), reads=[st], writes=[dst_h])
        else:
            self.op(eng, lambda e: e.tensor_copy(dst_ap, v), reads=[st], writes=[dst_h])

    def finish(self):
        for i, v in enumerate(self.dval):
            if v:
                self._wait("sp", ("dma", i), v)
        for e in self.ENG:
            if e != "sp" and self.ecnt[e]:
                self._wait("sp", ("eng", e), self.ecnt[e])


class Ctx:
    pass


def fm(ap_dram, t0, n):
    return ap_dram.rearrange("(c p) t -> p c t", p=128)[:, :, t0:t0 + n]


TILES = [(0, 256, True)] + [(256 + 512 * i, 512, False) for i in range(4)]


def phase_mod(kb, cx, c_lat, c_ctx, ada_w, ada_b):
    mod = cx.mod
    sT = kb.sb([128, 8, 2], F32, "sT")
    cin = kb.sb([128, 8, 2], F32, "cin")
    with kb.nc.allow_non_contiguous_dma(reason="tiny"):
        kb.dma(cin[:, :, 0], c_lat[:].rearrange("(c p) -> p c", p=128), writes=[cin])
        kb.dma(cin[:, :, 1], c_ctx[:].rearrange("(c p) -> p c", p=128), writes=[cin])
    kb.op("act", lambda e: e.activation(sT[:], cin[:], AF.Silu), reads=[cin], writes=[sT])
    ident = cx.ident_f32
    wbuf = [kb.sb([128, 8, 512], F32, "adaw%d" % i) for i in range(4)]
    mrow = kb.sb([2, 6144], F32, "mrow")
    brow = kb.sb([2, 6144], F32, "brow")
    pm = cx.psum[0]
    pt = cx.psum[1]
    it = 0
    for L in range(2):
        kb.dma(brow[0:1, :], ada_b[L:L + 1, :], writes=[brow])
        kb.dma(brow[1:2, :], ada_b[L:L + 1, :], writes=[brow])
        for nt in range(12):
            wb = wbuf[it % 4]
            it += 1
            kb.dma(wb[:], ada_w[L].rearrange("(c p) n -> p c n", p=128)[:, :, nt * 512:(nt + 1) * 512], writes=[wb])
            for kc in range(8):
                kb.op("pe", lambda e: e.matmul(pm[0:2, :], sT[:, kc, :], wb[:, kc, :], start=(kc == 0), stop=(kc == 7)),
                      reads=[sT, wb], writes=[pm])
            kb.op("dve", lambda e: e.tensor_tensor(mrow[:, nt * 512:(nt + 1) * 512], pm[0:2, :], brow[:, nt * 512:(nt + 1) * 512], ALU.add),
                  reads=[pm, brow], writes=[mrow])
        for c in range(48):
            kb.op("pe", lambda e: e.matmul(pt[:, 2 * c:2 * c + 2], mrow[0:2, c * 128:(c + 1) * 128], ident[0:2, 0:2], start=True, stop=True),
                  reads=[mrow, ident], writes=[pt])
        kb.op("dve", lambda e: e.tensor_copy(mod[:, L, :, :], pt[:, 0:96].rearrange("p (c s) -> p s c", s=2)), reads=[pt], writes=[mod])


def mod_vec(cx, L, s, idx):
    return cx.mod[:, L, s, idx * 8:(idx + 1) * 8]


def phase_modulate(kb, cx, XT, L, normw_dram, shift_idx, scale_idx, hT, tiles=TILES, hT_t0=0, first=True):
    if first:
        nw = kb.sb([128, 8], F32)
        with kb.nc.allow_non_contiguous_dma(reason="tiny"):
            kb.dma(nw[:], normw_dram.rearrange("(c p) -> p c", p=128), writes=[nw])
        gmul = kb.sb([128, 2, 8], F32)
        for s in range(2):
            kb.op("dve", lambda e: e.scalar_tensor_tensor(gmul[:, s, :], mod_vec(cx, L, s, scale_idx), 1.0, nw[:], ALU.add, ALU.mult),
                  reads=[cx.mod, nw], writes=[gmul])
        cx.gmul_cur = gmul
    gmul = cx.gmul_cur
    xb = cx.xbuf
    for ti, (t0, n, is_ctx) in enumerate(tiles):
        s = 1 if is_ctx else 0
        par = getattr(cx, "mod_cnt", 0) % 2
        cx.mod_cnt = getattr(cx, "mod_cnt", 0) + 1
        x = xb[par]
        sqb = cx.sqb[par]
        rstd = cx.rstd[par]
        ps = cx.psum[2 + par]
        kb.dma(x[:, :, 0:n], fm(XT[:], t0, n), reads=[XT], writes=[x])
        kb.op("act", lambda e: e.activation(sqb[:, :, 0:n], x[:, :, 0:n], AF.Square), reads=[x], writes=[sqb])
        for c in range(8):
            kb.op("pe", lambda e: e.matmul(ps[:, 0:n], cx.ones_bf[:], sqb[:, c, 0:n], start=(c == 0), stop=(c == 7)),
                  reads=[sqb, cx.ones_bf], writes=[ps])
        kb.op("act", lambda e: e.activation(rstd[:, 0:n], ps[:, 0:n], AF.Sqrt, bias=cx.eps_col[:, 0:1], scale=1.0 / D), reads=[ps, cx.eps_col], writes=[rstd])
        kb.op("dve", lambda e: e.reciprocal(rstd[:, 0:n], rstd[:, 0:n]), reads=[rstd], writes=[rstd])
        for c in range(8):
            sq = cx.sqr[c % 4]
            kb.op("dve", lambda e: e.tensor_tensor(sq[:, 0:n], x[:, c, 0:n], rstd[:, 0:n], ALU.mult), reads=[x, rstd], writes=[sq])
            kb.op("act", lambda e: e.activation(hT[:, c, t0 - hT_t0:t0 - hT_t0 + n], sq[:, 0:n], AF.Identity,
                                                bias=mod_vec(cx, L, s, shift_idx)[:, c:c + 1], scale=gmul[:, s, c:c + 1]),
                  reads=[sq, cx.mod, gmul], writes=[hT])


def phase_ffn(kb, cx, XT, L, hT, hT_t0, tiles, w13_list, w2_list, hidden, gate_idx, gates=None, hc_group=4):
    nE = len(w13_list)
    nhc = hidden // 128
    groups = [(g0, min(hc_group, nhc - g0)) for g0 in range(0, nhc, hc_group)]
    ntok = sum(n for _, n, _ in tiles)
    acc = cx.ffn_acc
    w13b = cx.w13buf
    w2b = cx.w2buf
    act = cx.actbuf
    tmp = cx.ffn_tmp
    it = 0
    pidx = [0]
    units = []
    gi = 0
    for e_i in range(nE):
        for (g0, gn) in groups:
            for ti in range(len(tiles)):
                units.append((e_i, g0, gn, ti, gi))
            gi += 1
    offs = []
    o_ = 0
    for (t0, n, _) in tiles:
        offs.append(o_)
        o_ += n
    wcur = {}

    def load_w(gidx, e_i, g0, gn):
        wa = w13b[gidx % 2]
        wb = w2b[gidx % 2]
        w13 = w13_list[e_i].rearrange("(c p) n -> p c n", p=128)
        for half in range(2):
            for cc in range(0, 8, 2):
                kb.load_cast(wa[:, cc:cc + 2, half, 0:gn * 128], w13[:, cc:cc + 2, half * hidden + g0 * 128: half * hidden + (g0 + gn) * 128], wa)
        w2 = w2_list[e_i].rearrange("(c p) n -> p c n", p=128)
        for cc in range(gn):
            kb.load_cast(wb[:, cc, :], w2[:, g0 + cc, :], wb)
        wcur[gidx] = (wa, wb)

    ginfo = {}
    for (e_i, g0, gn, ti, gidx) in units:
        ginfo[gidx] = (e_i, g0, gn)
    ngroups = len(ginfo)

    def P1(u):
        e_i, g0, gn, ti, gidx = units[u]
        wa, wb = wcur[gidx]
        t0, n, is_ctx = tiles[ti]
        a = act[u % 2]
        for hc in range(gn):
            pg = cx.psum[pidx[0] % 8]; pidx[0] += 1
            pu = cx.psum[pidx[0] % 8]; pidx[0] += 1
            for kc in range(8):
                kb.op("pe", lambda e: e.matmul(pg[:, 0:n], wa[:, kc, 0, hc * 128:(hc + 1) * 128], hT[:, kc, t0 - hT_t0:t0 - hT_t0 + n],
                                               start=(kc == 0), stop=(kc == 7)), reads=[wa, hT], writes=[pg])
            for kc in range(8):
                kb.op("pe", lambda e: e.matmul(pu[:, 0:n], wa[:, kc, 1, hc * 128:(hc + 1) * 128], hT[:, kc, t0 - hT_t0:t0 - hT_t0 + n],
                                               start=(kc == 0), stop=(kc == 7)), reads=[wa, hT], writes=[pu])
            tt = tmp[hc % 2]
            kb.op("act", lambda e: e.activation(tt[:, 0:n], pg[:, 0:n], AF.Silu), reads=[pg], writes=[tt])
            if gates is None:
                kb.op("dve", lambda e: e.tensor_tensor(a[:, hc, 0:n], tt[:, 0:n], pu[:, 0:n], ALU.mult), reads=[tt, pu], writes=[a])
            else:
                kb.op("dve", lambda e: e.tensor_tensor(tt[:, 0:n], tt[:, 0:n], pu[:, 0:n], ALU.mult), reads=[tt, pu], writes=[tt])
                gt = gates[ti]
                kb.op("dve", lambda e: e.tensor_tensor(a[:, hc, 0:n], tt[:, 0:n], gt[:, e_i, 0:n], ALU.mult), reads=[tt, gt], writes=[a])

    def P2(u):
        e_i, g0, gn, ti, gidx = units[u]
        wa, wb = wcur[gidx]
        t0, n, is_ctx = tiles[ti]
        a = act[u % 2]
        off = offs[ti]
        for dc in range(8):
            po = cx.psum[pidx[0] % 8]; pidx[0] += 1
            for hc in range(gn):
                kb.op("pe", lambda e: e.matmul(po[:, 0:n], wb[:, hc, dc * 128:(dc + 1) * 128], a[:, hc, 0:n],
                                               start=(hc == 0), stop=(hc == gn - 1)), reads=[wb, a], writes=[po])
            if gidx == 0:
                kb.op("dve", lambda e: e.tensor_copy(acc[:, dc, off:off + n], po[:, 0:n]), reads=[po], writes=[acc])
            else:
                kb.op("dve", lambda e: e.tensor_tensor(acc[:, dc, off:off + n], acc[:, dc, off:off + n], po[:, 0:n], ALU.add), reads=[po, acc], writes=[acc])

    load_w(0, *ginfo[0])
    for u in range(len(units) + 1):
        if u < len(units):
            P1(u)
        if u >= 1:
            P2(u - 1)
        if u < len(units) and units[u][3] == 0 and units[u][4] + 1 < ngroups:
            g1 = units[u][4] + 1
            load_w(g1, *ginfo[g1])
    off = 0
    k = 0
    for ti, (t0, n, is_ctx) in enumerate(tiles):
        s = 1 if is_ctx else 0
        for c in range(8):
            x = tmp[k % 2]; k += 1
            kb.dma(x[:, 0:n], XT[c * 128:(c + 1) * 128, t0:t0 + n], reads=[XT], writes=[x])
            kb.op("dve", lambda e: e.scalar_tensor_tensor(x[:, 0:n], acc[:, c, off:off + n], mod_vec(cx, L, s, gate_idx)[:, c:c + 1], x[:, 0:n], ALU.mult, ALU.add),
                  reads=[acc, cx.mod, x], writes=[x])
            kb.dma(XT[c * 128:(c + 1) * 128, t0:t0 + n], x[:, 0:n], reads=[x], writes=[XT])
        off += n


def setup_common(kb, cx, consts):
    cx.psd = [kb.ps((128, 1024), F32, "psd%d" % i) for i in range(4)]
    cx.psum = []
    for i in range(4):
        cx.psum.append(T(cx.psd[i].t[:, 0:512], "psum%d" % (2 * i)))
        cx.psum.append(T(cx.psd[i].t[:, 512:1024], "psum%d" % (2 * i + 1)))
    cx.ident_f32 = kb.sb([128, 128], F32, "ident_f32")
    cx.ones_f32 = kb.sb([128, 128], F32, "ones_f32")
    cx.eps_col = kb.sb([128, 1], F32, "eps_col")
    kb.dma(cx.ident_f32[:], consts["ident"][:], writes=[cx.ident_f32])
    kb.op("dve", lambda e: e.memset(cx.ones_f32[:], 1.0), writes=[cx.ones_f32])
    kb.op("dve", lambda e: e.memset(cx.eps_col[:], EPS), writes=[cx.eps_col])
    cx.gn_eps_col = kb.sb([128, 1], F32, "gn_eps_col")
    kb.op("dve", lambda e: e.memset(cx.gn_eps_col[:], 1e-5), writes=[cx.gn_eps_col])
    cx.ident_bf = kb.sb([128, 128], BF16, "ident_bf")
    kb.op("dve", lambda e: e.tensor_copy(cx.ident_bf[:], cx.ident_f32[:]), reads=[cx.ident_f32], writes=[cx.ident_bf])


def setup_modbuf(kb, cx):
    cx.xbuf = [kb.sb([128, 8, 512], F32, "xbuf%d" % i) for i in range(2)]
    cx.sqr = [kb.sb([128, 512], F32, "sqr%d" % i) for i in range(4)]
    cx.sqb = [kb.sb([128, 8, 512], BF16, "sqb%d" % i) for i in range(2)]
    cx.rstd = [kb.sb([128, 512], F32, "rstd%d" % i) for i in range(2)]
    cx.ones_bf = kb.sb([128, 128], BF16, "ones_bf")
    kb.op("dve", lambda e: e.memset(cx.ones_bf[:], 1.0), writes=[cx.ones_bf])


def setup_ffn(kb, cx, ntok_max, hc_group=4):
    cx.ffn_acc = kb.sb([128, 8, ntok_max], F32, "ffn_acc")
    cx.w13buf = [kb.sb([128, 8, 2, hc_group * 128], BF16, "w13b%d" % i) for i in range(2)]
    cx.w2buf = [kb.sb([128, hc_group, 1024], BF16, "w2b%d" % i) for i in range(2)]
    cx.actbuf = [kb.sb([128, hc_group, 512], BF16, "actb%d" % i) for i in range(2)]
    cx.ffn_tmp = [kb.sb([128, 512], F32, "ffnt%d" % i) for i in range(2)]
    kb.set_stage(4, 1024)


def phase_mod_scoped(kb, cx, c_lat, c_ctx, ada_w, ada_b):
    cx.mod = kb.sb([128, 2, 2, 48], F32, "mod")
    with kb.scope():
        phase_mod(kb, cx, c_lat, c_ctx, ada_w, ada_b)


RET_PF = {0: 0, 1: 1}
RET_PB = {0: 1, 1: 0}
for _i in range(16):
    RET_PF[2 + _i] = 2 + _i
    RET_PB[2 + _i] = 2 + (15 - _i)


def bc(ap, shape):
    return ap.broadcast_to(list(shape))


def phase_retention(kb, cx, hT, w_in, dec_f, dec_b, gn_w, consts, MIXT, na_args=None):
    ps = cx.psum
    identb = cx.ident_bf
    wb = kb.sb([128, 8, 2048], BF16, "w_ret")
    wv = w_in.rearrange("(c p) n -> p c n", p=128)
    na = None
    if na_args is not None:
        na = NaProj(kb, cx, w_in, na_args["qn_w"], na_args["kn_w"])
    with kb.scope():
        kb.set_stage(8, 1024)
        for g in range(4):
            for cc in range(0, 8, 2):
                kb.load_cast(wb[:, cc:cc + 2, g * 512:(g + 1) * 512], wv[:, cc:cc + 2, g * 512:(g + 1) * 512], wb, engs=("act", "pool", "dve"))
        if na is not None:
            na.load_weights()
    if na is not None:
        na.alloc()
    cos_t = kb.sb([128, 18, 128], F32, "cos_t")
    sin_t = kb.sb([128, 18, 128], F32, "sin_t")
    kb.dma(cos_t[:], consts["rope_cos"][:].rearrange("(n p) d -> p n d", p=128), writes=[cos_t])
    kb.dma(sin_t[:], consts["rope_sin"][:].rearrange("(n p) d -> p n d", p=128), writes=[sin_t])
    qT = kb.sb([128, 18, 4, 128], BF16, "qT")
    kT = kb.sb([128, 18, 4, 128], BF16, "kT")
    v_all = kb.sb([128, 18, 512], BF16, "v_all")
    t1 = kb.sb([128, 512], F32, "rt1")
    t2 = kb.sb([128, 512], F32, "rt2")
    qk_tm = [kb.sb([128, 512], BF16, "qk_tm%d" % i) for i in range(2)]
    lg = kb.sb([128, 8], F32, "lg")
    kb.dma(lg[:, 0:4], dec_f.partition_broadcast(128), writes=[lg])
    kb.dma(lg[:, 4:8], dec_b.partition_broadcast(128), writes=[lg])
    kb.op("act", lambda e: e.activation(lg[:], lg[:], AF.Exp), reads=[lg], writes=[lg])
    kb.op("dve", lambda e: e.tensor_scalar(lg[:], lg[:], -1.0, None, ALU.mult), reads=[lg], writes=[lg])
    cst = kb.sb([128, 5, 128], F32, "ret_cst")
    kb.dma(cst[:], consts["ret_cst"][:].rearrange("k p t -> p k t"), writes=[cst])
    Dsame = kb.sb([128, 4, 128], F32, "Dsame")
    Dfull = kb.sb([128, 2, 4, 128], F32, "Dfull")
    GP = kb.sb([128, 2, 4, 18], F32, "GP")
    nlg = kb.sb([128, 4], F32, "nlg")
    kidx = kb.sb([128, 18], F32, "kidx")
    kb.dma(kidx[:], consts["kidx128"][:].partition_broadcast(128), writes=[kidx])
    ksc = 128.0 ** -0.5
    for h in range(4):
        kb.op("act", lambda e: e.activation(t1[:, 0:128], cst[:, 1, :], AF.Exp, scale=lg[:, h:h + 1]), reads=[cst, lg], writes=[t1])
        kb.op("dve", lambda e: e.scalar_tensor_tensor(t1[:, 0:128], t1[:, 0:128], ksc, cst[:, 3, :], ALU.mult, ALU.mult), reads=[t1, cst], writes=[t1])
        kb.op("act", lambda e: e.activation(t2[:, 0:128], cst[:, 2, :], AF.Exp, scale=lg[:, 4 + h:5 + h]), reads=[cst, lg], writes=[t2])
        kb.op("dve", lambda e: e.scalar_tensor_tensor(t2[:, 0:128], t2[:, 0:128], ksc, cst[:, 4, :], ALU.mult, ALU.mult), reads=[t2, cst], writes=[t2])
        kb.op("dve", lambda e: e.tensor_tensor(Dsame[:, h, :], t1[:, 0:128], t2[:, 0:128], ALU.add), reads=[t1, t2], writes=[Dsame])
        kb.op("act", lambda e: e.activation(Dfull[:, 0, h, :], cst[:, 0, :], AF.Exp, scale=lg[:, h:h + 1]), reads=[cst, lg], writes=[Dfull])
        kb.op("dve", lambda e: e.tensor_scalar(nlg[:, h:h + 1], lg[:, 4 + h:5 + h], -1.0, None, ALU.mult), reads=[lg], writes=[nlg])
        kb.op("act", lambda e: e.activation(Dfull[:, 1, h, :], cst[:, 0, :], AF.Exp, scale=nlg[:, h:h + 1]), reads=[cst, nlg], writes=[Dfull])
        for d in range(2):
            kb.op("act", lambda e: e.activation(GP[:, d, h, :], kidx[:], AF.Exp, scale=lg[:, 4 * d + h:4 * d + h + 1]), reads=[kidx, lg], writes=[GP])
    kb.op("dve", lambda e: e.tensor_scalar(Dfull[:].rearrange("p a b c -> p (a b c)"), Dfull[:].rearrange("p a b c -> p (a b c)"), ksc, None, ALU.mult), reads=[Dfull], writes=[Dfull])
    for n in range(18):
        tsl = slice(n * 128, (n + 1) * 128)
        pq, pk, pv = ps[0], ps[1], ps[2]
        for g, pp in ((0, pq), (1, pk), (2, pv)):
            for kc in range(8):
                kb.op("pe", lambda e: e.matmul(pp[:], hT[:, kc, tsl], wb[:, kc, g * 512:(g + 1) * 512], start=(kc == 0), stop=(kc == 7)),
                      reads=[hT, wb], writes=[pp])
        kb.op("act", lambda e: e.activation(v_all[:, n, :], pv[:], AF.Copy), reads=[pv], writes=[v_all])
        for gi, (pp, dstT) in enumerate(((pq, qT), (pk, kT))):
            tm = qk_tm[gi]
            kb.op("dve", lambda e: e.tensor_tensor(t1[:].rearrange("p (h d) -> p h d", h=4), pp[:].rearrange("p (h d) -> p h d", h=4),
                                                   bc(cos_t[:, n:n + 1, :], [128, 4, 128]), ALU.mult), reads=[pp, cos_t], writes=[t1])
            ppv = pp[:].rearrange("p (hb two f) -> p hb two f", two=2, f=32)
            t2v = t2[:].rearrange("p (hb two f) -> p hb two f", two=2, f=32)
            snv = sin_t[:, n, :].rearrange("p (b two f) -> p b two f", two=2, f=32)
            for half in range(2):
                kb.op("dve", lambda e: e.tensor_tensor(t2v[:, :, half, :].rearrange("p (h b) f -> p h b f", h=4),
                                                       ppv[:, :, 1 - half, :].rearrange("p (h b) f -> p h b f", h=4),
                                                       bc(snv[:, :, half, :].unsqueeze(1), [128, 4, 2, 32]), ALU.mult), reads=[pp, sin_t], writes=[t2])
            kb.op("dve", lambda e: e.tensor_tensor(tm[:], t1[:], t2[:], ALU.add), reads=[t1, t2], writes=[tm])
            ptr = ps[3 + gi]
            for h in range(4):
                kb.op("pe", lambda e: e.transpose(ptr[:].bitcast(BF16)[:, h * 128:(h + 1) * 128], tm[:, h * 128:(h + 1) * 128], identb[:]),
                      reads=[tm, identb], writes=[ptr])
            kb.op("act", lambda e: e.activation(dstT[:, n, :, :].rearrange("p h t -> p (h t)"), ptr[:].bitcast(BF16)[:, 0:512], AF.Copy), reads=[ptr], writes=[dstT])
        if na is not None:
            na.tile(n, hT, na_args["NQT"], na_args["NKT"], na_args["NV"], [ps[5], ps[6], ps[7]])
    gnw = kb.sb([128, 512], F32, "gnw")
    kb.dma(gnw[:], gn_w.partition_broadcast(128), writes=[gnw])
    ysb = kb.sb([128, 4, 128], F32, "ysb")
    ysq = kb.sb([128, 4, 128], F32, "ysq")
    st = kb.sb([128, 8], F32, "ystat")
    sg = kb.sb([128, 512], F32, "sg")
    mixb = kb.sb([128, 512], BF16, "mixb")
    mixT_sb = [kb.sb([128, 4, 128], BF16, "mixTsb%d" % i) for i in range(2)]
    pT = [kb.sb([128, 128], BF16, "pT%d" % i) for i in range(4)]
    cnt = 0
    for n in range(18):
        tsl = slice(n * 128, (n + 1) * 128)
        terms = [(n, None, None)]
        for m in range(18):
            if m == n:
                continue
            if RET_PF[m] < RET_PF[n]:
                terms.append((m, 0, RET_PF[n] - RET_PF[m]))
            if RET_PB[m] < RET_PB[n]:
                terms.append((m, 1, RET_PB[n] - RET_PB[m]))
        py = ps[5]
        items = [(h, ti, m, d, dp) for h in range(4) for ti, (m, d, dp) in enumerate(terms)]
        pas = [ps[1], ps[2], ps[6], ps[7]]

        def QK(i):
            h, ti, m, d, dp = items[i]
            pa = pas[i % 4]
            kb.op("pe", lambda e: e.matmul(pa[:, 0:128], kT[:, m, h, :], qT[:, n, h, :], start=True, stop=True), reads=[kT, qT], writes=[pa])

        def PV(i):
            h, ti, m, d, dp = items[i]
            pa = pas[i % 4]
            pt_ = pT[i % 4]
            if d is None:
                kb.op("dve", lambda e: e.tensor_tensor(pt_[:], pa[:, 0:128], Dsame[:, h, :], ALU.mult), reads=[pa, Dsame], writes=[pt_])
            else:
                kb.op("dve", lambda e: e.scalar_tensor_tensor(pt_[:], pa[:, 0:128], GP[:, d, h, dp:dp + 1], Dfull[:, d, h, :], ALU.mult, ALU.mult),
                      reads=[pa, GP, Dfull], writes=[pt_])
            kb.op("pe", lambda e: e.matmul(py[:, h * 128:(h + 1) * 128], pt_[:], v_all[:, m, h * 128:(h + 1) * 128], start=(ti == 0), stop=(ti == len(terms) - 1)),
                  reads=[pt_, v_all], writes=[py])
        LOOK = 2
        for i in range(min(LOOK, len(items))):
            QK(i)
        for i in range(len(items)):
            if i + LOOK < len(items):
                QK(i + LOOK)
            PV(i)
        pg = ps[0]
        for kc in range(8):
            kb.op("pe", lambda e: e.matmul(pg[:], hT[:, kc, tsl], wb[:, kc, 1536:2048], start=(kc == 0), stop=(kc == 7)), reads=[hT, wb], writes=[pg])
        kb.op("act", lambda e: e.activation(sg[:], pg[:], AF.Silu), reads=[pg], writes=[sg])
        kb.op("act", lambda e: e.activation(ysb[:].rearrange("p h e -> p (h e)"), py[:], AF.Copy), reads=[py], writes=[ysb])
        kb.op("dve", lambda e: e.tensor_reduce(st[:, 0:4], ysb[:], AX.X, ALU.add), reads=[ysb], writes=[st])
        kb.op("dve", lambda e: e.tensor_scalar(st[:, 0:4], st[:, 0:4], 1.0 / 128, None, ALU.mult), reads=[st], writes=[st])
        kb.op("dve", lambda e: e.tensor_tensor(ysb[:], ysb[:], bc(st[:, 0:4].unsqueeze(2), [128, 4, 128]), ALU.subtract), reads=[ysb, st], writes=[ysb])
        kb.op("dve", lambda e: e.tensor_tensor(ysq[:], ysb[:], ysb[:], ALU.mult), reads=[ysb], writes=[ysq])
        kb.op("dve", lambda e: e.tensor_reduce(st[:, 4:8], ysq[:], AX.X, ALU.add), reads=[ysq], writes=[st])
        kb.op("act", lambda e: e.activation(st[:, 4:8], st[:, 4:8], AF.Sqrt, bias=cx.gn_eps_col[:, 0:1], scale=1.0 / 128), reads=[st, cx.gn_eps_col], writes=[st])
        kb.op("dve", lambda e: e.reciprocal(st[:, 4:8], st[:, 4:8]), reads=[st], writes=[st])
        kb.op("dve", lambda e: e.tensor_tensor(ysb[:], ysb[:], bc(st[:, 4:8].unsqueeze(2), [128, 4, 128]), ALU.mult), reads=[ysb, st], writes=[ysb])
        kb.op("dve", lambda e: e.tensor_tensor(ysq[:].rearrange("p h e -> p (h e)"), ysb[:].rearrange("p h e -> p (h e)"), gnw[:], ALU.mult), reads=[ysb, gnw], writes=[ysq])
        kb.op("dve", lambda e: e.tensor_tensor(mixb[:], ysq[:].rearrange("p h e -> p (h e)"), sg[:], ALU.mult), reads=[ysq, sg], writes=[mixb])
        ptr = ps[3]
        for h in range(4):
            kb.op("pe", lambda e: e.transpose(ptr[:].bitcast(BF16)[:, h * 128:(h + 1) * 128], mixb[:, h * 128:(h + 1) * 128], identb[:]),
                  reads=[mixb, identb], writes=[ptr])
        mo = mixT_sb[n % 2]
        kb.op("act", lambda e: e.activation(mo[:].rearrange("p h t -> p (h t)"), ptr[:].bitcast(BF16)[:, 0:512], AF.Copy), reads=[ptr], writes=[mo])
        kb.dma(MIXT[0:512, tsl].rearrange("(c p) t -> p c t", p=128), mo[:], reads=[mo], writes=[MIXT])


class NaProj:
    def __init__(self, kb, cx, w_in, qn_w, kn_w):
        self.kb, self.cx = kb, cx
        self.w_in, self.qn_w, self.kn_w = w_in, qn_w, kn_w
        self.wb = kb.sb([128, 8, 1536], BF16, "w_na")
        self.k = 0

    def load_weights(self):
        kb = self.kb
        wv = self.w_in.rearrange("(c p) n -> p c n", p=128)
        for g in range(3):
            for cc in range(0, 8, 2):
                kb.load_cast(self.wb[:, cc:cc + 2, g * 512:(g + 1) * 512], wv[:, cc:cc + 2, 2048 + g * 512:2048 + (g + 1) * 512], self.wb, engs=("act", "pool", "dve"))

    def alloc(self):
        kb = self.kb
        nw = kb.sb([128, 2, 64], F32, "na_nw")
        kb.dma(nw[:, 0, :], self.qn_w.partition_broadcast(128), writes=[nw])
        kb.dma(nw[:, 1, :], self.kn_w.partition_broadcast(128), writes=[nw])
        kb.op("dve", lambda e: e.tensor_scalar(nw[:, 0, :], nw[:, 0, :], 0.125, None, ALU.mult), reads=[nw], writes=[nw])
        self.nw = nw
        self.sq = kb.sb([128, 8, 64], F32, "na_sq")
        self.st = kb.sb([128, 8], F32, "na_st")
        self.tmb = [kb.sb([128, 8, 64], BF16, "na_tm%d" % i) for i in range(2)]
        self.outT = [kb.sb([64, 8, 128], BF16, "na_oT%d" % i) for i in range(2)]
        self.vext = [kb.sb([64, 8, 65], BF16, "na_vx%d" % i) for i in range(2)]
        for i in range(2):
            kb.op("dve", lambda e: e.memset(self.vext[i][:], 1.0), writes=[self.vext[i]])

    def tile(self, n, hT, NQT, NKT, NV, banks):
        kb, cx, wb, nw, sq, st = self.kb, self.cx, self.wb, self.nw, self.sq, self.st
        identb = cx.ident_bf
        tsl = slice(n * 128, (n + 1) * 128)
        for gi, DST in ((0, NQT), (1, NKT)):
            pp = banks[gi]
            for kc in range(8):
                kb.op("pe", lambda e: e.matmul(pp[:], hT[:, kc, tsl], wb[:, kc, gi * 512:(gi + 1) * 512], start=(kc == 0), stop=(kc == 7)), reads=[hT, wb], writes=[pp])
            ppv = pp[:].rearrange("p (h d) -> p h d", h=8)
            kb.op("act", lambda e: e.activation(sq[:], ppv, AF.Square), reads=[pp], writes=[sq])
            kb.op("dve", lambda e: e.tensor_reduce(st[:], sq[:], AX.X, ALU.add), reads=[sq], writes=[st])
            kb.op("act", lambda e: e.activation(st[:], st[:], AF.Sqrt, bias=cx.eps_col[:, 0:1], scale=1.0 / 64), reads=[st, cx.eps_col], writes=[st])
            kb.op("dve", lambda e: e.reciprocal(st[:], st[:]), reads=[st], writes=[st])
            kb.op("dve", lambda e: e.tensor_tensor(sq[:], ppv, bc(st[:].unsqueeze(2), [128, 8, 64]), ALU.mult), reads=[pp, st], writes=[sq])
            tm = self.tmb[gi]
            kb.op("dve", lambda e: e.tensor_tensor(tm[:], sq[:], bc(nw[:, gi:gi + 1, :], [128, 8, 64]), ALU.mult), reads=[sq, nw], writes=[tm])
            ptr = banks[2]
            for h in range(8):
                kb.op("pe", lambda e: e.transpose(ptr[:].bitcast(BF16)[0:64, h * 128:(h + 1) * 128], tm[:, h, :], identb[:]), reads=[tm, identb], writes=[ptr])
            oT = self.outT[gi]
            kb.op("act", lambda e: e.activation(oT[:].rearrange("p h t -> p (h t)"), ptr[:].bitcast(BF16)[0:64, :], AF.Copy), reads=[ptr], writes=[oT])
            kb.dma(DST[:, :, tsl].rearrange("h d t -> d h t"), oT[:], reads=[oT], writes=[DST])
        for sub in range(2):
            pp = banks[sub]
            t0 = n * 128 + sub * 64
            for kc in range(8):
                kb.op("pe", lambda e: e.matmul(pp[0:64, :], hT[:, kc, t0:t0 + 64], wb[:, kc, 1024:1536], start=(kc == 0), stop=(kc == 7)), reads=[hT, wb], writes=[pp])
            vx = self.vext[self.k % 2]; self.k += 1
            kb.op("act", lambda e: e.activation(vx[:, :, 0:64], pp[0:64, :].rearrange("p (h d) -> p h d", h=8), AF.Copy), reads=[pp], writes=[vx])
            kb.dma(NV[2 * n + sub], vx[:].rearrange("p h d -> p (h d)"), reads=[vx], writes=[NV])


def phase_na_proj(kb, cx, hT, w_in, qn_w, kn_w, NQT, NKT, NV):
    na = NaProj(kb, cx, w_in, qn_w, kn_w)
    with kb.scope():
        kb.set_stage(8, 1024)
        na.load_weights()
    na.alloc()
    for n in range(18):
        na.tile(n, hT, NQT, NKT, NV, [cx.psum[0], cx.psum[1], cx.psum[2]])


def phase_na_attn(kb, cx, NQT, NKT, NV, consts, MIXT):
    ps = cx.psum
    identb = cx.ident_bf
    qT = kb.sb([64, 8, NTOK], BF16, "naqT")
    kT = kb.sb([64, 8, NTOK], BF16, "nakT")
    nv = kb.sb([64, 36, 520], BF16, "nanv")
    for h in range(8):
        kb.dma(qT[:, h, :], NQT[h], reads=[NQT], writes=[qT])
        kb.dma(kT[:, h, :], NKT[h], reads=[NKT], writes=[kT])
    for g in range(0, 36, 6):
        kb.dma(nv[:, g:g + 6, :], NV[g:g + 6].rearrange("n p f -> p n f"), reads=[NV], writes=[nv])
    E = kb.sb([64, 15, 512], F32, "naE")
    cm = kb.sb([64, 64], F32, "nacm")
    kb.dma(cm[:], consts["na_colmask"][:], writes=[cm])
    for ro in range(15):
        kb.dma(E[:, ro, :], consts["na_G"][ro], writes=[E])
    for ro in range(15):
        kb.op("act", lambda e: e.activation(E[:, ro, :], E[:, ro, :], AF.Exp), reads=[E], writes=[E])
        kb.op("dve", lambda e: e.tensor_tensor(E[:, ro, :].rearrange("p (h q) -> p h q", h=8), E[:, ro, :].rearrange("p (h q) -> p h q", h=8),
                                               bc(cm[:].unsqueeze(1), [64, 8, 64]), ALU.mult), reads=[E, cm], writes=[E])
    pf = [kb.sb([64, 512], F32, "napf%d" % i) for i in range(2)]
    pball = [kb.sb([64, 12, 512], BF16, "napb%d" % i) for i in range(2)]
    rc = kb.sb([64, 8], F32, "narc")
    ob = kb.sb([64, 8, 64], BF16, "naob")
    oT = [kb.sb([128, 4, 64], BF16, "naoT%d" % i) for i in range(2)]
    pss_l = [ps[2], ps[3], ps[4], ps[7]]
    cnt = [0]

    def keys_of(qt):
        if qt < 4:
            return [(kt, None) for kt in range(4)]
        r = qt - 4
        r0 = min(max(r - 4, 0), 24)
        return [(kt, None) for kt in range(4)] + [(4 + r0 + j_, r0 + j_ - r + 7) for j_ in range(8)]

    def S1(qt):
        q0 = qt * 64
        P_ = pball[qt % 2]
        for ki, (kt, ro) in enumerate(keys_of(qt)):
            k0 = kt * 64
            pss = pss_l[cnt[0] % 4]
            for h in range(8):
                kb.op("pe", lambda e: e.matmul(pss[0:64, h * 64:(h + 1) * 64], kT[:, h, k0:k0 + 64], qT[:, h, q0:q0 + 64], start=True, stop=True),
                      reads=[kT, qT], writes=[pss])
            if ro is None:
                kb.op("act", lambda e: e.activation(P_[:, ki, :], pss[0:64, :], AF.Exp), reads=[pss], writes=[P_])
            else:
                p_f = pf[cnt[0] % 2]
                kb.op("act", lambda e: e.activation(p_f[:], pss[0:64, :], AF.Exp), reads=[pss], writes=[p_f])
                kb.op("dve", lambda e: e.tensor_tensor(P_[:, ki, :], p_f[:], E[:, ro, :], ALU.mult), reads=[p_f, E], writes=[P_])
            cnt[0] += 1

    def S2(qt):
        q0 = qt * 64
        P_ = pball[qt % 2]
        keys = keys_of(qt)
        accA, accB = ps[0], ps[1]
        for h in range(8):
            acc = accA if h < 4 else accB
            hh = h % 4
            for ki, (kt, ro) in enumerate(keys):
                kb.op("pe", lambda e: e.matmul(acc[0:64, hh * 65:(hh + 1) * 65], P_[:, ki, h * 64:(h + 1) * 64], nv[:, kt, h * 65:(h + 1) * 65],
                                               start=(ki == 0), stop=(ki == len(keys) - 1)), reads=[P_, nv], writes=[acc])
        for half, acc in ((0, accA), (1, accB)):
            av = acc[0:64, 0:260].rearrange("p (h d) -> p h d", h=4)
            kb.op("dve", lambda e: e.reciprocal(rc[:, half * 4:(half + 1) * 4], av[:, :, 64]), reads=[acc], writes=[rc])
            kb.op("dve", lambda e: e.tensor_tensor(ob[:, half * 4:(half + 1) * 4, :], av[:, :, 0:64], bc(rc[:, half * 4:(half + 1) * 4].unsqueeze(2), [64, 4, 64]), ALU.mult),
                  reads=[acc, rc], writes=[ob])
        ptr = ps[5 + (qt % 2)]
        obv = ob[:].rearrange("p h d -> p (h d)")
        for c in range(4):
            kb.op("pe", lambda e: e.transpose(ptr[:].bitcast(BF16)[:, c * 64:(c + 1) * 64], obv[:, c * 128:(c + 1) * 128], identb[0:64, 0:64]), reads=[ob, identb], writes=[ptr])
        o = oT[qt % 2]
        kb.op("act", lambda e: e.activation(o[:].rearrange("p c t -> p (c t)"), ptr[:].bitcast(BF16)[:, 0:256], AF.Copy), reads=[ptr], writes=[o])
        kb.dma(MIXT[512:1024, q0:q0 + 64].rearrange("(c p) t -> p c t", p=128), o[:], reads=[o], writes=[MIXT])
    S1(0)
    for qt in range(36):
        if qt + 1 < 36:
            S1(qt + 1)
        S2(qt)


def phase_wout(kb, cx, L, w_out, MIXT, XT_in, XT_out, gate_idx=2, tiles=TILES):
    ps = cx.psum
    wb = kb.sb([128, 8, 1024], BF16, "w_out")
    wv = w_out.rearrange("(c p) n -> p c n", p=128)
    with kb.scope():
        kb.set_stage(8, 1024)
        for cc in range(8):
            kb.load_cast(wb[:, cc, :], wv[:, cc, :], wb, engs=("act", "pool", "dve"))
    mx = [kb.sb([128, 8, 512], BF16, "wo_mx%d" % i) for i in range(2)]
    xb = [kb.sb([128, 512], F32, "wo_x%d" % i) for i in range(3)]
    k = 0
    for ti, (t0, n, is_ctx) in enumerate(tiles):
        s = 1 if is_ctx else 0
        m = mx[ti % 2]
        kb.dma(m[:, :, 0:n], fm(MIXT[:], t0, n), reads=[MIXT], writes=[m])
        for dc in range(8):
            po = ps[dc]
            for kc in range(8):
                kb.op("pe", lambda e: e.matmul(po[:, 0:n], wb[:, kc, dc * 128:(dc + 1) * 128], m[:, kc, 0:n], start=(kc == 0), stop=(kc == 7)), reads=[wb, m], writes=[po])
            x = xb[k % 3]; k += 1
            kb.dma(x[:, 0:n], XT_in[dc * 128:(dc + 1) * 128, t0:t0 + n], reads=[XT_in], writes=[x])
            kb.op("dve", lambda e: e.scalar_tensor_tensor(x[:, 0:n], po[:, 0:n], mod_vec(cx, L, s, gate_idx)[:, dc:dc + 1], x[:, 0:n], ALU.mult, ALU.add),
                  reads=[po, cx.mod, x], writes=[x])
            kb.dma(XT_out[dc * 128:(dc + 1) * 128, t0:t0 + n], x[:, 0:n], reads=[x], writes=[XT_out])


def phase_gates(kb, cx, hT, hT_t0, router, gates_all):
    ps = cx.psum
    rb = kb.sb([128, 8, 8], BF16, "router_bf")
    kb.set_stage(1, 64)
    with kb.nc.allow_non_contiguous_dma(reason="tiny"):
        kb.load_cast(rb[:], router.rearrange("(c p) e -> p c e", p=128), rb)
    NT = 16
    lg = kb.sb([128, NT, 8], F32, "g_lg")
    l2 = kb.sb([128, NT, 8], F32, "g_l2")
    m1 = kb.sb([128, NT, 8], F32, "g_m1")
    m2 = kb.sb([128, NT, 8], F32, "g_m2")
    v1 = kb.sb([128, NT], F32, "g_v1")
    v2 = kb.sb([128, NT], F32, "g_v2")
    w1 = kb.sb([128, NT], F32, "g_w1")
    w2 = kb.sb([128, NT], F32, "g_w2")
    gt = kb.sb([128, NT, 8], F32, "g_gt")
    dg = [kb.sb([128, 8, 128], F32, "g_dg%d" % i) for i in range(2)]
    for n in range(NT):
        t0 = 256 + n * 128 - hT_t0
        pl = ps[n % 2]
        for kc in range(8):
            kb.op("pe", lambda e: e.matmul(pl[:, 0:8], hT[:, kc, t0:t0 + 128], rb[:, kc, :], start=(kc == 0), stop=(kc == 7)), reads=[hT, rb], writes=[pl])
        kb.op("act", lambda e: e.activation(lg[:, n, :], pl[:, 0:8], AF.Copy), reads=[pl], writes=[lg])

    def b3(x):
        return bc(x[:].unsqueeze(2), [128, NT, 8])
    kb.op("dve", lambda e: e.tensor_reduce(v1[:], lg[:], AX.X, ALU.max), reads=[lg], writes=[v1])
    kb.op("dve", lambda e: e.tensor_tensor(m1[:], lg[:], b3(v1), ALU.is_equal), reads=[lg, v1], writes=[m1])
    kb.op("dve", lambda e: e.scalar_tensor_tensor(l2[:], m1[:], -1e30, lg[:], ALU.mult, ALU.add), reads=[m1, lg], writes=[l2])
    kb.op("dve", lambda e: e.tensor_reduce(v2[:], l2[:], AX.X, ALU.max), reads=[l2], writes=[v2])
    kb.op("dve", lambda e: e.tensor_tensor(m2[:], l2[:], b3(v2), ALU.is_equal), reads=[l2, v2], writes=[m2])
    kb.op("dve", lambda e: e.tensor_tensor(w1[:], v2[:], v1[:], ALU.subtract), reads=[v1, v2], writes=[w1])
    kb.op("act", lambda e: e.activation(w1[:], w1[:], AF.Exp), reads=[w1], writes=[w1])
    kb.op("dve", lambda e: e.tensor_scalar(w1[:], w1[:], 1.0, None, ALU.add), reads=[w1], writes=[w1])
    kb.op("dve", lambda e: e.reciprocal(w1[:], w1[:]), reads=[w1], writes=[w1])
    kb.op("dve", lambda e: e.tensor_scalar(w2[:], w1[:], -1.0, 1.0, ALU.mult, ALU.add), reads=[w1], writes=[w2])
    kb.op("dve", lambda e: e.tensor_tensor(m1[:], m1[:], b3(w1), ALU.mult), reads=[m1, w1], writes=[m1])
    kb.op("dve", lambda e: e.tensor_tensor(m2[:], m2[:], b3(w2), ALU.mult), reads=[m2, w2], writes=[m2])
    kb.op("dve", lambda e: e.tensor_tensor(gt[:], m1[:], m2[:], ALU.add), reads=[m1, m2], writes=[gt])
    for n in range(NT):
        d = dg[n % 2]
        kb.op("dve", lambda e: e.tensor_tensor(d[:], bc(cx.ident_f32[:].unsqueeze(1), [128, 8, 128]), bc(gt[:, n, :].unsqueeze(2), [128, 8, 128]), ALU.mult),
              reads=[gt, cx.ident_f32], writes=[d])
        for half in range(2):
            pg = ps[2 + (2 * n + half) % 4]
            kb.op("pe", lambda e: e.matmul(pg[:], cx.ones_f32[:], d[:, half * 4:(half + 1) * 4, :], start=True, stop=True), reads=[d, cx.ones_f32], writes=[pg])
            kb.op("act", lambda e: e.activation(gates_all[:, half * 4:(half + 1) * 4, n * 128:(n + 1) * 128], pg[:].rearrange("p (e t) -> p e t", e=4), AF.Copy),
                  reads=[pg], writes=[gates_all])


W_NAMES = ["ada_w", "ada_b", "norm_mix_w", "norm_ffn_w", "ev_w_in", "ev_ret_decay_f", "ev_ret_decay_b", "ev_ret_gn_w", "ev_na_qn_w",
           "ev_na_kn_w", "ev_w_out", "ev_ffn_w13", "ev_ffn_w2", "od_router", "od_moe_w13", "od_moe_w2"]


def build_program(shapes, const_shapes):
    kb = KB(); cx = Ctx()

    def ein(name, shape):
        return kb.dram(name, list(shape), F32, "ExternalInput")
    c_lat = ein("c_lat", [1024]); c_ctx = ein("c_ctx", [1024])
    w = {k: ein(k, shapes[k]) for k in W_NAMES + RW_NAMES}
    consts = {k: ein(k, v) for k, v in const_shapes.items()}
    XT0 = ein("xT", [1024, NTOK])
    outT = kb.dram("outT", [1024, NLAT], F32, "ExternalOutput")
    XT1 = kb.dram("XT1", [1024, NTOK], F32)
    MIXT = kb.dram("MIXT", [1024, NTOK], BF16)
    NQT = kb.dram("NQT", [8, 64, NTOK], BF16); NKT = kb.dram("NKT", [8, 64, NTOK], BF16); NV = kb.dram("NV", [36, 64, 520], BF16)
    setup_common(kb, cx, consts)
    phase_mod_scoped(kb, cx, c_lat, c_ctx, w["ada_w"], w["ada_b"])
    with kb.scope():
        hT = kb.sb([128, 8, NTOK], BF16, "hT")
        with kb.scope():
            setup_modbuf(kb, cx)
            phase_modulate(kb, cx, XT0, 0, w["norm_mix_w"][0], 0, 1, hT)
        with kb.scope():
            phase_retention(kb, cx, hT, w["ev_w_in"][0], w["ev_ret_decay_f"][0], w["ev_ret_decay_b"][0], w["ev_ret_gn_w"][0], consts, MIXT,
                            na_args=dict(qn_w=w["ev_na_qn_w"][0], kn_w=w["ev_na_kn_w"][0], NQT=NQT, NKT=NKT, NV=NV))
    with kb.scope():
        phase_na_attn(kb, cx, NQT, NKT, NV, consts, MIXT)
    with kb.scope():
        phase_wout(kb, cx, 0, w["ev_w_out"][0], MIXT, XT0, XT1)
    with kb.scope():
        hT = kb.sb([128, 8, NTOK], BF16, "hT")
        with kb.scope():
            setup_modbuf(kb, cx)
            phase_modulate(kb, cx, XT1, 0, w["norm_ffn_w"][0], 3, 4, hT)
        with kb.scope():
            setup_ffn(kb, cx, NTOK)
            phase_ffn(kb, cx, XT1, 0, hT, 0, TILES, [w["ev_ffn_w13"][0]], [w["ev_ffn_w2"][0]], 2816, 5)
    XT2 = kb.dram("XT2", [1024, NTOK], F32)
    layer1_mixer(kb, cx, w, consts, XT1, XT2, MIXT)
    LT = TILES[1:]
    with kb.scope():
        hT = kb.sb([128, 8, NLAT], BF16, "hT")
        with kb.scope():
            setup_modbuf(kb, cx)
            phase_modulate(kb, cx, XT2, 1, w["norm_ffn_w"][1], 3, 4, hT, tiles=LT, hT_t0=256)
        gates_all = kb.sb([128, 8, NLAT], BF16, "gates_all")
        with kb.scope():
            phase_gates(kb, cx, hT, 256, w["od_router"][0], gates_all)
        gl = [T(gates_all.t[:, :, i * 512:(i + 1) * 512]) for i in range(4)]
        for g_ in gl:
            g_.ws = gates_all.ws; g_.rs = gates_all.rs
        with kb.scope():
            setup_ffn(kb, cx, NLAT)
            phase_ffn(kb, cx, XT2, 1, hT, 256, LT, [w["od_moe_w13"][0][e] for e in range(8)], [w["od_moe_w2"][0][e] for e in range(8)], 3584, 5, gates=gl)
    kb.dma(outT[:], XT2[:, 256:NTOK], reads=[XT2], writes=[outT])
    kb.finish()
    return kb


def host_consts():
    c = {}
    c["ident"] = np.eye(128, dtype=np.float32)
    half = 32
    freqs = (10000.0 ** (-np.arange(half, dtype=np.float32) / half)).astype(np.float32)
    t = np.arange(2048)
    rpos = (t // 64).astype(np.float32); cpos = (t % 64).astype(np.float32)
    cos = np.ones((2304, 128), np.float32); sin = np.zeros((2304, 128), np.float32)
    for blk, pos in ((0, rpos), (1, cpos)):
        ang = pos[:, None] * freqs[None, :]
        cs, sn = np.cos(ang).astype(np.float32), np.sin(ang).astype(np.float32)
        cos[256:, blk * 64:blk * 64 + 32] = cs; cos[256:, blk * 64 + 32:blk * 64 + 64] = cs
        sin[256:, blk * 64:blk * 64 + 32] = -sn; sin[256:, blk * 64 + 32:blk * 64 + 64] = sn
    c["rope_cos"] = cos; c["rope_sin"] = sin
    s = np.arange(128, dtype=np.float32)[:, None]; tt = np.arange(128, dtype=np.float32)[None, :]
    d = tt - s
    c["ret_cst"] = np.stack([d, np.maximum(d, 0), np.maximum(-d, 0), (d >= 0).astype(np.float32), (d <= 0).astype(np.float32)]).astype(np.float32)
    c["kidx128"] = (128.0 * np.arange(18)).astype(np.float32)
    return c


def na_tables(rpb):
    kc = np.arange(64)[:, None]; q = np.arange(64)[None, :]
    idx = np.clip(kc - q + 15, 0, 30)
    G = rpb[:, :, idx]
    G = np.ascontiguousarray(np.transpose(G, (1, 2, 0, 3))).reshape(15, 64, 512).astype(np.float32)
    cs = np.clip(np.arange(64) - 8, 0, 48)
    cm = ((kc >= cs[None, :]) & (kc < cs[None, :] + 16)).astype(np.float32)
    return G, cm


_PROG_CACHE = {}


def kernel(**inputs):
    inp = {k: np.ascontiguousarray(np.asarray(v, dtype=np.float32)) for k, v in inputs.items()}
    hc = host_consts()
    G, cm = na_tables(inp["ev_na_rpb"][0])
    hc["na_G"] = G
    hc["na_colmask"] = cm
    hc.update(rwkv_consts())
    shapes = {k: inp[k].shape for k in W_NAMES + RW_NAMES}
    cshapes = {k: v.shape for k, v in hc.items()}
    key = "prog"
    kb = build_program(shapes, cshapes)
    n = 8
    in_maps = []
    for b in range(n):
        m = {"c_lat": inp["c"][b], "c_ctx": inp["c_ctx"],
             "xT": np.ascontiguousarray(np.concatenate([inp["ctx"][b], inp["x"][b]], 0).T)}
        for k in W_NAMES + RW_NAMES:
            m[k] = inp[k]
        m.update(hc)
        in_maps.append(m)
    res = run_bass_kernel_spmd(kb.nc, in_maps, core_ids=list(range(n)))
    out = np.stack([np.ascontiguousarray(res.results[b]["outT"].T) for b in range(n)], 0)
    return out.astype(np.float32)


DEC_C = 0.6065306597126334
NCH = NTOK // 64


class BankRR:
    def __init__(self, banks):
        self.b = banks
        self.i = 0

    def __call__(self):
        p = self.b[self.i % len(self.b)]
        self.i += 1
        return p


def rwkv_weights_alloc(kb):
    Wd = {}
    Wd["W3"] = kb.sb([128, 3, 8, 1024], BF16, "rw_W3")
    Wd["w1b"] = kb.sb([128, 2, 8, 64], BF16, "rw_w1")
    Wd["a1b"] = kb.sb([128, 2, 8, 64], BF16, "rw_a1")
    Wd["w2b"] = kb.sb([64, 2, 1024], BF16, "rw_w2")
    Wd["a2b"] = kb.sb([64, 2, 1024], BF16, "rw_a2")
    Wd["g1b"] = kb.sb([128, 8, 160], BF16, "rw_g1")
    Wd["g2b"] = kb.sb([128, 1024], BF16, "rw_g2")
    Wd["g2c"] = kb.sb([32, 1024], BF16, "rw_g2c")
    return Wd


def rwkv_weights_load(kb, Wd, w):
    W3, w1b, a1b, w2b, a2b, g1b, g2b, g2c = (Wd[k_] for k_ in ("W3", "w1b", "a1b", "w2b", "a2b", "g1b", "g2b", "g2c"))
    for s in range(3):
        wv = w["od_w_rkv"][0][s].rearrange("(c p) n -> p c n", p=128)
        for cc in range(8):
            kb.load_cast(W3[:, s, cc, :], wv[:, cc, :], W3, engs=("act", "pool", "dve"))
            if cc % 2 == 1:
                yield
    for z in range(2):
        kb.load_cast(w1b[:, z, :, :], w["od_w1"][0][z].rearrange("(c p) r -> p c r", p=128), w1b)
        kb.load_cast(a1b[:, z, :, :], w["od_a1"][0][z].rearrange("(c p) r -> p c r", p=128), a1b)
        kb.load_cast(w2b[:, z, :], w["od_w2"][0][z], w2b)
        kb.load_cast(a2b[:, z, :], w["od_a2"][0][z], a2b)
        yield
    for hf in range(2):
        kb.load_cast(g1b[:, hf * 4:(hf + 1) * 4, :], w["od_g1"][0].rearrange("(c p) r -> p c r", p=128)[:, hf * 4:(hf + 1) * 4, :], g1b)
    kb.load_cast(g2b[:], w["od_g2"][0][0:128, :], g2b)
    kb.load_cast(g2c[:], w["od_g2"][0][128:160, :], g2c)
    yield


def phase_rwkv_feat(kb, cx, HT, w, consts, D_, Wd):
    nb = BankRR(cx.psum)
    W3, w1b, a1b, w2b, a2b, g1b, g2b, g2c = (Wd[k_] for k_ in ("W3", "w1b", "a1b", "w2b", "a2b", "g1b", "g2b", "g2c"))
    mu = kb.sb([128, 6, 8], F32, "rw_mu")
    with kb.nc.allow_non_contiguous_dma(reason="tiny"):
        for s in range(6):
            kb.dma(mu[:, s, :], w["od_mu"][0][s].rearrange("(c p) -> p c", p=128), writes=[mu])
    tabs = kb.sb([128, 7, 1024], F32, "rw_tabs")
    srcs = [w["od_w0"][0][0], w["od_w0"][0][1], w["od_a0"][0][0], w["od_a0"][0][1], w["od_k_k"][0], w["od_k_a"][0], w["od_r_k"][0]]
    for i, s_ in enumerate(srcs):
        kb.dma(tabs[:, i, :], s_.partition_broadcast(128), writes=[tabs])
    CM = kb.sb([128, 5, 128], F32, "rw_CM")
    kb.dma(CM[:], consts["rw_CM"][:].rearrange("k s t -> s k t"), writes=[CM])
    tiny = kb.sb([128, 1], F32, "rw_tiny")
    kb.op("dve", lambda e: e.memset(tiny[:], 1e-24), writes=[tiny])
    xx = kb.sb([128, 8, 128], F32, "rw_xx")
    XM = [[kb.sb([128, 8, 128], BF16, "rw_xm%d_%d" % (i, s)) for s in range(6)] for i in range(2)]
    HID = [(kb.sb([64, 2, 128], BF16, "rw_hw%d" % i), kb.sb([64, 2, 128], BF16, "rw_ha%d" % i),
            kb.sb([128, 128], BF16, "rw_hg%d" % i), kb.sb([32, 128], BF16, "rw_hg2%d" % i)) for i in range(2)]

    FW = 512
    NHU = FW // 64

    def mkws(tag):
        d = {}
        for nm in ("r_sb", "k_sb", "v_sb", "kk", "kkn", "tmp", "tmp2", "tmp3", "sig0", "sig1", "asg0", "asg1", "kd0", "kd1", "bz"):
            d[nm] = kb.sb([128, FW], F32, "rw_%s_%s" % (nm, tag))
        d["st8"] = kb.sb([128, NHU], F32, "rw_st_%s" % tag)
        return d
    WSS = [mkws("a")]
    OBL = [kb.sb([128, 9, FW], BF16, "rw_OB%d" % i) for i in range(2)]
    OFL = [kb.sb([128, 4, FW], F32, "rw_OF%d" % i) for i in range(2)]
    ucnt = [0]

    class _V:
        def __init__(self, tile, slot):
            self.tile, self.slot = tile, slot

        def __getitem__(self, k):
            return self.tile.t[:, self.slot, :][k]

    hloc_l = [kb.sb([128, 8, 130], BF16, "rw_hloc%d" % i) for i in range(2)]

    def prologue(n):
        xm = XM[n % 2]
        hw_b, ha_b, hg_b, hg2_b = HID[n % 2]
        t0 = n * 128
        tsl = slice(t0, t0 + 128)
        left_b = (t0 == 0 or t0 == 256)
        right_b = (t0 + 128 == 256 or t0 + 128 == NTOK)
        lo = 1 if left_b else 0
        hi = 127 if right_b else 128
        hloc = hloc_l[n % 2]
        g0 = t0 if left_b else t0 - 1
        g1 = t0 + 128 if right_b else t0 + 129
        kb.dma(hloc[:, :, g0 - (t0 - 1):g1 - (t0 - 1)], fm(HT[:], g0, g1 - g0), reads=[HT], writes=[hloc])
        kb.op("dve", lambda e: e.tensor_tensor(xx[:, :, lo:hi], hloc[:, :, lo:hi], hloc[:, :, lo + 2:hi + 2], ALU.add), reads=[hloc], writes=[xx])
        if left_b:
            kb.op("dve", lambda e: e.tensor_copy(xx[:, :, 0:1], hloc[:, :, 2:3]), reads=[hloc], writes=[xx])
        if right_b:
            kb.op("dve", lambda e: e.tensor_copy(xx[:, :, 127:128], hloc[:, :, 127:128]), reads=[hloc], writes=[xx])
        kb.op("dve", lambda e: e.tensor_scalar(xx[:], xx[:], 0.5, None, ALU.mult), reads=[xx], writes=[xx])
        kb.op("dve", lambda e: e.tensor_tensor(xx[:], xx[:], hloc[:, :, 1:129], ALU.subtract), reads=[xx, hloc], writes=[xx])
        for s in range(6):
            eng = "dve" if s % 2 == 0 else "pool"
            kb.op(eng, lambda e: e.tensor_tensor(xm[s][:], xx[:], bc(mu[:, s, :].unsqueeze(2), [128, 8, 128]), ALU.mult), reads=[xx, mu], writes=[xm[s]])
            kb.op(eng, lambda e: e.tensor_tensor(xm[s][:], xm[s][:], hloc[:, :, 1:129], ALU.add), reads=[xm[s], hloc], writes=[xm[s]])
            yield
        ph = nb()
        for z in range(2):
            for kc in range(8):
                kb.op("pe", lambda e: e.matmul(ph[0:64, z * 128:(z + 1) * 128], w1b[:, z, kc, :], xm[3][:, kc, :], start=(kc == 0), stop=(kc == 7)), reads=[w1b, xm[3]], writes=[ph])
        kb.op("act", lambda e: e.activation(hw_b[:].rearrange("p z t -> p (z t)"), ph[0:64, 0:256], AF.Tanh), reads=[ph], writes=[hw_b])
        yield
        ph = nb()
        for z in range(2):
            for kc in range(8):
                kb.op("pe", lambda e: e.matmul(ph[0:64, z * 128:(z + 1) * 128], a1b[:, z, kc, :], xm[4][:, kc, :], start=(kc == 0), stop=(kc == 7)), reads=[a1b, xm[4]], writes=[ph])
        kb.op("act", lambda e: e.activation(ha_b[:].rearrange("p z t -> p (z t)"), ph[0:64, 0:256], AF.Copy), reads=[ph], writes=[ha_b])
        yield
        ph = nb()
        for kc in range(8):
            kb.op("pe", lambda e: e.matmul(ph[:, 0:128], g1b[:, kc, 0:128], xm[5][:, kc, :], start=(kc == 0), stop=(kc == 7)), reads=[g1b, xm[5]], writes=[ph])
        kb.op("act", lambda e: e.activation(hg_b[:], ph[:, 0:128], AF.Sigmoid), reads=[ph], writes=[hg_b])
        yield
        ph = nb()
        for kc in range(8):
            kb.op("pe", lambda e: e.matmul(ph[0:32, 0:128], g1b[:, kc, 128:160], xm[5][:, kc, :], start=(kc == 0), stop=(kc == 7)), reads=[g1b, xm[5]], writes=[ph])
        kb.op("act", lambda e: e.activation(hg2_b[:], ph[0:32, 0:128], AF.Sigmoid), reads=[ph], writes=[hg2_b])
        yield

    def units(n):
        t0 = n * 128
        tsl = slice(t0, t0 + 128)
        xm = XM[n % 2]
        hw_b, ha_b, hg_b, hg2_b = HID[n % 2]
        def unit(q, WS):
            r_sb, k_sb, v_sb, kk, kkn, tmp, tmp2, tmp3, bz, st8 = (WS[k_] for k_ in ('r_sb', 'k_sb', 'v_sb', 'kk', 'kkn', 'tmp', 'tmp2', 'tmp3', 'bz', 'st8'))
            sig = [WS['sig0'], WS['sig1']]; asg = [WS['asg0'], WS['asg1']]; kd = [WS['kd0'], WS['kd1']]
            fsl = slice(q * FW, (q + 1) * FW)
            OB = OBL[ucnt[0] % 2]
            OF = OFL[ucnt[0] % 2]
            ucnt[0] += 1
            for s, dst in ((0, r_sb), (1, k_sb), (2, v_sb)):
                pp = nb()
                for kc in range(8):
                    kb.op("pe", lambda e: e.matmul(pp[:], xm[s][:, kc, :], W3[:, s, kc, fsl], start=(kc == 0), stop=(kc == 7)), reads=[xm[s], W3], writes=[pp])
                kb.op("act", lambda e: e.activation(dst[:], pp[:], AF.Copy), reads=[pp], writes=[dst])
            o = _V(OB, 0)
            kb.op("pool", lambda e: e.tensor_copy(o[:], v_sb[:]), reads=[v_sb], writes=[OB])
            yield
            for z in range(2):
                pp = nb()
                kb.op("pe", lambda e: e.matmul(pp[:], hw_b[:, z, :], w2b[:, z, fsl], start=True, stop=True), reads=[hw_b, w2b], writes=[pp])
                kb.op("dve", lambda e: e.tensor_tensor(sig[z][:], pp[:], tabs[:, z, fsl], ALU.add), reads=[pp, tabs], writes=[sig[z]])
                kb.op("act", lambda e: e.activation(sig[z][:], sig[z][:], AF.Sigmoid), reads=[sig[z]], writes=[sig[z]])
                pp = nb()
                kb.op("pe", lambda e: e.matmul(pp[:], ha_b[:, z, :], a2b[:, z, fsl], start=True, stop=True), reads=[ha_b, a2b], writes=[pp])
                kb.op("dve", lambda e: e.tensor_tensor(asg[z][:], pp[:], tabs[:, 2 + z, fsl], ALU.add), reads=[pp, tabs], writes=[asg[z]])
                kb.op("act", lambda e: e.activation(asg[z][:], asg[z][:], AF.Sigmoid), reads=[asg[z]], writes=[asg[z]])
                yield
            kb.op("dve", lambda e: e.tensor_tensor(kk[:], k_sb[:], tabs[:, 4, fsl], ALU.mult), reads=[k_sb, tabs], writes=[kk])
            kb.op("act", lambda e: e.activation(tmp[:], kk[:], AF.Square), reads=[kk], writes=[tmp])
            kb.op("dve", lambda e: e.tensor_reduce(st8[:], tmp[:].rearrange("p (h j) -> p h j", h=8), AX.X, ALU.add), reads=[tmp], writes=[st8])
            kb.op("act", lambda e: e.activation(st8[:], st8[:], AF.Sqrt, bias=tiny[:, 0:1], scale=1.0), reads=[st8, tiny], writes=[st8])
            kb.op("dve", lambda e: e.reciprocal(st8[:], st8[:]), reads=[st8], writes=[st8])
            kb.op("dve", lambda e: e.tensor_tensor(kkn[:].rearrange("p (h j) -> p h j", h=8), kk[:].rearrange("p (h j) -> p h j", h=8),
                                                   bc(st8[:].unsqueeze(2), [128, 8, 64]), ALU.mult), reads=[kk, st8], writes=[kkn])
            yield
            for z in range(2):
                kb.op("dve", lambda e: e.scalar_tensor_tensor(tmp[:], asg[z][:], -1.0, tabs[:, 5, fsl], ALU.add, ALU.mult), reads=[asg[z], tabs], writes=[tmp])
                kb.op("dve", lambda e: e.scalar_tensor_tensor(kd[z][:], tmp[:], 1.0, k_sb[:], ALU.add, ALU.mult), reads=[tmp, k_sb], writes=[kd[z]])
                kb.op("pool", lambda e: e.tensor_tensor(bz[:], kkn[:], asg[z][:], ALU.mult), reads=[kkn, asg[z]], writes=[bz])
                pci = nb()
                kb.op("pe", lambda e: e.matmul(pci[:], CM[:, 2 * z, :], sig[z][:], start=True, stop=True), reads=[CM, sig[z]], writes=[pci])
                kb.op("act", lambda e: e.activation(tmp2[:], pci[:], AF.Exp, scale=-DEC_C), reads=[pci], writes=[tmp2])
                o = _V(OB, 1 + z)
                kb.op("dve", lambda e: e.tensor_tensor(o[:], r_sb[:], tmp2[:], ALU.mult), reads=[r_sb, tmp2], writes=[OB])
                yield
                kb.op("act", lambda e: e.activation(tmp3[:], pci[:], AF.Exp, scale=DEC_C), reads=[pci], writes=[tmp3])
                o = _V(OB, 3 + z)
                kb.op("dve", lambda e: e.tensor_tensor(o[:], bz[:], tmp3[:], ALU.mult), reads=[bz, tmp3], writes=[OB])
                o = _V(OB, 5 + z)
                kb.op("pool", lambda e: e.tensor_tensor(o[:], kd[z][:], tmp3[:], ALU.mult), reads=[kd[z], tmp3], writes=[OB])
                yield
                pce = nb()
                kb.op("pe", lambda e: e.matmul(pce[:], CM[:, 2 * z + 1, :], sig[z][:], start=True, stop=True), reads=[CM, sig[z]], writes=[pce])
                kb.op("act", lambda e: e.activation(tmp2[:], pce[:], AF.Exp, scale=-DEC_C), reads=[pce], writes=[tmp2])
                o = _V(OB, 7 + z)
                kb.op("dve", lambda e: e.scalar_tensor_tensor(o[:], kkn[:], -1.0, tmp2[:], ALU.mult, ALU.mult), reads=[kkn, tmp2], writes=[OB])
                ptot = nb()
                kb.op("pe", lambda e: e.matmul(ptot[:], CM[:, 4, :], sig[z][:], start=True, stop=True), reads=[CM, sig[z]], writes=[ptot])
                o2 = _V(OF, z)
                kb.op("act", lambda e: e.activation(o2[:], ptot[:], AF.Exp, scale=-DEC_C), reads=[ptot], writes=[OF])
                yield
            kb.op("dve", lambda e: e.tensor_tensor(tmp[:], r_sb[:], tabs[:, 6, fsl], ALU.mult), reads=[r_sb, tabs], writes=[tmp])
            kb.op("pool", lambda e: e.tensor_tensor(tmp2[:], kd[0][:], kd[1][:], ALU.add), reads=[kd[0], kd[1]], writes=[tmp2])
            kb.op("dve", lambda e: e.tensor_tensor(tmp[:], tmp[:], tmp2[:], ALU.mult), reads=[tmp, tmp2], writes=[tmp])
            kb.op("dve", lambda e: e.tensor_reduce(st8[:], tmp[:].rearrange("p (h j) -> p h j", h=8), AX.X, ALU.add), reads=[tmp], writes=[st8])
            o2 = _V(OF, 2)
            kb.op("dve", lambda e: e.tensor_tensor(o2[:].rearrange("p (h j) -> p h j", h=8), v_sb[:].rearrange("p (h j) -> p h j", h=8),
                                                   bc(st8[:].unsqueeze(2), [128, 8, 64]), ALU.mult), reads=[v_sb, st8], writes=[OF])
            yield
            pg = nb()
            kb.op("pe", lambda e: e.matmul(pg[:], hg_b[:], g2b[:, fsl], start=True, stop=False), reads=[hg_b, g2b], writes=[pg])
            kb.op("pe", lambda e: e.matmul(pg[:], hg2_b[:], g2c[:, fsl], start=False, stop=True), reads=[hg2_b, g2c], writes=[pg])
            o2 = _V(OF, 3)
            kb.op("act", lambda e: e.activation(o2[:], pg[:], AF.Copy), reads=[pg], writes=[OF])
            kb.dma(D_["BIG"][tsl, :, fsl], OB[:], reads=[OB], writes=[D_["BIG"]])
            kb.dma(D_["BIGF"][tsl, :, fsl], OF[:], reads=[OF], writes=[D_["BIGF"]])


        for q_ in range(2):
            yield from unit(q_, WSS[0])

    run_gens([prologue(0)])
    for n in range(18):
        gens = [units(n)]
        if n + 1 < 18:
            gens.append(prologue(n + 1))
        run_gens(gens)


class RwkvScan:
    def __init__(self, kb, cx, z, D_, consts):
        self.kb, self.cx, self.z, self.D_ = kb, cx, z, D_
        self.nd = cx.nd
        MK = kb.sb([128, 4, 64], F32, "sc_MK")
        kb.dma(MK[:], consts["rw_MK2"][:].rearrange("k r c -> r k c"), writes=[MK])
        self.MK = MK
        self.I2 = kb.sb([128, 64], F32, "sc_I2")
        kb.dma(self.I2[:], consts["rw_I2"][:], writes=[self.I2])
        Ef = kb.sb([64, 2, 128], F32, "sc_Ef")
        kb.dma(Ef[:], consts["rw_E"][:].rearrange("k r c -> r k c"), writes=[Ef])
        self.E = kb.sb([64, 2, 128], BF16, "sc_E")
        kb.op("dve", lambda e: e.tensor_copy(self.E[:], Ef[:]), reads=[Ef], writes=[self.E])
        if z == 0:
            self.mT_strict, self.mT_incl, self.mL_strict = 0, 1, 2
        else:
            self.mT_strict, self.mT_incl, self.mL_strict = 2, 3, 0

        def b16(name):
            return kb.sb([128, 8, 64], BF16, name)
        self.U0f = kb.sb([128, 8, 64], F32, "sc_U0f")
        self.U0b = b16("sc_U0b")
        kb.op("dve", lambda e: e.memset(self.U0f[:], 0.0), writes=[self.U0f])
        kb.op("dve", lambda e: e.memset(self.U0b[:], 0.0), writes=[self.U0b])
        self.tin = [[kb.sb([64, 1024], BF16, "sc_in%d_%d" % (i, j)) for j in range(5)] for i in range(2)]
        self.etin = [kb.sb([64, 1024], F32, "sc_et%d" % i) for i in range(2)]
        self.yout = kb.sb([128, 8, 64], F32, "sc_yo")
        self.sets = []
        for i in range(2):
            d = {}
            for nm in ("aT", "rT", "LakT", "MrbT", "MrkT", "QTb", "Bst", "Kst", "Vst"):
                d[nm] = b16("sc_%s%d" % (nm, i))
            d["etT"] = kb.sb([128, 8, 64], F32, "sc_etT%d" % i)
            self.sets.append(d)
        self.bT, self.kT = b16("sc_bT"), b16("sc_kT")
        self.Lp = [b16("sc_L%d" % i) for i in range(2)]
        self.LTp = [b16("sc_LT%d" % i) for i in range(2)]
        self.LTf = kb.sb([128, 8, 64], F32, "sc_LTf")
        self.QTf = kb.sb([128, 8, 64], F32, "sc_QTf")
        self.Xb, self.Pb = b16("sc_Xb"), b16("sc_Pb")
        self.order = (list(range(4)) + list(range(4, NCH))) if z == 0 else ([3, 2, 1, 0] + list(range(NCH - 1, 3, -1)))

    def mm_heads(self, pd, specs, reads):
        kb = self.kb
        for p in range(8):
            for h2 in range(2):
                ps_ = slice(h2 * 64, (h2 + 1) * 64)
                for k, (lt, rt) in enumerate(specs):
                    kb.op("pe", lambda e: e.matmul(pd[ps_, p * 64:(p + 1) * 64], lt[ps_, p, :], rt[ps_, p, :], start=(k == 0), stop=(k == len(specs) - 1)),
                          reads=reads, writes=[pd])

    def prep(self, ci):
        kb, cx, z, D_, nd, MK = self.kb, self.cx, self.z, self.D_, self.nd, self.MK
        identb, identf = cx.ident_bf, cx.ident_f32
        c = self.order[ci]
        c0 = c * 64
        ti = self.tin[ci % 2]
        S = self.sets[ci % 2]
        aT, rT, etT, LakT, MrbT, MrkT, QTb = S["aT"], S["rT"], S["etT"], S["LakT"], S["MrbT"], S["MrkT"], S["QTb"]
        bT, kT, LTf, QTf = self.bT, self.kT, self.LTf, self.QTf
        names = ["AT", "RT", "BT", "KT"]
        for j, nm in enumerate(names):
            kb.dma(ti[j][:], D_[nm][z][c0:c0 + 64, :], reads=[D_[nm][z]], writes=[ti[j]])
        kb.dma(ti[4][:], D_["V"][c0:c0 + 64, :], reads=[D_["V"]], writes=[ti[4]])
        et = self.etin[ci % 2]
        kb.dma(et[:], D_["ETOT"][z][c0:c0 + 64, :], reads=[D_["ETOT"][z]], writes=[et])
        A_, R_, B_, K_, V_ = ti
        for src, dst in ((A_, aT), (R_, rT), (B_, bT), (K_, kT)):
            pd = nd()
            pdb = pd[:].bitcast(BF16)
            for p in range(8):
                kb.op("pe", lambda e: e.transpose(pdb[:, p * 64:(p + 1) * 64], src[:, p * 128:(p + 1) * 128], identb[0:64, 0:64]), reads=[src, identb], writes=[pd])
            kb.op("act", lambda e: e.activation(dst[:].rearrange("p h t -> p (h t)"), pdb[:, 0:512], AF.Copy), reads=[pd], writes=[dst])
            yield
        pd = nd()
        for p in range(8):
            kb.op("pe", lambda e: e.transpose(pd[:, p * 64:(p + 1) * 64], et[:, p * 128:(p + 1) * 128], identf[0:64, 0:64]), reads=[et, identf], writes=[pd])
        kb.op("act", lambda e: e.activation(etT[:].rearrange("p h t -> p (h t)"), pd[:], AF.Copy), reads=[pd], writes=[etT])
        yield
        for src, nm in ((B_, "Bst"), (K_, "Kst"), (V_, "Vst")):
            dst = S[nm]
            pd = nd()
            sv = src[:].rearrange("s (p h2 i) -> s p h2 i", h2=2, i=64)
            for h2 in range(2):
                kb.op("pe", lambda e: e.matmul(pd[:], self.E[:, h2, :], sv[:, :, h2, :], start=(h2 == 0), stop=(h2 == 1)), reads=[src, self.E], writes=[pd])
            kb.op("dve", lambda e: e.tensor_copy(dst[:].rearrange("p h t -> p (h t)"), pd[:]), reads=[pd], writes=[dst])
            yield

        def v3(p):
            return p[:].rearrange("p (h t) -> p h t", h=8)

        def pair(lhsT_t, rhs_t, mask_i, dst, eng="dve"):
            pd = nd()
            self.mm_heads(pd, [(lhsT_t, rhs_t)], [lhsT_t, rhs_t])
            kb.op(eng, lambda e: e.tensor_tensor(dst[:], v3(pd), bc(MK[:, mask_i, :].unsqueeze(1), [128, 8, 64]), ALU.mult), reads=[pd, MK], writes=[dst])
        pair(bT, aT, self.mT_strict, LTf)
        yield
        L1, L1T = self.Lp[0], self.LTp[0]
        pair(aT, bT, self.mL_strict, L1)
        yield
        pair(kT, aT, self.mT_strict, LakT)
        yield
        pair(bT, rT, self.mT_incl, MrbT)
        yield
        pair(kT, rT, self.mT_incl, MrkT)
        kb.op("act", lambda e: e.activation(L1T[:], LTf[:], AF.Copy), reads=[LTf], writes=[L1T])
        kb.op("dve", lambda e: e.tensor_tensor(QTf[:], LTf[:], bc(self.I2[:].unsqueeze(1), [128, 8, 64]), ALU.add), reads=[LTf, self.I2], writes=[QTf])
        kb.op("act", lambda e: e.activation(QTb[:], QTf[:], AF.Copy), reads=[QTf], writes=[QTb])
        yield
        for lvl in range(5):
            L2, L2T = self.Lp[(lvl + 1) % 2], self.LTp[(lvl + 1) % 2]
            pd = nd()
            self.mm_heads(pd, [(L1T, L1)], [L1T, L1])
            kb.op("act", lambda e: e.activation(L2[:].rearrange("p h t -> p (h t)"), pd[:], AF.Copy), reads=[pd], writes=[L2])
            if lvl < 4:
                pd = nd()
                self.mm_heads(pd, [(L1, L1T)], [L1T, L1])
                kb.op("dve", lambda e: e.tensor_copy(L2T[:].rearrange("p h t -> p (h t)"), pd[:]), reads=[pd], writes=[L2T])
            yield
            pd = nd()
            self.mm_heads(pd, [(L2, QTb)], [L2, QTb])
            kb.op("dve", lambda e: e.tensor_tensor(QTf[:].rearrange("p h t -> p (h t)"), QTf[:].rearrange("p h t -> p (h t)"), pd[:], ALU.add), reads=[pd, QTf], writes=[QTf])
            kb.op("act", lambda e: e.activation(QTb[:], QTf[:], AF.Copy), reads=[QTf], writes=[QTb])
            L1, L1T = L2, L2T
            yield

    def seq(self, ci):
        kb, cx, z, D_, nd = self.kb, self.cx, self.z, self.D_, self.nd
        c = self.order[ci]
        is_lat = c >= 4
        c0 = c * 64
        S = self.sets[ci % 2]
        aT, rT, etT, LakT, MrbT, MrkT, QTb = S["aT"], S["rT"], S["etT"], S["LakT"], S["MrbT"], S["MrkT"], S["QTb"]
        Bst, Kst, Vst = S["Bst"], S["Kst"], S["Vst"]
        U0f, U0b, Xb, Pb = self.U0f, self.U0b, self.Xb, self.Pb
        pd = nd()
        self.mm_heads(pd, [(LakT, Vst), (aT, U0b)], [LakT, Vst, aT, U0b])
        kb.op("act", lambda e: e.activation(Xb[:].rearrange("p h t -> p (h t)"), pd[:], AF.Copy), reads=[pd], writes=[Xb])
        yield
        pd = nd()
        self.mm_heads(pd, [(QTb, Xb)], [QTb, Xb])
        kb.op("act", lambda e: e.activation(Pb[:].rearrange("p h t -> p (h t)"), pd[:], AF.Copy), reads=[pd], writes=[Pb])
        yield
        if is_lat:
            pd = nd()
            self.mm_heads(pd, [(rT, U0b), (MrbT, Pb), (MrkT, Vst)], [rT, U0b, MrbT, Pb, MrkT, Vst])
            yo = self.yout
            kb.op("act", lambda e: e.activation(yo[:].rearrange("p h t -> p (h t)"), pd[:], AF.Copy), reads=[pd], writes=[yo])
            yd = D_["Y"][z][c0 - 256:c0 - 192, :].rearrange("t (p h2 i) -> t p h2 i", h2=2, i=64)
            for h2 in range(2):
                kb.dma(yd[:, :, h2, :], yo[h2 * 64:(h2 + 1) * 64, :, :], reads=[yo], writes=[D_["Y"][z]])
            yield
        pd = nd()
        self.mm_heads(pd, [(Bst, Pb), (Kst, Vst)], [Bst, Pb, Kst, Vst])
        kb.op("dve", lambda e: e.tensor_tensor(U0f[:].rearrange("p h t -> p (h t)"), U0f[:].rearrange("p h t -> p (h t)"), pd[:], ALU.add), reads=[pd, U0f], writes=[U0f])
        kb.op("dve", lambda e: e.tensor_tensor(U0f[:], U0f[:], etT[:], ALU.mult), reads=[U0f, etT], writes=[U0f])
        kb.op("act", lambda e: e.activation(U0b[:], U0f[:], AF.Copy), reads=[U0f], writes=[U0b])
        yield


def run_gens(gens):
    gens = list(gens)
    while gens:
        for g in list(gens):
            try:
                next(g)
            except StopIteration:
                gens.remove(g)


def phase_rwkv_scans(kb, cx, D_, consts):
    cx.nd = BankRR(cx.psum)
    sc = [RwkvScan(kb, cx, z, D_, consts) for z in range(2)]
    run_gens([s_.prep(0) for s_ in sc])
    for ci in range(NCH):
        gens = []
        for s_ in sc:
            if ci + 1 < NCH:
                gens.append(s_.prep(ci + 1))
            gens.append(s_.seq(ci))
        run_gens(gens)


def phase_rwkv_readout(kb, cx, w, D_, MIXT):
    ps = cx.psum
    identb = cx.ident_bf
    tabs = kb.sb([128, 2, 1024], F32, "ro_tabs")
    kb.dma(tabs[:, 0, :], w["od_ln_w"][0].partition_broadcast(128), writes=[tabs])
    kb.dma(tabs[:, 1, :], w["od_ln_b"][0].partition_broadcast(128), writes=[tabs])
    epsc = kb.sb([128, 1], F32, "ro_eps")
    kb.op("dve", lambda e: e.memset(epsc[:], 64e-5), writes=[epsc])
    ys = [kb.sb([128, 16, 64], F32, "ro_ys%d" % i) for i in range(2)]
    bo = [kb.sb([128, 1024], F32, "ro_bo%d" % i) for i in range(2)]
    gg = [kb.sb([128, 1024], F32, "ro_g%d" % i) for i in range(2)]
    sq_l = [kb.sb([128, 16, 64], F32, "ro_sq%d" % i) for i in range(2)]
    st_l = [kb.sb([128, 32], F32, "ro_st%d" % i) for i in range(2)]
    ob_l = [kb.sb([128, 1024], BF16, "ro_ob%d" % i) for i in range(2)]
    oT = [kb.sb([128, 8, 128], BF16, "ro_oT%d" % i) for i in range(2)]
    def tile(n):
        rs = slice(n * 128, (n + 1) * 128)
        tsl = slice(256 + n * 128, 256 + (n + 1) * 128)
        y, b_, g_ = ys[n % 2], bo[n % 2], gg[n % 2]
        sq, st, ob = sq_l[n % 2], st_l[n % 2], ob_l[n % 2]
        yv = y[:].rearrange("p h j -> p (h j)")
        kb.dma(yv, D_["Y"][0][rs, :], reads=[D_["Y"][0]], writes=[y])
        kb.dma(sq[:].rearrange("p h j -> p (h j)"), D_["Y"][1][rs, :], reads=[D_["Y"][1]], writes=[sq])
        kb.op("dve", lambda e: e.tensor_tensor(y[:], y[:], sq[:], ALU.add), reads=[y, sq], writes=[y])
        kb.dma(b_[:], D_["BONUS"][tsl, :], reads=[D_["BONUS"]], writes=[b_])
        kb.dma(g_[:], D_["G"][tsl, :], reads=[D_["G"]], writes=[g_])
        yield
        kb.op("dve", lambda e: e.tensor_reduce(st[:, 0:16], y[:], AX.X, ALU.add), reads=[y], writes=[st])
        kb.op("dve", lambda e: e.tensor_scalar(st[:, 0:16], st[:, 0:16], 1.0 / 64, None, ALU.mult), reads=[st], writes=[st])
        kb.op("dve", lambda e: e.tensor_tensor(y[:], y[:], bc(st[:, 0:16].unsqueeze(2), [128, 16, 64]), ALU.subtract), reads=[y, st], writes=[y])
        kb.op("act", lambda e: e.activation(sq[:], y[:], AF.Square), reads=[y], writes=[sq])
        yield
        kb.op("dve", lambda e: e.tensor_reduce(st[:, 16:32], sq[:], AX.X, ALU.add), reads=[sq], writes=[st])
        kb.op("act", lambda e: e.activation(st[:, 16:32], st[:, 16:32], AF.Sqrt, bias=epsc[:, 0:1], scale=1.0 / 64), reads=[st, epsc], writes=[st])
        kb.op("dve", lambda e: e.reciprocal(st[:, 16:32], st[:, 16:32]), reads=[st], writes=[st])
        yield
        kb.op("dve", lambda e: e.tensor_tensor(y[:], y[:], bc(st[:, 16:32].unsqueeze(2), [128, 16, 64]), ALU.mult), reads=[y, st], writes=[y])
        kb.op("dve", lambda e: e.tensor_tensor(yv, yv, tabs[:, 0, :], ALU.mult), reads=[y, tabs], writes=[y])
        yield
        kb.op("pool", lambda e: e.tensor_tensor(b_[:], b_[:], tabs[:, 1, :], ALU.add), reads=[b_, tabs], writes=[b_])
        kb.op("dve", lambda e: e.tensor_tensor(yv, yv, b_[:], ALU.add), reads=[y, b_], writes=[y])
        kb.op("dve", lambda e: e.tensor_tensor(ob[:], yv, g_[:], ALU.mult), reads=[y, g_], writes=[ob])
        yield
        o = oT[n % 2]
        for half in range(2):
            ptr = ps[(2 * n + half) % 4]
            for c in range(4):
                cc = half * 4 + c
                kb.op("pe", lambda e: e.transpose(ptr[:].bitcast(BF16)[:, c * 128:(c + 1) * 128], ob[:, cc * 128:(cc + 1) * 128], identb[:]), reads=[ob, identb], writes=[ptr])
            kb.op("act", lambda e: e.activation(o[:, half * 4:(half + 1) * 4, :].rearrange("p c t -> p (c t)"), ptr[:].bitcast(BF16)[:, 0:512], AF.Copy), reads=[ptr], writes=[o])
        kb.dma(fm(MIXT[:], 256 + n * 128, 128), o[:], reads=[o], writes=[MIXT])
        yield
    for n0 in range(0, 16, 2):
        run_gens([tile(n0), tile(n0 + 1)])


RW_NAMES = ["od_mu", "od_w_rkv", "od_w0", "od_w1", "od_w2", "od_a0", "od_a1", "od_a2", "od_g1", "od_g2", "od_k_k", "od_k_a", "od_r_k",
            "od_ln_w", "od_ln_b", "od_w_o"]


def rwkv_dram(kb):
    D_ = {}
    BIG = kb.dram("rw_BIG", [NTOK, 9, 1024], BF16)
    BIGF = kb.dram("rw_BIGF", [NTOK, 4, 1024], F32)
    D_["BIG"], D_["BIGF"] = BIG, BIGF

    def view(big, slot, name):
        v = T(big.t[:, slot, :], name)
        v.ws, v.rs = big.ws, big.rs
        return v
    D_["V"] = view(BIG, 0, "rw_V")
    for k_, nm in enumerate(("RT", "BT", "KT", "AT")):
        D_[nm] = [view(BIG, 1 + 2 * k_ + z, "rw_%s%d" % (nm, z)) for z in range(2)]
    D_["ETOT"] = [view(BIGF, z, "rw_ETOT%d" % z) for z in range(2)]
    D_["BONUS"] = view(BIGF, 2, "rw_BONUS")
    D_["G"] = view(BIGF, 3, "rw_G")
    D_["Y"] = [kb.dram("rw_Y%d" % z, [NLAT, 1024], F32) for z in range(2)]
    return D_


def layer1_mixer(kb, cx, w, consts, XT1, XT2, MIXT):
    D_ = rwkv_dram(kb)
    HT = kb.dram("rw_HT", [1024, NTOK], BF16)
    with kb.scope():
        Wd = rwkv_weights_alloc(kb)
        with kb.scope():
            hT = kb.sb([128, 8, NTOK], BF16, "hT")
            setup_modbuf(kb, cx)
            kb.set_stage(4, 1024)
            wl = rwkv_weights_load(kb, Wd, w)
            for _ in range(4):
                next(wl, None)
            for ti_ in range(len(TILES)):
                phase_modulate(kb, cx, XT1, 1, w["norm_mix_w"][1], 0, 1, hT, tiles=TILES[ti_:ti_ + 1], first=(ti_ == 0))
                for _ in range(3):
                    next(wl, None)
            for _ in wl:
                pass
            for c in range(8):
                kb.dma(HT[c * 128:(c + 1) * 128, :], hT[:, c, :], reads=[hT], writes=[HT])
        phase_rwkv_feat(kb, cx, HT, w, consts, D_, Wd)
    with kb.scope():
        phase_rwkv_scans(kb, cx, D_, consts)
    with kb.scope():
        phase_rwkv_readout(kb, cx, w, D_, MIXT)
    with kb.scope():
        phase_wout(kb, cx, 1, w["od_w_o"][0], MIXT, XT1, XT2, tiles=TILES[1:])
    return D_


def rwkv_consts():
    c = {}
    s = np.arange(128)[:, None]; t = np.arange(128)[None, :]
    same = (s // 64) == (t // 64)
    c["rw_CM"] = np.stack([same & (s <= t), same & (s < t), same & (s >= t), same & (s > t), same]).astype(np.float32)
    r = np.arange(64)[:, None]; cc = np.arange(64)[None, :]
    c["rw_MK"] = np.stack([r < cc, r <= cc, r > cc, r >= cc]).astype(np.float32)
    c["rw_MK2"] = np.concatenate([c["rw_MK"], c["rw_MK"]], axis=1)
    c["rw_I2"] = np.concatenate([np.eye(64), np.eye(64)], 0).astype(np.float32)
    E = np.zeros((2, 64, 128), np.float32)
    E[0, np.arange(64), np.arange(64)] = 1.0
    E[1, np.arange(64), 64 + np.arange(64)] = 1.0
    c["rw_E"] = E
    return c
```

```python
import numpy as np
import concourse.bass as bass
import concourse.mybir as mybir
from concourse.bass_utils import run_bass_kernel_spmd

F32 = mybir.dt.float32
BF16 = mybir.dt.bfloat16
AF = mybir.ActivationFunctionType
ALU = mybir.AluOpType
AX = mybir.AxisListType

D = 1024
NCTX = 256
NLAT = 2048
NTOK = NCTX + NLAT
EPS = 1e-6


class H:
    __slots__ = ("name", "ws", "rs")

    def __init__(self, name=""):
        self.name = name
        self.ws = {}
        self.rs = {}


class T(H):
    __slots__ = ("t",)

    def __init__(self, t, name=""):
        H.__init__(self, name)
        self.t = t

    def __getitem__(self, k):
        return self.t[k]


class KB:
    ENG = ("pe", "dve", "act", "pool", "sp")

    def __init__(self, n_dma_sems=48):
        nc = bass.Bass("TRN2", target_bir_lowering=False)
        self.nc = nc
        self.eng = dict(pe=nc.tensor, dve=nc.vector, act=nc.scalar, pool=nc.gpsimd, sp=nc.sync)
        self.esem = {e: nc.alloc_semaphore("es_" + e) for e in self.ENG}
        self.ecnt = {e: 0 for e in self.ENG}
        self.known = {e: {} for e in self.ENG}
        self.dsems = [nc.alloc_semaphore("ds_%d" % i) for i in range(n_dma_sems)]
        self.dval = [0] * n_dma_sems
        self.dnext = 0
        self.n_ins = 0
        self.n_wait = 0
        self.uid = 0
        self.stacks = []
        self.stage_i = 0
        self.pending = None
        self.attach_waits = True
        self.snaps = {}

    def sb(self, shape, dtype=F32, name=None):
        self.uid += 1
        name = (name or "sb") + "_%d" % self.uid
        if self.stacks:
            t = self.stacks[-1].enter_context(self.nc.sbuf_tensor(name, list(shape), dtype))
        else:
            t = self.nc.alloc_sbuf_tensor(name, list(shape), dtype)
        return T(t, name)

    def scope(self):
        kb = self

        class _S:
            def __enter__(s2):
                import contextlib
                st = contextlib.ExitStack()
                kb.stacks.append(st)
                return st

            def __exit__(s2, *a):
                kb.barrier()
                st = kb.stacks.pop()
                st.close()
                return False
        return _S()

    def barrier(self):
        for e in self.ENG:
            for i, v in enumerate(self.dval):
                if v:
                    self._wait(e, ("dma", i), v)
            for p in self.ENG:
                if p != e and self.ecnt[p]:
                    self._wait(e, ("eng", p), self.ecnt[p])

    def ps(self, shape=(128, 512), dtype=F32, name=None):
        self.uid += 1
        name = name or "ps%d" % self.uid
        return T(self.nc.alloc_psum_tensor(name, list(shape), dtype), name)

    def dram(self, name, shape, dtype=F32, kind="Internal"):
        return T(self.nc.dram_tensor(name, list(shape), dtype, kind=kind), name)

    def _wait(self, e, key, val):
        if key[0] == "eng":
            if key[1] == e and e == "pe":
                return
            sem = self.esem[key[1]]
        else:
            sem = self.dsems[key[1]]
            val = max(val, self.dval[key[1]])
        if self.known[e].get(key, 0) >= val:
            return
        self.known[e][key] = val
        self.n_wait += 1
        sn = self.snaps.get((key, val))
        if sn is not None:
            kn = self.known[e]
            for k2, v2 in sn.items():
                if kn.get(k2, 0) < v2:
                    kn[k2] = v2
        if self.pending is not None:
            self.pending.append((sem, val))
        else:
            self.eng[e].wait_ge(sem, val)

    def _deps(self, e, reads, writes):
        me = ("eng", e)
        for h in reads:
            for k, v in h.ws.items():
                self._wait(e, k, v)
        for h in writes:
            for k, v in h.ws.items():
                self._wait(e, k, v)
            for k, v in h.rs.items():
                self._wait(e, k, v)

    def _commit(self, key, val, reads, writes):
        for h in reads:
            if h.rs.get(key, 0) < val:
                h.rs[key] = val
        for h in writes:
            if h.ws.get(key, 0) < val:
                h.ws[key] = val

    def op(self, e, fn, reads=(), writes=()):
        if self.attach_waits:
            self.pending = []
            self._deps(e, reads, writes)
            pend, self.pending = self.pending, None
            for (sem, val) in pend[:-1]:
                self.eng[e].wait_ge(sem, val)
            ins = fn(self.eng[e])
            if pend:
                ins._wait_ge(pend[-1][0], pend[-1][1])
        else:
            self._deps(e, reads, writes)
            ins = fn(self.eng[e])
        self.ecnt[e] += 1
        ins.then_inc(self.esem[e], 1)
        self.snaps[(("eng", e), self.ecnt[e])] = dict(self.known[e])
        self._commit(("eng", e), self.ecnt[e], reads, writes)
        self.n_ins += 1
        return ins

    def dma(self, out, in_, reads=(), writes=(), q="sp", **kw):
        self.pending = []
        self._deps(q, reads, writes)
        pend, self.pending = self.pending, None
        for (sem, val) in pend[:-1]:
            self.eng[q].wait_ge(sem, val)
        i = self.dnext
        self.dnext = (self.dnext + 1) % len(self.dsems)
        ins = self.eng[q].dma_start(out=out, in_=in_, **kw)
        if pend:
            ins._wait_ge(pend[-1][0], pend[-1][1])
        self.dval[i] += 16
        ins.then_inc(self.dsems[i], 16)
        self.snaps[(("dma", i), self.dval[i])] = dict(self.known[q])
        self._commit(("dma", i), self.dval[i], reads, writes)
        self.n_ins += 1
        return ins

    def set_stage(self, n=4, size=1024):
        self.stage = [self.sb([128, size], F32, "stage%d" % i) for i in range(n)]
        self.stage_size = size

    def load_cast(self, dst_ap, src_ap, dst_h, engs=("act", "pool", "act")):
        shp = list(dst_ap.shape)
        P = shp[0]
        n = 1
        for d_ in shp[1:]:
            n *= d_
        assert n <= self.stage_size, (shp, self.stage_size)
        st = self.stage[self.stage_i % len(self.stage)]
        eng = engs[self.stage_i % len(engs)]
        self.stage_i += 1
        v = st[0:P, 0:n]
        if len(shp) == 3:
            v = v.rearrange("p (a b) -> p a b", a=shp[1])
        elif len(shp) == 4:
            v = v.rearrange("p (a b c) -> p a b c", a=shp[1], b=shp[2])
        self.dma(v, src_ap, writes=[st])
        if eng == "act":
            self.op("act", lambda e: e.activation(dst_ap, v, AF.Copy), reads=[st], writes=[dst_h])
        else:
            self.op(eng, lambda e: e.tensor_copy(dst_ap, v), reads=[st], writes=[dst_h])

    def finish(self):
        for i, v in enumerate(self.dval):
            if v:
                self._wait("sp", ("dma", i), v)
        for e in self.ENG:
            if e != "sp" and self.ecnt[e]:
                self._wait("sp", ("eng", e), self.ecnt[e])


class Ctx:
    pass


def fm(ap_dram, t0, n):
    return ap_dram.rearrange("(c p) t -> p c t", p=128)[:, :, t0:t0 + n]


TILES = [(0, 256, True)] + [(256 + 512 * i, 512, False) for i in range(4)]


def phase_mod(kb, cx, c_lat, c_ctx, ada_w, ada_b):
    mod = cx.mod
    sT = kb.sb([128, 8, 2], F32, "sT")
    cin = kb.sb([128, 8, 2], F32, "cin")
    with kb.nc.allow_non_contiguous_dma(reason="tiny"):
        kb.dma(cin[:, :, 0], c_lat[:].rearrange("(c p) -> p c", p=128), writes=[cin])
        kb.dma(cin[:, :, 1], c_ctx[:].rearrange("(c p) -> p c", p=128), writes=[cin])
    kb.op("act", lambda e: e.activation(sT[:], cin[:], AF.Silu), reads=[cin], writes=[sT])
    ident = cx.ident_f32
    wbuf = [kb.sb([128, 8, 512], F32, "adaw%d" % i) for i in range(4)]
    mrow = kb.sb([2, 6144], F32, "mrow")
    brow = kb.sb([2, 6144], F32, "brow")
    pm = cx.psum[0]
    pt = cx.psum[1]
    it = 0
    for L in range(2):
        kb.dma(brow[0:1, :], ada_b[L:L + 1, :], writes=[brow])
        kb.dma(brow[1:2, :], ada_b[L:L + 1, :], writes=[brow])
        for nt in range(12):
            wb = wbuf[it % 4]
            it += 1
            kb.dma(wb[:], ada_w[L].rearrange("(c p) n -> p c n", p=128)[:, :, nt * 512:(nt + 1) * 512], writes=[wb])
            for kc in range(8):
                kb.op("pe", lambda e: e.matmul(pm[0:2, :], sT[:, kc, :], wb[:, kc, :], start=(kc == 0), stop=(kc == 7)),
                      reads=[sT, wb], writes=[pm])
            kb.op("dve", lambda e: e.tensor_tensor(mrow[:, nt * 512:(nt + 1) * 512], pm[0:2, :], brow[:, nt * 512:(nt + 1) * 512], ALU.add),
                  reads=[pm, brow], writes=[mrow])
        for c in range(48):
            kb.op("pe", lambda e: e.matmul(pt[:, 2 * c:2 * c + 2], mrow[0:2, c * 128:(c + 1) * 128], ident[0:2, 0:2], start=True, stop=True),
                  reads=[mrow, ident], writes=[pt])
        kb.op("dve", lambda e: e.tensor_copy(mod[:, L, :, :], pt[:, 0:96].rearrange("p (c s) -> p s c", s=2)), reads=[pt], writes=[mod])


def mod_vec(cx, L, s, idx):
    return cx.mod[:, L, s, idx * 8:(idx + 1) * 8]


def phase_modulate(kb, cx, XT, L, normw_dram, shift_idx, scale_idx, hT, tiles=TILES, hT_t0=0, first=True):
    if first:
        nw = kb.sb([128, 8], F32)
        with kb.nc.allow_non_contiguous_dma(reason="tiny"):
            kb.dma(nw[:], normw_dram.rearrange("(c p) -> p c", p=128), writes=[nw])
        gmul = kb.sb([128, 2, 8], F32)
        for s in range(2):
            kb.op("dve", lambda e: e.scalar_tensor_tensor(gmul[:, s, :], mod_vec(cx, L, s, scale_idx), 1.0, nw[:], ALU.add, ALU.mult),
                  reads=[cx.mod, nw], writes=[gmul])
        cx.gmul_cur = gmul
    gmul = cx.gmul_cur
    xb = cx.xbuf
    for ti, (t0, n, is_ctx) in enumerate(tiles):
        s = 1 if is_ctx else 0
        par = getattr(cx, "mod_cnt", 0) % 2
        cx.mod_cnt = getattr(cx, "mod_cnt", 0) + 1
        x = xb[par]
        sqb = cx.sqb[par]
        rstd = cx.rstd[par]
        ps = cx.psum[2 + par]
        kb.dma(x[:, :, 0:n], fm(XT[:], t0, n), reads=[XT], writes=[x])
        kb.op("act", lambda e: e.activation(sqb[:, :, 0:n], x[:, :, 0:n], AF.Square), reads=[x], writes=[sqb])
        for c in range(8):
            kb.op("pe", lambda e: e.matmul(ps[:, 0:n], cx.ones_bf[:], sqb[:, c, 0:n], start=(c == 0), stop=(c == 7)),
                  reads=[sqb, cx.ones_bf], writes=[ps])
        kb.op("act", lambda e: e.activation(rstd[:, 0:n], ps[:, 0:n], AF.Sqrt, bias=cx.eps_col[:, 0:1], scale=1.0 / D), reads=[ps, cx.eps_col], writes=[rstd])
        kb.op("dve", lambda e: e.reciprocal(rstd[:, 0:n], rstd[:, 0:n]), reads=[rstd], writes=[rstd])
        for c in range(8):
            sq = cx.sqr[c % 4]
            kb.op("dve", lambda e: e.tensor_tensor(sq[:, 0:n], x[:, c, 0:n], rstd[:, 0:n], ALU.mult), reads=[x, rstd], writes=[sq])
            kb.op("act", lambda e: e.activation(hT[:, c, t0 - hT_t0:t0 - hT_t0 + n], sq[:, 0:n], AF.Identity,
                                                bias=mod_vec(cx, L, s, shift_idx)[:, c:c + 1], scale=gmul[:, s, c:c + 1]),
                  reads=[sq, cx.mod, gmul], writes=[hT])


def phase_ffn(kb, cx, XT, L, hT, hT_t0, tiles, w13_list, w2_list, hidden, gate_idx, gates=None, hc_group=4):
    nE = len(w13_list)
    nhc = hidden // 128
    groups = [(g0, min(hc_group, nhc - g0)) for g0 in range(0, nhc, hc_group)]
    ntok = sum(n for _, n, _ in tiles)
    acc = cx.ffn_acc
    w13b = cx.w13buf
    w2b = cx.w2buf
    act = cx.actbuf
    tmp = cx.ffn_tmp
    it = 0
    pidx = [0]
    units = []
    gi = 0
    for e_i in range(nE):
        for (g0, gn) in groups:
            for ti in range(len(tiles)):
                units.append((e_i, g0, gn, ti, gi))
            gi += 1
    offs = []
    o_ = 0
    for (t0, n, _) in tiles:
        offs.append(o_)
        o_ += n
    wcur = {}

    def load_w(gidx, e_i, g0, gn):
        wa = w13b[gidx % 2]
        wb = w2b[gidx % 2]
        w13 = w13_list[e_i].rearrange("(c p) n -> p c n", p=128)
        for half in range(2):
            for cc in range(0, 8, 2):
                kb.load_cast(wa[:, cc:cc + 2, half, 0:gn * 128], w13[:, cc:cc + 2, half * hidden + g0 * 128: half * hidden + (g0 + gn) * 128], wa)
        w2 = w2_list[e_i].rearrange("(c p) n -> p c n", p=128)
        for cc in range(gn):
            kb.load_cast(wb[:, cc, :], w2[:, g0 + cc, :], wb)
        wcur[gidx] = (wa, wb)

    ginfo = {}
    for (e_i, g0, gn, ti, gidx) in units:
        ginfo[gidx] = (e_i, g0, gn)
    ngroups = len(ginfo)

    def P1(u):
        e_i, g0, gn, ti, gidx = units[u]
        wa, wb = wcur[gidx]
        t0, n, is_ctx = tiles[ti]
        a = act[u % 2]
        for hc in range(gn):
            pg = cx.psum[pidx[0] % 8]; pidx[0] += 1
            pu = cx.psum[pidx[0] % 8]; pidx[0] += 1
            for kc in range(8):
                kb.op("pe", lambda e: e.matmul(pg[:, 0:n], wa[:, kc, 0, hc * 128:(hc + 1) * 128], hT[:, kc, t0 - hT_t0:t0 - hT_t0 + n],
                                               start=(kc == 0), stop=(kc == 7)), reads=[wa, hT], writes=[pg])
            for kc in range(8):
                kb.op("pe", lambda e: e.matmul(pu[:, 0:n], wa[:, kc, 1, hc * 128:(hc + 1) * 128], hT[:, kc, t0 - hT_t0:t0 - hT_t0 + n],
                                               start=(kc == 0), stop=(kc == 7)), reads=[wa, hT], writes=[pu])
            tt = tmp[hc % 2]
            kb.op("act", lambda e: e.activation(tt[:, 0:n], pg[:, 0:n], AF.Silu), reads=[pg], writes=[tt])
            if gates is None:
                kb.op("dve", lambda e: e.tensor_tensor(a[:, hc, 0:n], tt[:, 0:n], pu[:, 0:n], ALU.mult), reads=[tt, pu], writes=[a])
            else:
                kb.op("dve", lambda e: e.tensor_tensor(tt[:, 0:n], tt[:, 0:n], pu[:, 0:n], ALU.mult), reads=[tt, pu], writes=[tt])
                gt = gates[ti]
                kb.op("dve", lambda e: e.tensor_tensor(a[:, hc, 0:n], tt[:, 0:n], gt[:, e_i, 0:n], ALU.mult), reads=[tt, gt], writes=[a])

    def P2(u):
        e_i, g0, gn, ti, gidx = units[u]
        wa, wb = wcur[gidx]
        t0, n, is_ctx = tiles[ti]
        a = act[u % 2]
        off = offs[ti]
        for dc in range(8):
            po = cx.psum[pidx[0] % 8]; pidx[0] += 1
            for hc in range(gn):
                kb.op("pe", lambda e: e.matmul(po[:, 0:n], wb[:, hc, dc * 128:(dc + 1) * 128], a[:, hc, 0:n],
                                               start=(hc == 0), stop=(hc == gn - 1)), reads=[wb, a], writes=[po])
            if gidx == 0:
                kb.op("dve", lambda e: e.tensor_copy(acc[:, dc, off:off + n], po[:, 0:n]), reads=[po], writes=[acc])
            else:
                kb.op("dve", lambda e: e.tensor_tensor(acc[:, dc, off:off + n], acc[:, dc, off:off + n], po[:, 0:n], ALU.add), reads=[po, acc], writes=[acc])

    load_w(0, *ginfo[0])
    for u in range(len(units) + 1):
        if u < len(units):
            P1(u)
        if u >= 1:
            P2(u - 1)
        if u < len(units) and units[u][3] == 0 and units[u][4] + 1 < ngroups:
            g1 = units[u][4] + 1
            load_w(g1, *ginfo[g1])
    rt = []
    for wbuf_ in w13b:
        v_ = wbuf_[:].rearrange("p a b c -> p (a b c)").bitcast(F32)
        for i_ in range(v_.shape[1] // 512):
            t_ = T(v_[:, i_ * 512:(i_ + 1) * 512], "ffn_rt")
            t_.ws, t_.rs = dict(wbuf_.ws), dict(wbuf_.rs)
            rt.append(t_)
    its = []
    off = 0
    for ti, (t0, n, is_ctx) in enumerate(tiles):
        for c in range(8):
            its.append((t0, n, 1 if is_ctx else 0, c, off))
        off += n
    LOOK = min(12, len(rt) - 2)

    def ld(k):
        t0, n, s_, c, off_ = its[k]
        x = rt[k % len(rt)]
        kb.dma(x[:, 0:n], XT[c * 128:(c + 1) * 128, t0:t0 + n], reads=[XT], writes=[x])
    for k in range(min(LOOK, len(its))):
        ld(k)
    for k in range(len(its)):
        t0, n, s_, c, off_ = its[k]
        x = rt[k % len(rt)]
        kb.op("dve", lambda e: e.scalar_tensor_tensor(x[:, 0:n], acc[:, c, off_:off_ + n], mod_vec(cx, L, s_, gate_idx)[:, c:c + 1], x[:, 0:n], ALU.mult, ALU.add),
              reads=[acc, cx.mod, x], writes=[x])
        kb.dma(XT[c * 128:(c + 1) * 128, t0:t0 + n], x[:, 0:n], reads=[x], writes=[XT])
        if k + LOOK < len(its):
            ld(k + LOOK)


def setup_common(kb, cx, consts):
    cx.psd = [kb.ps((128, 1024), F32, "psd%d" % i) for i in range(4)]
    cx.psum = []
    for i in range(4):
        cx.psum.append(T(cx.psd[i].t[:, 0:512], "psum%d" % (2 * i)))
        cx.psum.append(T(cx.psd[i].t[:, 512:1024], "psum%d" % (2 * i + 1)))
    cx.ident_f32 = kb.sb([128, 128], F32, "ident_f32")
    cx.ones_f32 = kb.sb([128, 128], F32, "ones_f32")
    cx.eps_col = kb.sb([128, 1], F32, "eps_col")
    kb.dma(cx.ident_f32[:], consts["ident"][:], writes=[cx.ident_f32])
    kb.op("dve", lambda e: e.memset(cx.ones_f32[:], 1.0), writes=[cx.ones_f32])
    kb.op("dve", lambda e: e.memset(cx.eps_col[:], EPS), writes=[cx.eps_col])
    cx.gn_eps_col = kb.sb([128, 1], F32, "gn_eps_col")
    kb.op("dve", lambda e: e.memset(cx.gn_eps_col[:], 1e-5), writes=[cx.gn_eps_col])
    cx.ident_bf = kb.sb([128, 128], BF16, "ident_bf")
    kb.op("dve", lambda e: e.tensor_copy(cx.ident_bf[:], cx.ident_f32[:]), reads=[cx.ident_f32], writes=[cx.ident_bf])


def setup_modbuf(kb, cx):
    cx.xbuf = [kb.sb([128, 8, 512], F32, "xbuf%d" % i) for i in range(2)]
    cx.sqr = [kb.sb([128, 512], F32, "sqr%d" % i) for i in range(4)]
    cx.sqb = [kb.sb([128, 8, 512], BF16, "sqb%d" % i) for i in range(2)]
    cx.rstd = [kb.sb([128, 512], F32, "rstd%d" % i) for i in range(2)]
    cx.ones_bf = kb.sb([128, 128], BF16, "ones_bf")
    kb.op("dve", lambda e: e.memset(cx.ones_bf[:], 1.0), writes=[cx.ones_bf])


def setup_ffn(kb, cx, ntok_max, hc_group=4):
    cx.ffn_acc = kb.sb([128, 8, ntok_max], F32, "ffn_acc")
    cx.w13buf = [kb.sb([128, 8, 2, hc_group * 128], BF16, "w13b%d" % i) for i in range(2)]
    cx.w2buf = [kb.sb([128, hc_group, 1024], BF16, "w2b%d" % i) for i in range(2)]
    cx.actbuf = [kb.sb([128, hc_group, 512], BF16, "actb%d" % i) for i in range(2)]
    cx.ffn_tmp = [kb.sb([128, 512], F32, "ffnt%d" % i) for i in range(2)]
    kb.set_stage(4, 1024)


def phase_mod_scoped(kb, cx, c_lat, c_ctx, ada_w, ada_b):
    cx.mod = kb.sb([128, 2, 2, 48], F32, "mod")
    with kb.scope():
        phase_mod(kb, cx, c_lat, c_ctx, ada_w, ada_b)


RET_PF = {0: 0, 1: 1}
RET_PB = {0: 1, 1: 0}
for _i in range(16):
    RET_PF[2 + _i] = 2 + _i
    RET_PB[2 + _i] = 2 + (15 - _i)


def bc(ap, shape):
    return ap.broadcast_to(list(shape))


def phase_retention(kb, cx, hT, w_in, dec_f, dec_b, gn_w, consts, MIXT, na_args=None):
    ps = cx.psum
    identb = cx.ident_bf
    wb = kb.sb([128, 8, 2048], BF16, "w_ret")
    wv = w_in.rearrange("(c p) n -> p c n", p=128)
    na = None
    if na_args is not None:
        na = NaProj(kb, cx, w_in, na_args["qn_w"], na_args["kn_w"])
    with kb.scope():
        kb.set_stage(8, 1024)
        for g in range(4):
            for cc in range(0, 8, 2):
                kb.load_cast(wb[:, cc:cc + 2, g * 512:(g + 1) * 512], wv[:, cc:cc + 2, g * 512:(g + 1) * 512], wb, engs=("act", "pool", "dve"))
        if na is not None:
            na.load_weights()
    if na is not None:
        na.alloc()
    cos_t = kb.sb([128, 18, 128], F32, "cos_t")
    sin_t = kb.sb([128, 18, 128], F32, "sin_t")
    kb.dma(cos_t[:], consts["rope_cos"][:].rearrange("(n p) d -> p n d", p=128), writes=[cos_t])
    kb.dma(sin_t[:], consts["rope_sin"][:].rearrange("(n p) d -> p n d", p=128), writes=[sin_t])
    qT = kb.sb([128, 18, 4, 128], BF16, "qT")
    kT = kb.sb([128, 18, 4, 128], BF16, "kT")
    v_all = kb.sb([128, 18, 512], BF16, "v_all")
    t1 = kb.sb([128, 512], F32, "rt1")
    t2 = kb.sb([128, 512], F32, "rt2")
    qk_tm = [kb.sb([128, 512], BF16, "qk_tm%d" % i) for i in range(2)]
    lg = kb.sb([128, 8], F32, "lg")
    kb.dma(lg[:, 0:4], dec_f.partition_broadcast(128), writes=[lg])
    kb.dma(lg[:, 4:8], dec_b.partition_broadcast(128), writes=[lg])
    kb.op("act", lambda e: e.activation(lg[:], lg[:], AF.Exp), reads=[lg], writes=[lg])
    kb.op("dve", lambda e: e.tensor_scalar(lg[:], lg[:], -1.0, None, ALU.mult), reads=[lg], writes=[lg])
    cst = kb.sb([128, 5, 128], F32, "ret_cst")
    kb.dma(cst[:], consts["ret_cst"][:].rearrange("k p t -> p k t"), writes=[cst])
    Dsame = kb.sb([128, 4, 128], F32, "Dsame")
    Dfull = kb.sb([128, 2, 4, 128], F32, "Dfull")
    GP = kb.sb([128, 2, 4, 18], F32, "GP")
    nlg = kb.sb([128, 4], F32, "nlg")
    kidx = kb.sb([128, 18], F32, "kidx")
    kb.dma(kidx[:], consts["kidx128"][:].partition_broadcast(128), writes=[kidx])
    ksc = 128.0 ** -0.5
    for h in range(4):
        kb.op("act", lambda e: e.activation(t1[:, 0:128], cst[:, 1, :], AF.Exp, scale=lg[:, h:h + 1]), reads=[cst, lg], writes=[t1])
        kb.op("dve", lambda e: e.scalar_tensor_tensor(t1[:, 0:128], t1[:, 0:128], ksc, cst[:, 3, :], ALU.mult, ALU.mult), reads=[t1, cst], writes=[t1])
        kb.op("act", lambda e: e.activation(t2[:, 0:128], cst[:, 2, :], AF.Exp, scale=lg[:, 4 + h:5 + h]), reads=[cst, lg], writes=[t2])
        kb.op("dve", lambda e: e.scalar_tensor_tensor(t2[:, 0:128], t2[:, 0:128], ksc, cst[:, 4, :], ALU.mult, ALU.mult), reads=[t2, cst], writes=[t2])
        kb.op("dve", lambda e: e.tensor_tensor(Dsame[:, h, :], t1[:, 0:128], t2[:, 0:128], ALU.add), reads=[t1, t2], writes=[Dsame])
        kb.op("act", lambda e: e.activation(Dfull[:, 0, h, :], cst[:, 0, :], AF.Exp, scale=lg[:, h:h + 1]), reads=[cst, lg], writes=[Dfull])
        kb.op("dve", lambda e: e.tensor_scalar(nlg[:, h:h + 1], lg[:, 4 + h:5 + h], -1.0, None, ALU.mult), reads=[lg], writes=[nlg])
        kb.op("act", lambda e: e.activation(Dfull[:, 1, h, :], cst[:, 0, :], AF.Exp, scale=nlg[:, h:h + 1]), reads=[cst, nlg], writes=[Dfull])
        for d in range(2):
            kb.op("act", lambda e: e.activation(GP[:, d, h, :], kidx[:], AF.Exp, scale=lg[:, 4 * d + h:4 * d + h + 1]), reads=[kidx, lg], writes=[GP])
    kb.op("dve", lambda e: e.tensor_scalar(Dfull[:].rearrange("p a b c -> p (a b c)"), Dfull[:].rearrange("p a b c -> p (a b c)"), ksc, None, ALU.mult), reads=[Dfull], writes=[Dfull])
    for n in range(18):
        tsl = slice(n * 128, (n + 1) * 128)
        pq, pk, pv = ps[0], ps[1], ps[2]
        for g, pp in ((0, pq), (1, pk), (2, pv)):
            for kc in range(8):
                kb.op("pe", lambda e: e.matmul(pp[:], hT[:, kc, tsl], wb[:, kc, g * 512:(g + 1) * 512], start=(kc == 0), stop=(kc == 7)),
                      reads=[hT, wb], writes=[pp])
        kb.op("act", lambda e: e.activation(v_all[:, n, :], pv[:], AF.Copy), reads=[pv], writes=[v_all])
        for gi, (pp, dstT) in enumerate(((pq, qT), (pk, kT))):
            tm = qk_tm[gi]
            kb.op("dve", lambda e: e.tensor_tensor(t1[:].rearrange("p (h d) -> p h d", h=4), pp[:].rearrange("p (h d) -> p h d", h=4),
                                                   bc(cos_t[:, n:n + 1, :], [128, 4, 128]), ALU.mult), reads=[pp, cos_t], writes=[t1])
            ppv = pp[:].rearrange("p (hb two f) -> p hb two f", two=2, f=32)
            t2v = t2[:].rearrange("p (hb two f) -> p hb two f", two=2, f=32)
            snv = sin_t[:, n, :].rearrange("p (b two f) -> p b two f", two=2, f=32)
            for half in range(2):
                kb.op("dve", lambda e: e.tensor_tensor(t2v[:, :, half, :].rearrange("p (h b) f -> p h b f", h=4),
                                                       ppv[:, :, 1 - half, :].rearrange("p (h b) f -> p h b f", h=4),
                                                       bc(snv[:, :, half, :].unsqueeze(1), [128, 4, 2, 32]), ALU.mult), reads=[pp, sin_t], writes=[t2])
            kb.op("dve", lambda e: e.tensor_tensor(tm[:], t1[:], t2[:], ALU.add), reads=[t1, t2], writes=[tm])
            ptr = ps[3 + gi]
            for h in range(4):
                kb.op("pe", lambda e: e.transpose(ptr[:].bitcast(BF16)[:, h * 128:(h + 1) * 128], tm[:, h * 128:(h + 1) * 128], identb[:]),
                      reads=[tm, identb], writes=[ptr])
            kb.op("act", lambda e: e.activation(dstT[:, n, :, :].rearrange("p h t -> p (h t)"), ptr[:].bitcast(BF16)[:, 0:512], AF.Copy), reads=[ptr], writes=[dstT])
        if na is not None:
            na.tile(n, hT, na_args["NQT"], na_args["NKT"], na_args["NV"], [ps[5], ps[6], ps[7]])
    gnw = kb.sb([128, 512], F32, "gnw")
    kb.dma(gnw[:], gn_w.partition_broadcast(128), writes=[gnw])
    ysb = kb.sb([128, 4, 128], F32, "ysb")
    ysq = kb.sb([128, 4, 128], F32, "ysq")
    st = kb.sb([128, 8], F32, "ystat")
    sg = kb.sb([128, 512], F32, "sg")
    mixb = kb.sb([128, 512], BF16, "mixb")
    mixT_sb = [kb.sb([128, 4, 128], BF16, "mixTsb%d" % i) for i in range(2)]
    pT = [kb.sb([128, 128], BF16, "pT%d" % i) for i in range(4)]
    cnt = 0
    for n in range(18):
        tsl = slice(n * 128, (n + 1) * 128)
        terms = [(n, None, None)]
        for m in range(18):
            if m == n:
                continue
            if RET_PF[m] < RET_PF[n]:
                terms.append((m, 0, RET_PF[n] - RET_PF[m]))
            if RET_PB[m] < RET_PB[n]:
                terms.append((m, 1, RET_PB[n] - RET_PB[m]))
        py = ps[5]
        items = [(h, ti, m, d, dp) for h in range(4) for ti, (m, d, dp) in enumerate(terms)]
        pas = [ps[1], ps[2], ps[6], ps[7]]

        def QK(i):
            h, ti, m, d, dp = items[i]
            pa = pas[i % 4]
            kb.op("pe", lambda e: e.matmul(pa[:, 0:128], kT[:, m, h, :], qT[:, n, h, :], start=True, stop=True), reads=[kT, qT], writes=[pa])

        def PV(i):
            h, ti, m, d, dp = items[i]
            pa = pas[i % 4]
            pt_ = pT[i % 4]
            if d is None:
                kb.op("dve", lambda e: e.tensor_tensor(pt_[:], pa[:, 0:128], Dsame[:, h, :], ALU.mult), reads=[pa, Dsame], writes=[pt_])
            else:
                kb.op("dve", lambda e: e.scalar_tensor_tensor(pt_[:], pa[:, 0:128], GP[:, d, h, dp:dp + 1], Dfull[:, d, h, :], ALU.mult, ALU.mult),
                      reads=[pa, GP, Dfull], writes=[pt_])
            kb.op("pe", lambda e: e.matmul(py[:, h * 128:(h + 1) * 128], pt_[:], v_all[:, m, h * 128:(h + 1) * 128], start=(ti == 0), stop=(ti == len(terms) - 1)),
                  reads=[pt_, v_all], writes=[py])
        LOOK = 2
        for i in range(min(LOOK, len(items))):
            QK(i)
        for i in range(len(items)):
            if i + LOOK < len(items):
                QK(i + LOOK)
            PV(i)
        pg = ps[0]
        for kc in range(8):
            kb.op("pe", lambda e: e.matmul(pg[:], hT[:, kc, tsl], wb[:, kc, 1536:2048], start=(kc == 0), stop=(kc == 7)), reads=[hT, wb], writes=[pg])
        kb.op("act", lambda e: e.activation(sg[:], pg[:], AF.Silu), reads=[pg], writes=[sg])
        kb.op("act", lambda e: e.activation(ysb[:].rearrange("p h e -> p (h e)"), py[:], AF.Copy), reads=[py], writes=[ysb])
        kb.op("dve", lambda e: e.tensor_reduce(st[:, 0:4], ysb[:], AX.X, ALU.add), reads=[ysb], writes=[st])
        kb.op("dve", lambda e: e.tensor_scalar(st[:, 0:4], st[:, 0:4], 1.0 / 128, None, ALU.mult), reads=[st], writes=[st])
        kb.op("dve", lambda e: e.tensor_tensor(ysb[:], ysb[:], bc(st[:, 0:4].unsqueeze(2), [128, 4, 128]), ALU.subtract), reads=[ysb, st], writes=[ysb])
        kb.op("dve", lambda e: e.tensor_tensor(ysq[:], ysb[:], ysb[:], ALU.mult), reads=[ysb], writes=[ysq])
        kb.op("dve", lambda e: e.tensor_reduce(st[:, 4:8], ysq[:], AX.X, ALU.add), reads=[ysq], writes=[st])
        kb.op("act", lambda e: e.activation(st[:, 4:8], st[:, 4:8], AF.Sqrt, bias=cx.gn_eps_col[:, 0:1], scale=1.0 / 128), reads=[st, cx.gn_eps_col], writes=[st])
        kb.op("dve", lambda e: e.reciprocal(st[:, 4:8], st[:, 4:8]), reads=[st], writes=[st])
        kb.op("dve", lambda e: e.tensor_tensor(ysb[:], ysb[:], bc(st[:, 4:8].unsqueeze(2), [128, 4, 128]), ALU.mult), reads=[ysb, st], writes=[ysb])
        kb.op("dve", lambda e: e.tensor_tensor(ysq[:].rearrange("p h e -> p (h e)"), ysb[:].rearrange("p h e -> p (h e)"), gnw[:], ALU.mult), reads=[ysb, gnw], writes=[ysq])
        kb.op("dve", lambda e: e.tensor_tensor(mixb[:], ysq[:].rearrange("p h e -> p (h e)"), sg[:], ALU.mult), reads=[ysq, sg], writes=[mixb])
        ptr = ps[3]
        for h in range(4):
            kb.op("pe", lambda e: e.transpose(ptr[:].bitcast(BF16)[:, h * 128:(h + 1) * 128], mixb[:, h * 128:(h + 1) * 128], identb[:]),
                  reads=[mixb, identb], writes=[ptr])
        mo = mixT_sb[n % 2]
        kb.op("act", lambda e: e.activation(mo[:].rearrange("p h t -> p (h t)"), ptr[:].bitcast(BF16)[:, 0:512], AF.Copy), reads=[ptr], writes=[mo])
        kb.dma(MIXT[0:512, tsl].rearrange("(c p) t -> p c t", p=128), mo[:], reads=[mo], writes=[MIXT])


class NaProj:
    def __init__(self, kb, cx, w_in, qn_w, kn_w):
        self.kb, self.cx = kb, cx
        self.w_in, self.qn_w, self.kn_w = w_in, qn_w, kn_w
        self.wb = kb.sb([128, 8, 1536], BF16, "w_na")
        self.k = 0

    def load_weights(self):
        kb = self.kb
        wv = self.w_in.rearrange("(c p) n -> p c n", p=128)
        for g in range(3):
            for cc in range(0, 8, 2):
                kb.load_cast(self.wb[:, cc:cc + 2, g * 512:(g + 1) * 512], wv[:, cc:cc + 2, 2048 + g * 512:2048 + (g + 1) * 512], self.wb, engs=("act", "pool", "dve"))

    def alloc(self):
        kb = self.kb
        nw = kb.sb([128, 2, 64], F32, "na_nw")
        kb.dma(nw[:, 0, :], self.qn_w.partition_broadcast(128), writes=[nw])
        kb.dma(nw[:, 1, :], self.kn_w.partition_broadcast(128), writes=[nw])
        kb.op("dve", lambda e: e.tensor_scalar(nw[:, 0, :], nw[:, 0, :], 0.125, None, ALU.mult), reads=[nw], writes=[nw])
        self.nw = nw
        self.sq = kb.sb([128, 8, 64], F32, "na_sq")
        self.st = kb.sb([128, 8], F32, "na_st")
        self.tmb = [kb.sb([128, 8, 64], BF16, "na_tm%d" % i) for i in range(2)]
        self.outT = [kb.sb([64, 8, 128], BF16, "na_oT%d" % i) for i in range(2)]
        self.vext = [kb.sb([64, 8, 65], BF16, "na_vx%d" % i) for i in range(2)]
        for i in range(2):
            kb.op("dve", lambda e: e.memset(self.vext[i][:], 1.0), writes=[self.vext[i]])

    def tile(self, n, hT, NQT, NKT, NV, banks):
        kb, cx, wb, nw, sq, st = self.kb, self.cx, self.wb, self.nw, self.sq, self.st
        identb = cx.ident_bf
        tsl = slice(n * 128, (n + 1) * 128)
        for gi, DST in ((0, NQT), (1, NKT)):
            pp = banks[gi]
            for kc in range(8):
                kb.op("pe", lambda e: e.matmul(pp[:], hT[:, kc, tsl], wb[:, kc, gi * 512:(gi + 1) * 512], start=(kc == 0), stop=(kc == 7)), reads=[hT, wb], writes=[pp])
            ppv = pp[:].rearrange("p (h d) -> p h d", h=8)
            kb.op("act", lambda e: e.activation(sq[:], ppv, AF.Square), reads=[pp], writes=[sq])
            kb.op("dve", lambda e: e.tensor_reduce(st[:], sq[:], AX.X, ALU.add), reads=[sq], writes=[st])
            kb.op("act", lambda e: e.activation(st[:], st[:], AF.Sqrt, bias=cx.eps_col[:, 0:1], scale=1.0 / 64), reads=[st, cx.eps_col], writes=[st])
            kb.op("dve", lambda e: e.reciprocal(st[:], st[:]), reads=[st], writes=[st])
            kb.op("dve", lambda e: e.tensor_tensor(sq[:], ppv, bc(st[:].unsqueeze(2), [128, 8, 64]), ALU.mult), reads=[pp, st], writes=[sq])
            tm = self.tmb[gi]
            kb.op("dve", lambda e: e.tensor_tensor(tm[:], sq[:], bc(nw[:, gi:gi + 1, :], [128, 8, 64]), ALU.mult), reads=[sq, nw], writes=[tm])
            ptr = banks[2]
            for h in range(8):
                kb.op("pe", lambda e: e.transpose(ptr[:].bitcast(BF16)[0:64, h * 128:(h + 1) * 128], tm[:, h, :], identb[:]), reads=[tm, identb], writes=[ptr])
            oT = self.outT[gi]
            kb.op("act", lambda e: e.activation(oT[:].rearrange("p h t -> p (h t)"), ptr[:].bitcast(BF16)[0:64, :], AF.Copy), reads=[ptr], writes=[oT])
            kb.dma(DST[:, :, tsl].rearrange("h d t -> d h t"), oT[:], reads=[oT], writes=[DST])
        for sub in range(2):
            pp = banks[sub]
            t0 = n * 128 + sub * 64
            for kc in range(8):
                kb.op("pe", lambda e: e.matmul(pp[0:64, :], hT[:, kc, t0:t0 + 64], wb[:, kc, 1024:1536], start=(kc == 0), stop=(kc == 7)), reads=[hT, wb], writes=[pp])
            vx = self.vext[self.k % 2]; self.k += 1
            kb.op("act", lambda e: e.activation(vx[:, :, 0:64], pp[0:64, :].rearrange("p (h d) -> p h d", h=8), AF.Copy), reads=[pp], writes=[vx])
            kb.dma(NV[2 * n + sub], vx[:].rearrange("p h d -> p (h d)"), reads=[vx], writes=[NV])


def phase_na_proj(kb, cx, hT, w_in, qn_w, kn_w, NQT, NKT, NV):
    na = NaProj(kb, cx, w_in, qn_w, kn_w)
    with kb.scope():
        kb.set_stage(8, 1024)
        na.load_weights()
    na.alloc()
    for n in range(18):
        na.tile(n, hT, NQT, NKT, NV, [cx.psum[0], cx.psum[1], cx.psum[2]])


def phase_na_attn(kb, cx, NQT, NKT, NV, consts, MIXT):
    ps = cx.psum
    identb = cx.ident_bf
    qT = kb.sb([64, 8, NTOK], BF16, "naqT")
    kT = kb.sb([64, 8, NTOK], BF16, "nakT")
    nv = kb.sb([64, 36, 520], BF16, "nanv")
    for h in range(8):
        kb.dma(qT[:, h, :], NQT[h], reads=[NQT], writes=[qT])
        kb.dma(kT[:, h, :], NKT[h], reads=[NKT], writes=[kT])
    for g in range(0, 36, 6):
        kb.dma(nv[:, g:g + 6, :], NV[g:g + 6].rearrange("n p f -> p n f"), reads=[NV], writes=[nv])
    E = kb.sb([64, 15, 512], F32, "naE")
    cm = kb.sb([64, 64], F32, "nacm")
    kb.dma(cm[:], consts["na_colmask"][:], writes=[cm])
    for ro in range(15):
        kb.dma(E[:, ro, :], consts["na_G"][ro], writes=[E])
    for ro in range(15):
        kb.op("act", lambda e: e.activation(E[:, ro, :], E[:, ro, :], AF.Exp), reads=[E], writes=[E])
        kb.op("dve", lambda e: e.tensor_tensor(E[:, ro, :].rearrange("p (h q) -> p h q", h=8), E[:, ro, :].rearrange("p (h q) -> p h q", h=8),
                                               bc(cm[:].unsqueeze(1), [64, 8, 64]), ALU.mult), reads=[E, cm], writes=[E])
    pf = [kb.sb([64, 512], F32, "napf%d" % i) for i in range(2)]
    pball = [kb.sb([64, 12, 512], BF16, "napb%d" % i) for i in range(2)]
    rc = kb.sb([64, 8], F32, "narc")
    ob = kb.sb([64, 8, 64], BF16, "naob")
    oT = [kb.sb([128, 4, 64], BF16, "naoT%d" % i) for i in range(2)]
    pss_l = [ps[2], ps[3], ps[4], ps[7]]
    cnt = [0]

    def keys_of(qt):
        if qt < 4:
            return [(kt, None) for kt in range(4)]
        r = qt - 4
        r0 = min(max(r - 4, 0), 24)
        return [(kt, None) for kt in range(4)] + [(4 + r0 + j_, r0 + j_ - r + 7) for j_ in range(8)]

    def S1(qt):
        q0 = qt * 64
        P_ = pball[qt % 2]
        for ki, (kt, ro) in enumerate(keys_of(qt)):
            k0 = kt * 64
            pss = pss_l[cnt[0] % 4]
            for h in range(8):
                kb.op("pe", lambda e: e.matmul(pss[0:64, h * 64:(h + 1) * 64], kT[:, h, k0:k0 + 64], qT[:, h, q0:q0 + 64], start=True, stop=True),
                      reads=[kT, qT], writes=[pss])
            if ro is None:
                kb.op("act", lambda e: e.activation(P_[:, ki, :], pss[0:64, :], AF.Exp), reads=[pss], writes=[P_])
            else:
                p_f = pf[cnt[0] % 2]
                kb.op("act", lambda e: e.activation(p_f[:], pss[0:64, :], AF.Exp), reads=[pss], writes=[p_f])
                kb.op("dve", lambda e: e.tensor_tensor(P_[:, ki, :], p_f[:], E[:, ro, :], ALU.mult), reads=[p_f, E], writes=[P_])
            cnt[0] += 1

    def S2(qt):
        q0 = qt * 64
        P_ = pball[qt % 2]
        keys = keys_of(qt)
        accA, accB = ps[0], ps[1]
        for h in range(8):
            acc = accA if h < 4 else accB
            hh = h % 4
            for ki, (kt, ro) in enumerate(keys):
                kb.op("pe", lambda e: e.matmul(acc[0:64, hh * 65:(hh + 1) * 65], P_[:, ki, h * 64:(h + 1) * 64], nv[:, kt, h * 65:(h + 1) * 65],
                                               start=(ki == 0), stop=(ki == len(keys) - 1)), reads=[P_, nv], writes=[acc])
        for half, acc in ((0, accA), (1, accB)):
            av = acc[0:64, 0:260].rearrange("p (h d) -> p h d", h=4)
            kb.op("dve", lambda e: e.reciprocal(rc[:, half * 4:(half + 1) * 4], av[:, :, 64]), reads=[acc], writes=[rc])
            kb.op("dve", lambda e: e.tensor_tensor(ob[:, half * 4:(half + 1) * 4, :], av[:, :, 0:64], bc(rc[:, half * 4:(half + 1) * 4].unsqueeze(2), [64, 4, 64]), ALU.mult),
                  reads=[acc, rc], writes=[ob])
        ptr = ps[5 + (qt % 2)]
        obv = ob[:].rearrange("p h d -> p (h d)")
        for c in range(4):
            kb.op("pe", lambda e: e.transpose(ptr[:].bitcast(BF16)[:, c * 64:(c + 1) * 64], obv[:, c * 128:(c + 1) * 128], identb[0:64, 0:64]), reads=[ob, identb], writes=[ptr])
        o = oT[qt % 2]
        kb.op("act", lambda e: e.activation(o[:].rearrange("p c t -> p (c t)"), ptr[:].bitcast(BF16)[:, 0:256], AF.Copy), reads=[ptr], writes=[o])
        kb.dma(MIXT[512:1024, q0:q0 + 64].rearrange("(c p) t -> p c t", p=128), o[:], reads=[o], writes=[MIXT])
    S1(0)
    for qt in range(36):
        if qt + 1 < 36:
            S1(qt + 1)
        S2(qt)


def phase_wout(kb, cx, L, w_out, MIXT, XT_in, XT_out, gate_idx=2, tiles=TILES):
    ps = cx.psum
    wb = kb.sb([128, 8, 1024], BF16, "w_out")
    wv = w_out.rearrange("(c p) n -> p c n", p=128)
    with kb.scope():
        kb.set_stage(8, 1024)
        for cc in range(8):
            kb.load_cast(wb[:, cc, :], wv[:, cc, :], wb, engs=("act", "pool", "dve"))
    mx = [kb.sb([128, 8, 512], BF16, "wo_mx%d" % i) for i in range(2)]
    NX = 10
    xb = [kb.sb([128, 512], F32, "wo_x%d" % i) for i in range(NX)]
    its = []
    for ti, (t0, n, is_ctx) in enumerate(tiles):
        for dc in range(8):
            its.append((ti, t0, n, 1 if is_ctx else 0, dc))
    LOOK = NX - 2

    def ldm(ti):
        t0, n, _ = tiles[ti]
        kb.dma(mx[ti % 2][:, :, 0:n], fm(MIXT[:], t0, n), reads=[MIXT], writes=[mx[ti % 2]])

    def ldx(k):
        ti, t0, n, s_, dc = its[k]
        x = xb[k % NX]
        kb.dma(x[:, 0:n], XT_in[dc * 128:(dc + 1) * 128, t0:t0 + n], reads=[XT_in], writes=[x])
    ldm(0)
    if len(tiles) > 1:
        ldm(1)
    for k in range(min(LOOK, len(its))):
        ldx(k)
    for k in range(len(its)):
        ti, t0, n, s_, dc = its[k]
        m = mx[ti % 2]
        x = xb[k % NX]
        po = ps[k % 8]
        for kc in range(8):
            kb.op("pe", lambda e: e.matmul(po[:, 0:n], wb[:, kc, dc * 128:(dc + 1) * 128], m[:, kc, 0:n], start=(kc == 0), stop=(kc == 7)), reads=[wb, m], writes=[po])
        kb.op("dve", lambda e: e.scalar_tensor_tensor(x[:, 0:n], po[:, 0:n], mod_vec(cx, L, s_, gate_idx)[:, dc:dc + 1], x[:, 0:n], ALU.mult, ALU.add),
              reads=[po, cx.mod, x], writes=[x])
        kb.dma(XT_out[dc * 128:(dc + 1) * 128, t0:t0 + n], x[:, 0:n], reads=[x], writes=[XT_out])
        if k + LOOK < len(its):
            ldx(k + LOOK)
        if dc == 7 and ti + 2 < len(tiles):
            ldm(ti + 2)


def phase_gates(kb, cx, hT, hT_t0, router, gates_all):
    ps = cx.psum
    rb = kb.sb([128, 8, 8], BF16, "router_bf")
    kb.set_stage(1, 64)
    with kb.nc.allow_non_contiguous_dma(reason="tiny"):
        kb.load_cast(rb[:], router.rearrange("(c p) e -> p c e", p=128), rb)
    NT = 16
    lg = kb.sb([128, NT, 8], F32, "g_lg")
    l2 = kb.sb([128, NT, 8], F32, "g_l2")
    m1 = kb.sb([128, NT, 8], F32, "g_m1")
    m2 = kb.sb([128, NT, 8], F32, "g_m2")
    v1 = kb.sb([128, NT], F32, "g_v1")
    v2 = kb.sb([128, NT], F32, "g_v2")
    w1 = kb.sb([128, NT], F32, "g_w1")
    w2 = kb.sb([128, NT], F32, "g_w2")
    gt = kb.sb([128, NT, 8], F32, "g_gt")
    dg = [kb.sb([128, 8, 128], F32, "g_dg%d" % i) for i in range(2)]
    for n in range(NT):
        t0 = 256 + n * 128 - hT_t0
        pl = ps[n % 2]
        for kc in range(8):
            kb.op("pe", lambda e: e.matmul(pl[:, 0:8], hT[:, kc, t0:t0 + 128], rb[:, kc, :], start=(kc == 0), stop=(kc == 7)), reads=[hT, rb], writes=[pl])
        kb.op("act", lambda e: e.activation(lg[:, n, :], pl[:, 0:8], AF.Copy), reads=[pl], writes=[lg])

    def b3(x):
        return bc(x[:].unsqueeze(2), [128, NT, 8])
    kb.op("dve", lambda e: e.tensor_reduce(v1[:], lg[:], AX.X, ALU.max), reads=[lg], writes=[v1])
    kb.op("dve", lambda e: e.tensor_tensor(m1[:], lg[:], b3(v1), ALU.is_equal), reads=[lg, v1], writes=[m1])
    kb.op("dve", lambda e: e.scalar_tensor_tensor(l2[:], m1[:], -1e30, lg[:], ALU.mult, ALU.add), reads=[m1, lg], writes=[l2])
    kb.op("dve", lambda e: e.tensor_reduce(v2[:], l2[:], AX.X, ALU.max), reads=[l2], writes=[v2])
    kb.op("dve", lambda e: e.tensor_tensor(m2[:], l2[:], b3(v2), ALU.is_equal), reads=[l2, v2], writes=[m2])
    kb.op("dve", lambda e: e.tensor_tensor(w1[:], v2[:], v1[:], ALU.subtract), reads=[v1, v2], writes=[w1])
    kb.op("act", lambda e: e.activation(w1[:], w1[:], AF.Exp), reads=[w1], writes=[w1])
    kb.op("dve", lambda e: e.tensor_scalar(w1[:], w1[:], 1.0, None, ALU.add), reads=[w1], writes=[w1])
    kb.op("dve", lambda e: e.reciprocal(w1[:], w1[:]), reads=[w1], writes=[w1])
    kb.op("dve", lambda e: e.tensor_scalar(w2[:], w1[:], -1.0, 1.0, ALU.mult, ALU.add), reads=[w1], writes=[w2])
    kb.op("dve", lambda e: e.tensor_tensor(m1[:], m1[:], b3(w1), ALU.mult), reads=[m1, w1], writes=[m1])
    kb.op("dve", lambda e: e.tensor_tensor(m2[:], m2[:], b3(w2), ALU.mult), reads=[m2, w2], writes=[m2])
    kb.op("dve", lambda e: e.tensor_tensor(gt[:], m1[:], m2[:], ALU.add), reads=[m1, m2], writes=[gt])
    for n in range(NT):
        d = dg[n % 2]
        kb.op("dve", lambda e: e.tensor_tensor(d[:], bc(cx.ident_f32[:].unsqueeze(1), [128, 8, 128]), bc(gt[:, n, :].unsqueeze(2), [128, 8, 128]), ALU.mult),
              reads=[gt, cx.ident_f32], writes=[d])
        for half in range(2):
            pg = ps[2 + (2 * n + half) % 4]
            kb.op("pe", lambda e: e.matmul(pg[:], cx.ones_f32[:], d[:, half * 4:(half + 1) * 4, :], start=True, stop=True), reads=[d, cx.ones_f32], writes=[pg])
            kb.op("act", lambda e: e.activation(gates_all[:, half * 4:(half + 1) * 4, n * 128:(n + 1) * 128], pg[:].rearrange("p (e t) -> p e t", e=4), AF.Copy),
                  reads=[pg], writes=[gates_all])


W_NAMES = ["ada_w", "ada_b", "norm_mix_w", "norm_ffn_w", "ev_w_in", "ev_ret_decay_f", "ev_ret_decay_b", "ev_ret_gn_w", "ev_na_qn_w",
           "ev_na_kn_w", "ev_w_out", "ev_ffn_w13", "ev_ffn_w2", "od_router", "od_moe_w13", "od_moe_w2"]


def build_program(shapes, const_shapes):
    kb = KB(); cx = Ctx()

    def ein(name, shape):
        return kb.dram(name, list(shape), F32, "ExternalInput")
    c_lat = ein("c_lat", [1024]); c_ctx = ein("c_ctx", [1024])
    w = {k: ein(k, shapes[k]) for k in W_NAMES + RW_NAMES}
    consts = {k: ein(k, v) for k, v in const_shapes.items()}
    XT0 = ein("xT", [1024, NTOK])
    outT = kb.dram("outT", [1024, NLAT], F32, "ExternalOutput")
    XT1 = kb.dram("XT1", [1024, NTOK], F32)
    MIXT = kb.dram("MIXT", [1024, NTOK], BF16)
    NQT = kb.dram("NQT", [8, 64, NTOK], BF16); NKT = kb.dram("NKT", [8, 64, NTOK], BF16); NV = kb.dram("NV", [36, 64, 520], BF16)
    setup_common(kb, cx, consts)
    phase_mod_scoped(kb, cx, c_lat, c_ctx, w["ada_w"], w["ada_b"])
    with kb.scope():
        hT = kb.sb([128, 8, NTOK], BF16, "hT")
        with kb.scope():
            setup_modbuf(kb, cx)
            phase_modulate(kb, cx, XT0, 0, w["norm_mix_w"][0], 0, 1, hT)
        with kb.scope():
            phase_retention(kb, cx, hT, w["ev_w_in"][0], w["ev_ret_decay_f"][0], w["ev_ret_decay_b"][0], w["ev_ret_gn_w"][0], consts, MIXT,
                            na_args=dict(qn_w=w["ev_na_qn_w"][0], kn_w=w["ev_na_kn_w"][0], NQT=NQT, NKT=NKT, NV=NV))
    with kb.scope():
        phase_na_attn(kb, cx, NQT, NKT, NV, consts, MIXT)
    with kb.scope():
        phase_wout(kb, cx, 0, w["ev_w_out"][0], MIXT, XT0, XT1)
    with kb.scope():
        hT = kb.sb([128, 8, NTOK], BF16, "hT")
        with kb.scope():
            setup_modbuf(kb, cx)
            phase_modulate(kb, cx, XT1, 0, w["norm_ffn_w"][0], 3, 4, hT)
        with kb.scope():
            setup_ffn(kb, cx, NTOK)
            phase_ffn(kb, cx, XT1, 0, hT, 0, TILES, [w["ev_ffn_w13"][0]], [w["ev_ffn_w2"][0]], 2816, 5)
    XT2 = kb.dram("XT2", [1024, NTOK], F32)
    layer1_mixer(kb, cx, w, consts, XT1, XT2, MIXT)
    LT = TILES[1:]
    with kb.scope():
        hT = kb.sb([128, 8, NLAT], BF16, "hT")
        with kb.scope():
            setup_modbuf(kb, cx)
            phase_modulate(kb, cx, XT2, 1, w["norm_ffn_w"][1], 3, 4, hT, tiles=LT, hT_t0=256)
        gates_all = kb.sb([128, 8, NLAT], BF16, "gates_all")
        with kb.scope():
            phase_gates(kb, cx, hT, 256, w["od_router"][0], gates_all)
        gl = [T(gates_all.t[:, :, i * 512:(i + 1) * 512]) for i in range(4)]
        for g_ in gl:
            g_.ws = gates_all.ws; g_.rs = gates_all.rs
        with kb.scope():
            setup_ffn(kb, cx, NLAT)
            phase_ffn(kb, cx, XT2, 1, hT, 256, LT, [w["od_moe_w13"][0][e] for e in range(8)], [w["od_moe_w2"][0][e] for e in range(8)], 3584, 5, gates=gl)
    kb.dma(outT[:], XT2[:, 256:NTOK], reads=[XT2], writes=[outT])
    kb.finish()
    return kb


def host_consts():
    c = {}
    c["ident"] = np.eye(128, dtype=np.float32)
    half = 32
    freqs = (10000.0 ** (-np.arange(half, dtype=np.float32) / half)).astype(np.float32)
    t = np.arange(2048)
    rpos = (t // 64).astype(np.float32); cpos = (t % 64).astype(np.float32)
    cos = np.ones((2304, 128), np.float32); sin = np.zeros((2304, 128), np.float32)
    for blk, pos in ((0, rpos), (1, cpos)):
        ang = pos[:, None] * freqs[None, :]
        cs, sn = np.cos(ang).astype(np.float32), np.sin(ang).astype(np.float32)
        cos[256:, blk * 64:blk * 64 + 32] = cs; cos[256:, blk * 64 + 32:blk * 64 + 64] = cs
        sin[256:, blk * 64:blk * 64 + 32] = -sn; sin[256:, blk * 64 + 32:blk * 64 + 64] = sn
    c["rope_cos"] = cos; c["rope_sin"] = sin
    s = np.arange(128, dtype=np.float32)[:, None]; tt = np.arange(128, dtype=np.float32)[None, :]
    d = tt - s
    c["ret_cst"] = np.stack([d, np.maximum(d, 0), np.maximum(-d, 0), (d >= 0).astype(np.float32), (d <= 0).astype(np.float32)]).astype(np.float32)
    c["kidx128"] = (128.0 * np.arange(18)).astype(np.float32)
    return c


def na_tables(rpb):
    kc = np.arange(64)[:, None]; q = np.arange(64)[None, :]
    idx = np.clip(kc - q + 15, 0, 30)
    G = rpb[:, :, idx]
    G = np.ascontiguousarray(np.transpose(G, (1, 2, 0, 3))).reshape(15, 64, 512).astype(np.float32)
    cs = np.clip(np.arange(64) - 8, 0, 48)
    cm = ((kc >= cs[None, :]) & (kc < cs[None, :] + 16)).astype(np.float32)
    return G, cm


_PROG_CACHE = {}


def kernel(**inputs):
    inp = {k: np.ascontiguousarray(np.asarray(v, dtype=np.float32)) for k, v in inputs.items()}
    hc = host_consts()
    G, cm = na_tables(inp["ev_na_rpb"][0])
    hc["na_G"] = G
    hc["na_colmask"] = cm
    hc.update(rwkv_consts())
    shapes = {k: inp[k].shape for k in W_NAMES + RW_NAMES}
    cshapes = {k: v.shape for k, v in hc.items()}
    key = "prog"
    kb = build_program(shapes, cshapes)
    n = 8
    in_maps = []
    for b in range(n):
        m = {"c_lat": inp["c"][b], "c_ctx": inp["c_ctx"],
             "xT": np.ascontiguousarray(np.concatenate([inp["ctx"][b], inp["x"][b]], 0).T)}
        for k in W_NAMES + RW_NAMES:
            m[k] = inp[k]
        m.update(hc)
        in_maps.append(m)
    res = run_bass_kernel_spmd(kb.nc, in_maps, core_ids=list(range(n)))
    out = np.stack([np.ascontiguousarray(res.results[b]["outT"].T) for b in range(n)], 0)
    return out.astype(np.float32)


DEC_C = 0.6065306597126334
NCH = NTOK // 64


class BankRR:
    def __init__(self, banks):
        self.b = banks
        self.i = 0

    def __call__(self):
        p = self.b[self.i % len(self.b)]
        self.i += 1
        return p


def rwkv_weights_alloc(kb):
    Wd = {}
    Wd["W3"] = kb.sb([128, 3, 8, 1024], BF16, "rw_W3")
    Wd["w1b"] = kb.sb([128, 2, 8, 64], BF16, "rw_w1")
    Wd["a1b"] = kb.sb([128, 2, 8, 64], BF16, "rw_a1")
    Wd["w2b"] = kb.sb([64, 2, 1024], BF16, "rw_w2")
    Wd["a2b"] = kb.sb([64, 2, 1024], BF16, "rw_a2")
    Wd["g1b"] = kb.sb([128, 8, 160], BF16, "rw_g1")
    Wd["g2b"] = kb.sb([128, 1024], BF16, "rw_g2")
    Wd["g2c"] = kb.sb([32, 1024], BF16, "rw_g2c")
    return Wd


def rwkv_weights_load(kb, Wd, w):
    W3, w1b, a1b, w2b, a2b, g1b, g2b, g2c = (Wd[k_] for k_ in ("W3", "w1b", "a1b", "w2b", "a2b", "g1b", "g2b", "g2c"))
    for s in range(3):
        wv = w["od_w_rkv"][0][s].rearrange("(c p) n -> p c n", p=128)
        for cc in range(8):
            kb.load_cast(W3[:, s, cc, :], wv[:, cc, :], W3, engs=("act", "pool", "dve"))
            if cc % 2 == 1:
                yield
    for z in range(2):
        kb.load_cast(w1b[:, z, :, :], w["od_w1"][0][z].rearrange("(c p) r -> p c r", p=128), w1b)
        kb.load_cast(a1b[:, z, :, :], w["od_a1"][0][z].rearrange("(c p) r -> p c r", p=128), a1b)
        kb.load_cast(w2b[:, z, :], w["od_w2"][0][z], w2b)
        kb.load_cast(a2b[:, z, :], w["od_a2"][0][z], a2b)
        yield
    for hf in range(2):
        kb.load_cast(g1b[:, hf * 4:(hf + 1) * 4, :], w["od_g1"][0].rearrange("(c p) r -> p c r", p=128)[:, hf * 4:(hf + 1) * 4, :], g1b)
    kb.load_cast(g2b[:], w["od_g2"][0][0:128, :], g2b)
    kb.load_cast(g2c[:], w["od_g2"][0][128:160, :], g2c)
    yield


def phase_rwkv_feat(kb, cx, HT, w, consts, D_, Wd):
    nb = BankRR(cx.psum)
    W3, w1b, a1b, w2b, a2b, g1b, g2b, g2c = (Wd[k_] for k_ in ("W3", "w1b", "a1b", "w2b", "a2b", "g1b", "g2b", "g2c"))
    mu = kb.sb([128, 6, 8], F32, "rw_mu")
    with kb.nc.allow_non_contiguous_dma(reason="tiny"):
        for s in range(6):
            kb.dma(mu[:, s, :], w["od_mu"][0][s].rearrange("(c p) -> p c", p=128), writes=[mu])
    tabs = kb.sb([128, 7, 1024], F32, "rw_tabs")
    srcs = [w["od_w0"][0][0], w["od_w0"][0][1], w["od_a0"][0][0], w["od_a0"][0][1], w["od_k_k"][0], w["od_k_a"][0], w["od_r_k"][0]]
    for i, s_ in enumerate(srcs):
        kb.dma(tabs[:, i, :], s_.partition_broadcast(128), writes=[tabs])
    CM = kb.sb([128, 5, 128], F32, "rw_CM")
    kb.dma(CM[:], consts["rw_CM"][:].rearrange("k s t -> s k t"), writes=[CM])
    tiny = kb.sb([128, 1], F32, "rw_tiny")
    kb.op("dve", lambda e: e.memset(tiny[:], 1e-24), writes=[tiny])
    xx = kb.sb([128, 8, 128], F32, "rw_xx")
    XM = [[kb.sb([128, 8, 128], BF16, "rw_xm%d_%d" % (i, s)) for s in range(6)] for i in range(2)]
    HID = [(kb.sb([64, 2, 128], BF16, "rw_hw%d" % i), kb.sb([64, 2, 128], BF16, "rw_ha%d" % i),
            kb.sb([128, 128], BF16, "rw_hg%d" % i), kb.sb([32, 128], BF16, "rw_hg2%d" % i)) for i in range(2)]

    FW = 512
    NHU = FW // 64

    def mkws(tag):
        d = {}
        for nm in ("r_sb", "k_sb", "v_sb", "kk", "kkn", "tmp", "tmp2", "tmp3", "sig0", "sig1", "asg0", "asg1", "kd0", "kd1", "bz"):
            d[nm] = kb.sb([128, FW], F32, "rw_%s_%s" % (nm, tag))
        d["st8"] = kb.sb([128, NHU], F32, "rw_st_%s" % tag)
        return d
    WSS = [mkws("a")]
    OBL = [kb.sb([128, 9, FW], BF16, "rw_OB%d" % i) for i in range(2)]
    OFL = [kb.sb([128, 4, FW], F32, "rw_OF%d" % i) for i in range(2)]
    ucnt = [0]

    class _V:
        def __init__(self, tile, slot):
            self.tile, self.slot = tile, slot

        def __getitem__(self, k):
            return self.tile.t[:, self.slot, :][k]

    hloc_l = [kb.sb([128, 8, 130], BF16, "rw_hloc%d" % i) for i in range(2)]

    def prologue(n):
        xm = XM[n % 2]
        hw_b, ha_b, hg_b, hg2_b = HID[n % 2]
        t0 = n * 128
        tsl = slice(t0, t0 + 128)
        left_b = (t0 == 0 or t0 == 256)
        right_b = (t0 + 128 == 256 or t0 + 128 == NTOK)
        lo = 1 if left_b else 0
        hi = 127 if right_b else 128
        hloc = hloc_l[n % 2]
        g0 = t0 if left_b else t0 - 1
        g1 = t0 + 128 if right_b else t0 + 129
        kb.dma(hloc[:, :, g0 - (t0 - 1):g1 - (t0 - 1)], fm(HT[:], g0, g1 - g0), reads=[HT], writes=[hloc])
        kb.op("dve", lambda e: e.tensor_tensor(xx[:, :, lo:hi], hloc[:, :, lo:hi], hloc[:, :, lo + 2:hi + 2], ALU.add), reads=[hloc], writes=[xx])
        if left_b:
            kb.op("dve", lambda e: e.tensor_copy(xx[:, :, 0:1], hloc[:, :, 2:3]), reads=[hloc], writes=[xx])
        if right_b:
            kb.op("dve", lambda e: e.tensor_copy(xx[:, :, 127:128], hloc[:, :, 127:128]), reads=[hloc], writes=[xx])
        kb.op("dve", lambda e: e.tensor_scalar(xx[:], xx[:], 0.5, None, ALU.mult), reads=[xx], writes=[xx])
        kb.op("dve", lambda e: e.tensor_tensor(xx[:], xx[:], hloc[:, :, 1:129], ALU.subtract), reads=[xx, hloc], writes=[xx])
        for s in range(6):
            eng = "dve" if s % 2 == 0 else "pool"
            kb.op(eng, lambda e: e.tensor_tensor(xm[s][:], xx[:], bc(mu[:, s, :].unsqueeze(2), [128, 8, 128]), ALU.mult), reads=[xx, mu], writes=[xm[s]])
            kb.op(eng, lambda e: e.tensor_tensor(xm[s][:], xm[s][:], hloc[:, :, 1:129], ALU.add), reads=[xm[s], hloc], writes=[xm[s]])
            yield
        ph = nb()
        for z in range(2):
            for kc in range(8):
                kb.op("pe", lambda e: e.matmul(ph[0:64, z * 128:(z + 1) * 128], w1b[:, z, kc, :], xm[3][:, kc, :], start=(kc == 0), stop=(kc == 7)), reads=[w1b, xm[3]], writes=[ph])
        kb.op("act", lambda e: e.activation(hw_b[:].rearrange("p z t -> p (z t)"), ph[0:64, 0:256], AF.Tanh), reads=[ph], writes=[hw_b])
        yield
        ph = nb()
        for z in range(2):
            for kc in range(8):
                kb.op("pe", lambda e: e.matmul(ph[0:64, z * 128:(z + 1) * 128], a1b[:, z, kc, :], xm[4][:, kc, :], start=(kc == 0), stop=(kc == 7)), reads=[a1b, xm[4]], writes=[ph])
        kb.op("act", lambda e: e.activation(ha_b[:].rearrange("p z t -> p (z t)"), ph[0:64, 0:256], AF.Copy), reads=[ph], writes=[ha_b])
        yield
        ph = nb()
        for kc in range(8):
            kb.op("pe", lambda e: e.matmul(ph[:, 0:128], g1b[:, kc, 0:128], xm[5][:, kc, :], start=(kc == 0), stop=(kc == 7)), reads=[g1b, xm[5]], writes=[ph])
        kb.op("act", lambda e: e.activation(hg_b[:], ph[:, 0:128], AF.Sigmoid), reads=[ph], writes=[hg_b])
        yield
        ph = nb()
        for kc in range(8):
            kb.op("pe", lambda e: e.matmul(ph[0:32, 0:128], g1b[:, kc, 128:160], xm[5][:, kc, :], start=(kc == 0), stop=(kc == 7)), reads=[g1b, xm[5]], writes=[ph])
        kb.op("act", lambda e: e.activation(hg2_b[:], ph[0:32, 0:128], AF.Sigmoid), reads=[ph], writes=[hg2_b])
        yield

    def units(n):
        t0 = n * 128
        tsl = slice(t0, t0 + 128)
        xm = XM[n % 2]
        hw_b, ha_b, hg_b, hg2_b = HID[n % 2]
        def unit(q, WS):
            r_sb, k_sb, v_sb, kk, kkn, tmp, tmp2, tmp3, bz, st8 = (WS[k_] for k_ in ('r_sb', 'k_sb', 'v_sb', 'kk', 'kkn', 'tmp', 'tmp2', 'tmp3', 'bz', 'st8'))
            sig = [WS['sig0'], WS['sig1']]; asg = [WS['asg0'], WS['asg1']]; kd = [WS['kd0'], WS['kd1']]
            fsl = slice(q * FW, (q + 1) * FW)
            OB = OBL[ucnt[0] % 2]
            OF = OFL[ucnt[0] % 2]
            ucnt[0] += 1
            for s, dst in ((0, r_sb), (1, k_sb), (2, v_sb)):
                pp = nb()
                for kc in range(8):
                    kb.op("pe", lambda e: e.matmul(pp[:], xm[s][:, kc, :], W3[:, s, kc, fsl], start=(kc == 0), stop=(kc == 7)), reads=[xm[s], W3], writes=[pp])
                kb.op("act", lambda e: e.activation(dst[:], pp[:], AF.Copy), reads=[pp], writes=[dst])
            o = _V(OB, 0)
            kb.op("pool", lambda e: e.tensor_copy(o[:], v_sb[:]), reads=[v_sb], writes=[OB])
            yield
            for z in range(2):
                pp = nb()
                kb.op("pe", lambda e: e.matmul(pp[:], hw_b[:, z, :], w2b[:, z, fsl], start=True, stop=True), reads=[hw_b, w2b], writes=[pp])
                kb.op("dve", lambda e: e.tensor_tensor(sig[z][:], pp[:], tabs[:, z, fsl], ALU.add), reads=[pp, tabs], writes=[sig[z]])
                kb.op("act", lambda e: e.activation(sig[z][:], sig[z][:], AF.Sigmoid), reads=[sig[z]], writes=[sig[z]])
                pp = nb()
                kb.op("pe", lambda e: e.matmul(pp[:], ha_b[:, z, :], a2b[:, z, fsl], start=True, stop=True), reads=[ha_b, a2b], writes=[pp])
                kb.op("dve", lambda e: e.tensor_tensor(asg[z][:], pp[:], tabs[:, 2 + z, fsl], ALU.add), reads=[pp, tabs], writes=[asg[z]])
                kb.op("act", lambda e: e.activation(asg[z][:], asg[z][:], AF.Sigmoid), reads=[asg[z]], writes=[asg[z]])
                yield
            kb.op("dve", lambda e: e.tensor_tensor(kk[:], k_sb[:], tabs[:, 4, fsl], ALU.mult), reads=[k_sb, tabs], writes=[kk])
            kb.op("act", lambda e: e.activation(tmp[:], kk[:], AF.Square), reads=[kk], writes=[tmp])
            kb.op("dve", lambda e: e.tensor_reduce(st8[:], tmp[:].rearrange("p (h j) -> p h j", h=8), AX.X, ALU.add), reads=[tmp], writes=[st8])
            kb.op("act", lambda e: e.activation(st8[:], st8[:], AF.Sqrt, bias=tiny[:, 0:1], scale=1.0), reads=[st8, tiny], writes=[st8])
            kb.op("dve", lambda e: e.reciprocal(st8[:], st8[:]), reads=[st8], writes=[st8])
            kb.op("dve", lambda e: e.tensor_tensor(kkn[:].rearrange("p (h j) -> p h j", h=8), kk[:].rearrange("p (h j) -> p h j", h=8),
                                                   bc(st8[:].unsqueeze(2), [128, 8, 64]), ALU.mult), reads=[kk, st8], writes=[kkn])
            yield
            for z in range(2):
                kb.op("dve", lambda e: e.scalar_tensor_tensor(tmp[:], asg[z][:], -1.0, tabs[:, 5, fsl], ALU.add, ALU.mult), reads=[asg[z], tabs], writes=[tmp])
                kb.op("dve", lambda e: e.scalar_tensor_tensor(kd[z][:], tmp[:], 1.0, k_sb[:], ALU.add, ALU.mult), reads=[tmp, k_sb], writes=[kd[z]])
                kb.op("pool", lambda e: e.tensor_tensor(bz[:], kkn[:], asg[z][:], ALU.mult), reads=[kkn, asg[z]], writes=[bz])
                pci = nb()
                kb.op("pe", lambda e: e.matmul(pci[:], CM[:, 2 * z, :], sig[z][:], start=True, stop=True), reads=[CM, sig[z]], writes=[pci])
                kb.op("act", lambda e: e.activation(tmp2[:], pci[:], AF.Exp, scale=-DEC_C), reads=[pci], writes=[tmp2])
                o = _V(OB, 1 + z)
                kb.op("dve", lambda e: e.tensor_tensor(o[:], r_sb[:], tmp2[:], ALU.mult), reads=[r_sb, tmp2], writes=[OB])
                yield
                kb.op("act", lambda e: e.activation(tmp3[:], pci[:], AF.Exp, scale=DEC_C), reads=[pci], writes=[tmp3])
                o = _V(OB, 3 + z)
                kb.op("dve", lambda e: e.tensor_tensor(o[:], bz[:], tmp3[:], ALU.mult), reads=[bz, tmp3], writes=[OB])
                o = _V(OB, 5 + z)
                kb.op("pool", lambda e: e.tensor_tensor(o[:], kd[z][:], tmp3[:], ALU.mult), reads=[kd[z], tmp3], writes=[OB])
                yield
                pce = nb()
                kb.op("pe", lambda e: e.matmul(pce[:], CM[:, 2 * z + 1, :], sig[z][:], start=True, stop=True), reads=[CM, sig[z]], writes=[pce])
                kb.op("act", lambda e: e.activation(tmp2[:], pce[:], AF.Exp, scale=-DEC_C), reads=[pce], writes=[tmp2])
                o = _V(OB, 7 + z)
                kb.op("dve", lambda e: e.scalar_tensor_tensor(o[:], kkn[:], -1.0, tmp2[:], ALU.mult, ALU.mult), reads=[kkn, tmp2], writes=[OB])
                ptot = nb()
                kb.op("pe", lambda e: e.matmul(ptot[:], CM[:, 4, :], sig[z][:], start=True, stop=True), reads=[CM, sig[z]], writes=[ptot])
                o2 = _V(OF, z)
                kb.op("act", lambda e: e.activation(o2[:], ptot[:], AF.Exp, scale=-DEC_C), reads=[ptot], writes=[OF])
                yield
            kb.op("dve", lambda e: e.tensor_tensor(tmp[:], r_sb[:], tabs[:, 6, fsl], ALU.mult), reads=[r_sb, tabs], writes=[tmp])
            kb.op("pool", lambda e: e.tensor_tensor(tmp2[:], kd[0][:], kd[1][:], ALU.add), reads=[kd[0], kd[1]], writes=[tmp2])
            kb.op("dve", lambda e: e.tensor_tensor(tmp[:], tmp[:], tmp2[:], ALU.mult), reads=[tmp, tmp2], writes=[tmp])
            kb.op("dve", lambda e: e.tensor_reduce(st8[:], tmp[:].rearrange("p (h j) -> p h j", h=8), AX.X, ALU.add), reads=[tmp], writes=[st8])
            o2 = _V(OF, 2)
            kb.op("dve", lambda e: e.tensor_tensor(o2[:].rearrange("p (h j) -> p h j", h=8), v_sb[:].rearrange("p (h j) -> p h j", h=8),
                                                   bc(st8[:].unsqueeze(2), [128, 8, 64]), ALU.mult), reads=[v_sb, st8], writes=[OF])
            yield
            pg = nb()
            kb.op("pe", lambda e: e.matmul(pg[:], hg_b[:], g2b[:, fsl], start=True, stop=False), reads=[hg_b, g2b], writes=[pg])
            kb.op("pe", lambda e: e.matmul(pg[:], hg2_b[:], g2c[:, fsl], start=False, stop=True), reads=[hg2_b, g2c], writes=[pg])
            o2 = _V(OF, 3)
            kb.op("act", lambda e: e.activation(o2[:], pg[:], AF.Copy), reads=[pg], writes=[OF])
            kb.dma(D_["BIG"][tsl, :, fsl], OB[:], reads=[OB], writes=[D_["BIG"]])
            kb.dma(D_["BIGF"][tsl, :, fsl], OF[:], reads=[OF], writes=[D_["BIGF"]])


        for q_ in range(2):
            yield from unit(q_, WSS[0])

    run_gens([prologue(0)])
    for n in range(18):
        gens = [units(n)]
        if n + 1 < 18:
            gens.append(prologue(n + 1))
        run_gens(gens)


class RwkvScan:
    def __init__(self, kb, cx, z, D_, consts):
        self.kb, self.cx, self.z, self.D_ = kb, cx, z, D_
        self.nd = cx.nd
        MK = kb.sb([128, 4, 64], F32, "sc_MK")
        kb.dma(MK[:], consts["rw_MK2"][:].rearrange("k r c -> r k c"), writes=[MK])
        self.MK = MK
        self.I2 = kb.sb([128, 64], F32, "sc_I2")
        kb.dma(self.I2[:], consts["rw_I2"][:], writes=[self.I2])
        Ef = kb.sb([64, 2, 128], F32, "sc_Ef")
        kb.dma(Ef[:], consts["rw_E"][:].rearrange("k r c -> r k c"), writes=[Ef])
        self.E = kb.sb([64, 2, 128], BF16, "sc_E")
        kb.op("dve", lambda e: e.tensor_copy(self.E[:], Ef[:]), reads=[Ef], writes=[self.E])
        if z == 0:
            self.mT_strict, self.mT_incl, self.mL_strict = 0, 1, 2
        else:
            self.mT_strict, self.mT_incl, self.mL_strict = 2, 3, 0

        def b16(name):
            return kb.sb([128, 8, 64], BF16, name)
        self.U0f = kb.sb([128, 8, 64], F32, "sc_U0f")
        self.U0b = b16("sc_U0b")
        kb.op("dve", lambda e: e.memset(self.U0f[:], 0.0), writes=[self.U0f])
        kb.op("dve", lambda e: e.memset(self.U0b[:], 0.0), writes=[self.U0b])
        self.tin = [[kb.sb([64, 1024], BF16, "sc_in%d_%d" % (i, j)) for j in range(5)] for i in range(2)]
        self.etin = [kb.sb([64, 1024], F32, "sc_et%d" % i) for i in range(2)]
        self.yout = kb.sb([128, 8, 64], F32, "sc_yo")
        self.sets = []
        for i in range(2):
            d = {}
            for nm in ("aT", "rT", "LakT", "MrbT", "MrkT", "QTb", "Bst", "Kst", "Vst"):
                d[nm] = b16("sc_%s%d" % (nm, i))
            d["etT"] = kb.sb([128, 8, 64], F32, "sc_etT%d" % i)
            self.sets.append(d)
        self.bT, self.kT = b16("sc_bT"), b16("sc_kT")
        self.Lp = [b16("sc_L%d" % i) for i in range(2)]
        self.LTp = [b16("sc_LT%d" % i) for i in range(2)]
        self.LTf = kb.sb([128, 8, 64], F32, "sc_LTf")
        self.QTf = kb.sb([128, 8, 64], F32, "sc_QTf")
        self.Xb, self.Pb = b16("sc_Xb"), b16("sc_Pb")
        self.order = (list(range(4)) + list(range(4, NCH))) if z == 0 else ([3, 2, 1, 0] + list(range(NCH - 1, 3, -1)))

    def mm_heads(self, pd, specs, reads):
        kb = self.kb
        for p in range(8):
            for h2 in range(2):
                ps_ = slice(h2 * 64, (h2 + 1) * 64)
                for k, (lt, rt) in enumerate(specs):
                    kb.op("pe", lambda e: e.matmul(pd[ps_, p * 64:(p + 1) * 64], lt[ps_, p, :], rt[ps_, p, :], start=(k == 0), stop=(k == len(specs) - 1)),
                          reads=reads, writes=[pd])

    def prep(self, ci):
        kb, cx, z, D_, nd, MK = self.kb, self.cx, self.z, self.D_, self.nd, self.MK
        identb, identf = cx.ident_bf, cx.ident_f32
        c = self.order[ci]
        c0 = c * 64
        ti = self.tin[ci % 2]
        S = self.sets[ci % 2]
        aT, rT, etT, LakT, MrbT, MrkT, QTb = S["aT"], S["rT"], S["etT"], S["LakT"], S["MrbT"], S["MrkT"], S["QTb"]
        bT, kT, LTf, QTf = self.bT, self.kT, self.LTf, self.QTf
        names = ["AT", "RT", "BT", "KT"]
        for j, nm in enumerate(names):
            kb.dma(ti[j][:], D_[nm][z][c0:c0 + 64, :], reads=[D_[nm][z]], writes=[ti[j]])
        kb.dma(ti[4][:], D_["V"][c0:c0 + 64, :], reads=[D_["V"]], writes=[ti[4]])
        et = self.etin[ci % 2]
        kb.dma(et[:], D_["ETOT"][z][c0:c0 + 64, :], reads=[D_["ETOT"][z]], writes=[et])
        A_, R_, B_, K_, V_ = ti
        for src, dst in ((A_, aT), (R_, rT), (B_, bT), (K_, kT)):
            pd = nd()
            pdb = pd[:].bitcast(BF16)
            for p in range(8):
                kb.op("pe", lambda e: e.transpose(pdb[:, p * 64:(p + 1) * 64], src[:, p * 128:(p + 1) * 128], identb[0:64, 0:64]), reads=[src, identb], writes=[pd])
            kb.op("act", lambda e: e.activation(dst[:].rearrange("p h t -> p (h t)"), pdb[:, 0:512], AF.Copy), reads=[pd], writes=[dst])
            yield
        pd = nd()
        for p in range(8):
            kb.op("pe", lambda e: e.transpose(pd[:, p * 64:(p + 1) * 64], et[:, p * 128:(p + 1) * 128], identf[0:64, 0:64]), reads=[et, identf], writes=[pd])
        kb.op("act", lambda e: e.activation(etT[:].rearrange("p h t -> p (h t)"), pd[:], AF.Copy), reads=[pd], writes=[etT])
        yield
        for src, nm in ((B_, "Bst"), (K_, "Kst"), (V_, "Vst")):
            dst = S[nm]
            pd = nd()
            sv = src[:].rearrange("s (p h2 i) -> s p h2 i", h2=2, i=64)
            for h2 in range(2):
                kb.op("pe", lambda e: e.matmul(pd[:], self.E[:, h2, :], sv[:, :, h2, :], start=(h2 == 0), stop=(h2 == 1)), reads=[src, self.E], writes=[pd])
            kb.op("dve", lambda e: e.tensor_copy(dst[:].rearrange("p h t -> p (h t)"), pd[:]), reads=[pd], writes=[dst])
            yield

        def v3(p):
            return p[:].rearrange("p (h t) -> p h t", h=8)

        def pair(lhsT_t, rhs_t, mask_i, dst, eng="dve"):
            pd = nd()
            self.mm_heads(pd, [(lhsT_t, rhs_t)], [lhsT_t, rhs_t])
            kb.op(eng, lambda e: e.tensor_tensor(dst[:], v3(pd), bc(MK[:, mask_i, :].unsqueeze(1), [128, 8, 64]), ALU.mult), reads=[pd, MK], writes=[dst])
        pair(bT, aT, self.mT_strict, LTf)
        yield
        L1, L1T = self.Lp[0], self.LTp[0]
        pair(aT, bT, self.mL_strict, L1)
        yield
        pair(kT, aT, self.mT_strict, LakT)
        yield
        pair(bT, rT, self.mT_incl, MrbT)
        yield
        pair(kT, rT, self.mT_incl, MrkT)
        kb.op("act", lambda e: e.activation(L1T[:], LTf[:], AF.Copy), reads=[LTf], writes=[L1T])
        kb.op("dve", lambda e: e.tensor_tensor(QTf[:], LTf[:], bc(self.I2[:].unsqueeze(1), [128, 8, 64]), ALU.add), reads=[LTf, self.I2], writes=[QTf])
        kb.op("act", lambda e: e.activation(QTb[:], QTf[:], AF.Copy), reads=[QTf], writes=[QTb])
        yield
        for lvl in range(5):
            L2, L2T = self.Lp[(lvl + 1) % 2], self.LTp[(lvl + 1) % 2]
            pd = nd()
            self.mm_heads(pd, [(L1T, L1)], [L1T, L1])
            kb.op("act", lambda e: e.activation(L2[:].rearrange("p h t -> p (h t)"), pd[:], AF.Copy), reads=[pd], writes=[L2])
            if lvl < 4:
                pd = nd()
                self.mm_heads(pd, [(L1, L1T)], [L1T, L1])
                kb.op("dve", lambda e: e.tensor_copy(L2T[:].rearrange("p h t -> p (h t)"), pd[:]), reads=[pd], writes=[L2T])
            yield
            pd = nd()
            self.mm_heads(pd, [(L2, QTb)], [L2, QTb])
            kb.op("dve", lambda e: e.tensor_tensor(QTf[:].rearrange("p h t -> p (h t)"), QTf[:].rearrange("p h t -> p (h t)"), pd[:], ALU.add), reads=[pd, QTf], writes=[QTf])
            kb.op("act", lambda e: e.activation(QTb[:], QTf[:], AF.Copy), reads=[QTf], writes=[QTb])
            L1, L1T = L2, L2T
            yield

    def seq(self, ci):
        kb, cx, z, D_, nd = self.kb, self.cx, self.z, self.D_, self.nd
        c = self.order[ci]
        is_lat = c >= 4
        c0 = c * 64
        S = self.sets[ci % 2]
        aT, rT, etT, LakT, MrbT, MrkT, QTb = S["aT"], S["rT"], S["etT"], S["LakT"], S["MrbT"], S["MrkT"], S["QTb"]
        Bst, Kst, Vst = S["Bst"], S["Kst"], S["Vst"]
        U0f, U0b, Xb, Pb = self.U0f, self.U0b, self.Xb, self.Pb
        pd = nd()
        self.mm_heads(pd, [(LakT, Vst), (aT, U0b)], [LakT, Vst, aT, U0b])
        kb.op("act", lambda e: e.activation(Xb[:].rearrange("p h t -> p (h t)"), pd[:], AF.Copy), reads=[pd], writes=[Xb])
        yield
        pd = nd()
        self.mm_heads(pd, [(QTb, Xb)], [QTb, Xb])
        kb.op("act", lambda e: e.activation(Pb[:].rearrange("p h t -> p (h t)"), pd[:], AF.Copy), reads=[pd], writes=[Pb])
        yield
        if is_lat:
            pd = nd()
            self.mm_heads(pd, [(rT, U0b), (MrbT, Pb), (MrkT, Vst)], [rT, U0b, MrbT, Pb, MrkT, Vst])
            yo = self.yout
            kb.op("act", lambda e: e.activation(yo[:].rearrange("p h t -> p (h t)"), pd[:], AF.Copy), reads=[pd], writes=[yo])
            yd = D_["Y"][z][c0 - 256:c0 - 192, :].rearrange("t (p h2 i) -> t p h2 i", h2=2, i=64)
            for h2 in range(2):
                kb.dma(yd[:, :, h2, :], yo[h2 * 64:(h2 + 1) * 64, :, :], reads=[yo], writes=[D_["Y"][z]])
            yield
        pd = nd()
        self.mm_heads(pd, [(Bst, Pb), (Kst, Vst)], [Bst, Pb, Kst, Vst])
        kb.op("dve", lambda e: e.tensor_tensor(U0f[:].rearrange("p h t -> p (h t)"), U0f[:].rearrange("p h t -> p (h t)"), pd[:], ALU.add), reads=[pd, U0f], writes=[U0f])
        kb.op("dve", lambda e: e.tensor_tensor(U0f[:], U0f[:], etT[:], ALU.mult), reads=[U0f, etT], writes=[U0f])
        kb.op("act", lambda e: e.activation(U0b[:], U0f[:], AF.Copy), reads=[U0f], writes=[U0b])
        yield


def run_gens(gens):
    gens = list(gens)
    while gens:
        for g in list(gens):
            try:
                next(g)
            except StopIteration:
                gens.remove(g)


def phase_rwkv_scans(kb, cx, D_, consts):
    cx.nd = BankRR(cx.psum)
    sc = [RwkvScan(kb, cx, z, D_, consts) for z in range(2)]
    run_gens([s_.prep(0) for s_ in sc])
    for ci in range(NCH):
        gens = []
        for s_ in sc:
            if ci + 1 < NCH:
                gens.append(s_.prep(ci + 1))
            gens.append(s_.seq(ci))
        run_gens(gens)


def phase_rwkv_readout(kb, cx, w, D_, MIXT):
    ps = cx.psum
    identb = cx.ident_bf
    tabs = kb.sb([128, 2, 1024], F32, "ro_tabs")
    kb.dma(tabs[:, 0, :], w["od_ln_w"][0].partition_broadcast(128), writes=[tabs])
    kb.dma(tabs[:, 1, :], w["od_ln_b"][0].partition_broadcast(128), writes=[tabs])
    epsc = kb.sb([128, 1], F32, "ro_eps")
    kb.op("dve", lambda e: e.memset(epsc[:], 64e-5), writes=[epsc])
    ys = [kb.sb([128, 16, 64], F32, "ro_ys%d" % i) for i in range(2)]
    bo = [kb.sb([128, 1024], F32, "ro_bo%d" % i) for i in range(2)]
    gg = [kb.sb([128, 1024], F32, "ro_g%d" % i) for i in range(2)]
    sq_l = [kb.sb([128, 16, 64], F32, "ro_sq%d" % i) for i in range(2)]
    st_l = [kb.sb([128, 32], F32, "ro_st%d" % i) for i in range(2)]
    ob_l = [kb.sb([128, 1024], BF16, "ro_ob%d" % i) for i in range(2)]
    oT = [kb.sb([128, 8, 128], BF16, "ro_oT%d" % i) for i in range(2)]
    def tile(n):
        rs = slice(n * 128, (n + 1) * 128)
        tsl = slice(256 + n * 128, 256 + (n + 1) * 128)
        y, b_, g_ = ys[n % 2], bo[n % 2], gg[n % 2]
        sq, st, ob = sq_l[n % 2], st_l[n % 2], ob_l[n % 2]
        yv = y[:].rearrange("p h j -> p (h j)")
        kb.dma(yv, D_["Y"][0][rs, :], reads=[D_["Y"][0]], writes=[y])
        kb.dma(sq[:].rearrange("p h j -> p (h j)"), D_["Y"][1][rs, :], reads=[D_["Y"][1]], writes=[sq])
        kb.op("dve", lambda e: e.tensor_tensor(y[:], y[:], sq[:], ALU.add), reads=[y, sq], writes=[y])
        kb.dma(b_[:], D_["BONUS"][tsl, :], reads=[D_["BONUS"]], writes=[b_])
        kb.dma(g_[:], D_["G"][tsl, :], reads=[D_["G"]], writes=[g_])
        yield
        kb.op("dve", lambda e: e.tensor_reduce(st[:, 0:16], y[:], AX.X, ALU.add), reads=[y], writes=[st])
        kb.op("dve", lambda e: e.tensor_scalar(st[:, 0:16], st[:, 0:16], 1.0 / 64, None, ALU.mult), reads=[st], writes=[st])
        kb.op("dve", lambda e: e.tensor_tensor(y[:], y[:], bc(st[:, 0:16].unsqueeze(2), [128, 16, 64]), ALU.subtract), reads=[y, st], writes=[y])
        kb.op("act", lambda e: e.activation(sq[:], y[:], AF.Square), reads=[y], writes=[sq])
        yield
        kb.op("dve", lambda e: e.tensor_reduce(st[:, 16:32], sq[:], AX.X, ALU.add), reads=[sq], writes=[st])
        kb.op("act", lambda e: e.activation(st[:, 16:32], st[:, 16:32], AF.Sqrt, bias=epsc[:, 0:1], scale=1.0 / 64), reads=[st, epsc], writes=[st])
        kb.op("dve", lambda e: e.reciprocal(st[:, 16:32], st[:, 16:32]), reads=[st], writes=[st])
        yield
        kb.op("dve", lambda e: e.tensor_tensor(y[:], y[:], bc(st[:, 16:32].unsqueeze(2), [128, 16, 64]), ALU.mult), reads=[y, st], writes=[y])
        kb.op("dve", lambda e: e.tensor_tensor(yv, yv, tabs[:, 0, :], ALU.mult), reads=[y, tabs], writes=[y])
        yield
        kb.op("pool", lambda e: e.tensor_tensor(b_[:], b_[:], tabs[:, 1, :], ALU.add), reads=[b_, tabs], writes=[b_])
        kb.op("dve", lambda e: e.tensor_tensor(yv, yv, b_[:], ALU.add), reads=[y, b_], writes=[y])
        kb.op("dve", lambda e: e.tensor_tensor(ob[:], yv, g_[:], ALU.mult), reads=[y, g_], writes=[ob])
        yield
        o = oT[n % 2]
        for half in range(2):
            ptr = ps[(2 * n + half) % 4]
            for c in range(4):
                cc = half * 4 + c
                kb.op("pe", lambda e: e.transpose(ptr[:].bitcast(BF16)[:, c * 128:(c + 1) * 128], ob[:, cc * 128:(cc + 1) * 128], identb[:]), reads=[ob, identb], writes=[ptr])
            kb.op("act", lambda e: e.activation(o[:, half * 4:(half + 1) * 4, :].rearrange("p c t -> p (c t)"), ptr[:].bitcast(BF16)[:, 0:512], AF.Copy), reads=[ptr], writes=[o])
        kb.dma(fm(MIXT[:], 256 + n * 128, 128), o[:], reads=[o], writes=[MIXT])
        yield
    for n0 in range(0, 16, 2):
        run_gens([tile(n0), tile(n0 + 1)])


RW_NAMES = ["od_mu", "od_w_rkv", "od_w0", "od_w1", "od_w2", "od_a0", "od_a1", "od_a2", "od_g1", "od_g2", "od_k_k", "od_k_a", "od_r_k",
            "od_ln_w", "od_ln_b", "od_w_o"]


def rwkv_dram(kb):
    D_ = {}
    BIG = kb.dram("rw_BIG", [NTOK, 9, 1024], BF16)
    BIGF = kb.dram("rw_BIGF", [NTOK, 4, 1024], F32)
    D_["BIG"], D_["BIGF"] = BIG, BIGF

    def view(big, slot, name):
        v = T(big.t[:, slot, :], name)
        v.ws, v.rs = big.ws, big.rs
        return v
    D_["V"] = view(BIG, 0, "rw_V")
    for k_, nm in enumerate(("RT", "BT", "KT", "AT")):
        D_[nm] = [view(BIG, 1 + 2 * k_ + z, "rw_%s%d" % (nm, z)) for z in range(2)]
    D_["ETOT"] = [view(BIGF, z, "rw_ETOT%d" % z) for z in range(2)]
    D_["BONUS"] = view(BIGF, 2, "rw_BONUS")
    D_["G"] = view(BIGF, 3, "rw_G")
    D_["Y"] = [kb.dram("rw_Y%d" % z, [NLAT, 1024], F32) for z in range(2)]
    return D_


def layer1_mixer(kb, cx, w, consts, XT1, XT2, MIXT):
    D_ = rwkv_dram(kb)
    HT = kb.dram("rw_HT", [1024, NTOK], BF16)
    with kb.scope():
        Wd = rwkv_weights_alloc(kb)
        with kb.scope():
            hT = kb.sb([128, 8, NTOK], BF16, "hT")
            setup_modbuf(kb, cx)
            kb.set_stage(4, 1024)
            wl = rwkv_weights_load(kb, Wd, w)
            for _ in range(4):
                next(wl, None)
            for ti_ in range(len(TILES)):
                phase_modulate(kb, cx, XT1, 1, w["norm_mix_w"][1], 0, 1, hT, tiles=TILES[ti_:ti_ + 1], first=(ti_ == 0))
                for _ in range(3):
                    next(wl, None)
            for _ in wl:
                pass
            for c in range(8):
                kb.dma(HT[c * 128:(c + 1) * 128, :], hT[:, c, :], reads=[hT], writes=[HT])
        phase_rwkv_feat(kb, cx, HT, w, consts, D_, Wd)
    with kb.scope():
        phase_rwkv_scans(kb, cx, D_, consts)
    with kb.scope():
        phase_rwkv_readout(kb, cx, w, D_, MIXT)
    with kb.scope():
        phase_wout(kb, cx, 1, w["od_w_o"][0], MIXT, XT1, XT2, tiles=TILES[1:])
    return D_


def rwkv_consts():
    c = {}
    s = np.arange(128)[:, None]; t = np.arange(128)[None, :]
    same = (s // 64) == (t // 64)
    c["rw_CM"] = np.stack([same & (s <= t), same & (s < t), same & (s >= t), same & (s > t), same]).astype(np.float32)
    r = np.arange(64)[:, None]; cc = np.arange(64)[None, :]
    c["rw_MK"] = np.stack([r < cc, r <= cc, r > cc, r >= cc]).astype(np.float32)
    c["rw_MK2"] = np.concatenate([c["rw_MK"], c["rw_MK"]], axis=1)
    c["rw_I2"] = np.concatenate([np.eye(64), np.eye(64)], 0).astype(np.float32)
    E = np.zeros((2, 64, 128), np.float32)
    E[0, np.arange(64), np.arange(64)] = 1.0
    E[1, np.arange(64), 64 + np.arange(64)] = 1.0
    c["rw_E"] = E
    return c
```
